# Optimizing a Trainium2 kernel written in Bass

```python
import math
import jax, jax.numpy as jnp
from jax import lax
import numpy as np

D_MODEL = 2048
BATCH = 2
SEQ = 4096
DEPTH = 1

HEAD_DIM = 128
MOBA_HEADS = 8
NSA_HEADS = 8
NSA_KV_HEADS = 2
NSA_GROUP = NSA_HEADS // NSA_KV_HEADS
MOBA_BLOCK = 256
MOBA_TOPK = 3
MOBA_Q_CHUNK = 64
CMP_LEN = 32
CMP_STRIDE = 16
CMP_HIDDEN = 2 * HEAD_DIM
SLC_BLOCK = 64
SLC_TOPK = 16
SLC_Q_CHUNK = 64
WINDOW = 512
SEQ_ALIGN = 512
D_FF = 4 * D_MODEL
CONV_WIDTH = 3
ROPE_THETA = 10000.0
EPS = 1e-6
NEG = -1e30
BIG = 1e9

MOBA_WIDTH = MOBA_HEADS * HEAD_DIM
NSA_WIDTH = NSA_HEADS * HEAD_DIM
KV_WIDTH = NSA_KV_HEADS * HEAD_DIM
IN_SPLITS = [MOBA_WIDTH, MOBA_WIDTH, MOBA_WIDTH, NSA_WIDTH] + [KV_WIDTH] * 6 + [3 * NSA_HEADS]
IN_COLS = 3 * MOBA_WIDTH + NSA_WIDTH + 6 * KV_WIDTH + 3 * NSA_HEADS

kernel_name = "hymba_moba_nsa_convffn_adaln"


def rmsnorm(x, w):
    xf = x.astype(jnp.float32)
    r = xf * lax.rsqrt(jnp.mean(xf * xf, axis=-1, keepdims=True) + EPS)
    return (r * w.astype(jnp.float32)).astype(x.dtype)


def rope(x, positions):
    half = x.shape[-1] // 2
    inv_freq = ROPE_THETA ** (-jnp.arange(half, dtype=jnp.float32) / half)
    ang = positions.astype(jnp.float32)[..., None] * inv_freq
    cos = jnp.cos(ang)[:, :, None, :]
    sin = jnp.sin(ang)[:, :, None, :]
    xf = x.astype(jnp.float32)
    x1, x2 = xf[..., :half], xf[..., half:]
    return jnp.concatenate([x1 * cos - x2 * sin, x2 * cos + x1 * sin], axis=-1).astype(x.dtype)


def moba_attention(q, k, v):
    B, S, H, D = q.shape
    nb = S // MOBA_BLOCK
    scale = D ** -0.5
    q = q.transpose(0, 2, 1, 3)
    kb = k.transpose(0, 2, 1, 3).reshape(B, H, nb, MOBA_BLOCK, D)
    vb = v.transpose(0, 2, 1, 3).reshape(B, H, nb, MOBA_BLOCK, D)
    kmean = jnp.mean(kb.astype(jnp.float32), axis=3)
    gate = jnp.einsum('bhsd,bhnd->bhsn', q.astype(jnp.float32), kmean)
    qblk = jnp.arange(S) // MOBA_BLOCK
    past = jnp.arange(nb)[None, :] < qblk[:, None]
    gate = jnp.where(past, gate, NEG)
    kk = min(MOBA_TOPK, nb)
    _, sel = lax.top_k(gate, kk)
    bi = jnp.arange(B)[:, None, None, None]
    hi = jnp.arange(H)[None, :, None, None]
    qc_len = MOBA_Q_CHUNK

    def chunk(ci):
        start = ci * qc_len
        qc = lax.dynamic_slice_in_dim(q, start, qc_len, axis=2)
        selc = lax.dynamic_slice_in_dim(sel, start, qc_len, axis=2)
        own = start // MOBA_BLOCK
        k_own = lax.dynamic_index_in_dim(kb, own, axis=2, keepdims=False)
        v_own = lax.dynamic_index_in_dim(vb, own, axis=2, keepdims=False)
        k_sel = kb[bi, hi, selc]
        v_sel = vb[bi, hi, selc]
        qpos = start + jnp.arange(qc_len)
        kpos_own = own * MOBA_BLOCK + jnp.arange(MOBA_BLOCK)
        s_own = jnp.einsum('bhqd,bhkd->bhqk', qc, k_own).astype(jnp.float32) * scale
        s_own = jnp.where(kpos_own[None, :] <= qpos[:, None], s_own, NEG)
        s_sel = jnp.einsum('bhqd,bhqnkd->bhqnk', qc, k_sel).astype(jnp.float32) * scale
        valid = jnp.arange(kk)[None, :] < (qpos // MOBA_BLOCK)[:, None]
        s_sel = jnp.where(valid[:, :, None], s_sel, NEG)
        s = jnp.concatenate([s_own, s_sel.reshape(B, H, qc_len, kk * MOBA_BLOCK)], axis=-1)
        p = jax.nn.softmax(s, axis=-1).astype(v.dtype)
        p_sel = p[..., MOBA_BLOCK:].reshape(B, H, qc_len, kk, MOBA_BLOCK)
        return (jnp.einsum('bhqk,bhkd->bhqd', p[..., :MOBA_BLOCK], v_own)
                + jnp.einsum('bhqnk,bhqnkd->bhqd', p_sel, v_sel))

    out = lax.map(chunk, jnp.arange(S // qc_len))
    return out.transpose(1, 0, 3, 2, 4).reshape(B, S, H * D)


def compress(x, pos_emb, w1, w2):
    B, S, Hk, D = x.shape
    nch = S // CMP_STRIDE
    r = CMP_LEN // CMP_STRIDE
    nc = nch - r + 1
    chunks = x.reshape(B, nch, CMP_STRIDE, Hk, D)
    blocks = jnp.concatenate([chunks[:, i:i + nc] for i in range(r)], axis=2)
    blocks = blocks + pos_emb[None, None, :, None, :]
    flat = blocks.transpose(0, 3, 1, 2, 4).reshape(B, Hk, nc, CMP_LEN * D)
    hid = jax.nn.gelu(jnp.einsum('bknf,fh->bknh', flat, w1), approximate=True)
    return jnp.einsum('bknh,hd->bknd', hid, w2)


def nsa_attention(q, k_cmp, v_cmp, k_slc, v_slc, k_win, v_win, gate_logits,
                  cmp_pos_k, cmp_w1_k, cmp_w2_k, cmp_pos_v, cmp_w1_v, cmp_w2_v):
    B, S, H, D = q.shape
    Hk, G = NSA_KV_HEADS, NSA_GROUP
    scale = D ** -0.5
    t = jnp.arange(S)
    qg = q.reshape(B, S, Hk, G, D).transpose(0, 2, 3, 1, 4)

    kc = compress(k_cmp, cmp_pos_k, cmp_w1_k, cmp_w2_k)
    vc = compress(v_cmp, cmp_pos_v, cmp_w1_v, cmp_w2_v)
    nc = kc.shape[2]
    cmp_end = jnp.arange(nc) * CMP_STRIDE + CMP_LEN - 1
    cmp_mask = cmp_end[None, :] <= t[:, None]
    s_cmp = jnp.einsum('bkgsd,bknd->bkgsn', qg, kc).astype(jnp.float32) * scale
    s_cmp = jnp.where(cmp_mask, s_cmp, NEG)
    p_cmp = jnp.where(cmp_mask, jax.nn.softmax(s_cmp, axis=-1), 0.0)
    o_cmp = jnp.einsum('bkgsn,bknd->bkgsd', p_cmp.astype(vc.dtype), vc)

    imp = jnp.sum(p_cmp, axis=2)
    rs, rc = SLC_BLOCK // CMP_STRIDE, CMP_LEN // CMP_STRIDE
    n_slc = S // SLC_BLOCK
    imp_pad = jnp.pad(imp, ((0, 0), (0, 0), (0, 0), (rc - 1, rs)))
    p_slc = 0.0
    for o in range(rs + rc - 1):
        w_o = float(sum(1 for m in range(rs) for n in range(rc) if m - n + rc - 1 == o))
        p_slc = p_slc + w_o * imp_pad[..., o:o + rs * n_slc:rs]
    jt = t // SLC_BLOCK
    j = jnp.arange(n_slc)
    valid = j[None, :] <= jt[:, None]
    forced = (j[None, :] == 0) | (j[None, :] == jt[:, None]) | (j[None, :] == jt[:, None] - 1)
    score = jnp.where(valid & forced, BIG, jnp.where(valid, p_slc, NEG))
    n_sel = min(SLC_TOPK, n_slc)
    _, sel = lax.top_k(score, n_sel)

    ksb = k_slc.transpose(0, 2, 1, 3).reshape(B, Hk, n_slc, SLC_BLOCK, D)
    vsb = v_slc.transpose(0, 2, 1, 3).reshape(B, Hk, n_slc, SLC_BLOCK, D)
    bi = jnp.arange(B)[:, None, None, None]
    hi = jnp.arange(Hk)[None, :, None, None]
    qn = SLC_Q_CHUNK

    def slc_chunk(ci):
        start = ci * qn
        qc = lax.dynamic_slice_in_dim(qg, start, qn, axis=3)
        selc = lax.dynamic_slice_in_dim(sel, start, qn, axis=2)
        kg = ksb[bi, hi, selc]
        vg = vsb[bi, hi, selc]
        s = jnp.einsum('bkgqd,bkqnjd->bkgqnj', qc, kg).astype(jnp.float32) * scale
        kpos = selc[..., None] * SLC_BLOCK + jnp.arange(SLC_BLOCK)
        qpos = start + jnp.arange(qn)
        mask = kpos <= qpos[None, None, :, None, None]
        s = jnp.where(mask[:, :, None], s, NEG)
        p = jax.nn.softmax(s.reshape(B, Hk, G, qn, n_sel * SLC_BLOCK), axis=-1)
        p = p.reshape(s.shape).astype(vg.dtype)
        return jnp.einsum('bkgqnj,bkqnjd->bkgqd', p, vg)

    o_slc = lax.map(slc_chunk, jnp.arange(S // qn))
    o_slc = o_slc.transpose(1, 2, 3, 0, 4, 5).reshape(B, Hk, G, S, D)

    nw = S // WINDOW
    kwb = k_win.transpose(0, 2, 1, 3).reshape(B, Hk, nw, WINDOW, D)
    vwb = v_win.transpose(0, 2, 1, 3).reshape(B, Hk, nw, WINDOW, D)
    pad5 = ((0, 0), (0, 0), (1, 0), (0, 0), (0, 0))
    kcat = jnp.concatenate([jnp.pad(kwb, pad5)[:, :, :-1], kwb], axis=3)
    vcat = jnp.concatenate([jnp.pad(vwb, pad5)[:, :, :-1], vwb], axis=3)
    qw = qg.reshape(B, Hk, G, nw, WINDOW, D)
    s_w = jnp.einsum('bkgnqd,bknjd->bkgnqj', qw, kcat).astype(jnp.float32) * scale
    qi = jnp.arange(WINDOW)[:, None]
    kj = jnp.arange(2 * WINDOW)[None, :] - WINDOW
    band = (kj <= qi) & (kj > qi - WINDOW)
    first = (jnp.arange(nw) == 0)[:, None, None]
    wmask = band[None] & (~first | (kj >= 0)[None])
    s_w = jnp.where(wmask, s_w, NEG)
    p_w = jax.nn.softmax(s_w, axis=-1).astype(vcat.dtype)
    o_win = jnp.einsum('bkgnqj,bknjd->bkgnqd', p_w, vcat).reshape(B, Hk, G, S, D)

    g = jax.nn.sigmoid(gate_logits.astype(jnp.float32)).reshape(B, S, 3, Hk, G)
    g = g.transpose(2, 0, 3, 4, 1)[..., None].astype(q.dtype)
    o = g[0] * o_cmp + g[1] * o_slc + g[2] * o_win
    return o.transpose(0, 3, 1, 2, 4).reshape(B, S, H * D)


def hybrid_mixer(h, positions, w_in, w_out, cmp_pos_k, cmp_w1_k, cmp_w2_k,
                 cmp_pos_v, cmp_w1_v, cmp_w2_v):
    B, S, _ = h.shape
    sp = -(-S // SEQ_ALIGN) * SEQ_ALIGN
    pad = sp - S
    h = jnp.pad(h, ((0, 0), (0, pad), (0, 0)))
    pos = jnp.concatenate(
        [positions, positions[:, -1:] + 1 + jnp.arange(pad, dtype=positions.dtype)[None, :]], axis=1)
    proj = jnp.einsum('bsd,de->bse', h, w_in)
    splits = np.cumsum(IN_SPLITS)[:-1].tolist()
    mq, mk, mv, nq, kc, vc, ks, vs, kw, vw, ng = jnp.split(proj, splits, axis=-1)

    def heads(a, n):
        return a.reshape(B, sp, n, HEAD_DIM)

    o_moba = moba_attention(rope(heads(mq, MOBA_HEADS), pos), rope(heads(mk, MOBA_HEADS), pos),
                            heads(mv, MOBA_HEADS))
    o_nsa = nsa_attention(rope(heads(nq, NSA_HEADS), pos),
                          rope(heads(kc, NSA_KV_HEADS), pos), heads(vc, NSA_KV_HEADS),
                          rope(heads(ks, NSA_KV_HEADS), pos), heads(vs, NSA_KV_HEADS),
                          rope(heads(kw, NSA_KV_HEADS), pos), heads(vw, NSA_KV_HEADS),
                          ng, cmp_pos_k, cmp_w1_k, cmp_w2_k, cmp_pos_v, cmp_w1_v, cmp_w2_v)
    o = jnp.concatenate([o_moba, o_nsa], axis=-1)[:, :S]
    return jnp.einsum('bse,ed->bsd', o, w_out)


def conv_ffn(h, w_up, conv_w, conv_b, w_down):
    u = jnp.einsum('bsd,df->bsf', h, w_up)
    ch = u.shape[-1]
    u = lax.conv_general_dilated(u, conv_w[:, None, :], window_strides=(1,),
                                 padding=((CONV_WIDTH - 1, 0),),
                                 dimension_numbers=('NWC', 'WIO', 'NWC'),
                                 feature_group_count=ch) + conv_b
    gate, val = jnp.split(u, 2, axis=-1)
    return jnp.einsum('bsf,fd->bsd', jax.nn.gelu(gate, approximate=True) * val, w_down)


def setup_inputs(seed: int = 0) -> dict:
    key = jax.random.key(seed)
    ks = jax.random.split(key, 24)
    f32 = jnp.float32

    def nrm(k, shape, fan_in):
        return jax.random.normal(k, shape, f32) * (fan_in ** -0.5)

    def gain(k):
        return 1.0 + 0.1 * jax.random.normal(k, (DEPTH, D_MODEL), f32)

    x = jax.random.normal(ks[0], (BATCH, SEQ, D_MODEL), f32)
    c = jax.random.normal(ks[1], (BATCH, D_MODEL), f32)
    off = jax.random.randint(ks[2], (BATCH,), 0, 1024, dtype=jnp.int32)
    positions = (off[:, None] + jnp.arange(SEQ, dtype=jnp.int32)[None, :]).astype(jnp.int32)
    return {
        "x": x,
        "c": c,
        "positions": positions,
        "w_ada": nrm(ks[3], (DEPTH, D_MODEL, 6 * D_MODEL), D_MODEL),
        "b_ada": 0.01 * jax.random.normal(ks[4], (DEPTH, 6 * D_MODEL), f32),
        "norm_pre_mix": gain(ks[5]),
        "norm_post_mix": gain(ks[6]),
        "norm_pre_ffn": gain(ks[7]),
        "norm_post_ffn": gain(ks[8]),
        "w_in": nrm(ks[9], (DEPTH, D_MODEL, IN_COLS), D_MODEL),
        "w_out": nrm(ks[10], (DEPTH, D_MODEL, D_MODEL), D_MODEL),
        "cmp_pos_k": 0.02 * jax.random.normal(ks[11], (DEPTH, CMP_LEN, HEAD_DIM), f32),
        "cmp_w1_k": nrm(ks[12], (DEPTH, CMP_LEN * HEAD_DIM, CMP_HIDDEN), CMP_LEN * HEAD_DIM),
        "cmp_w2_k": nrm(ks[13], (DEPTH, CMP_HIDDEN, HEAD_DIM), CMP_HIDDEN),
        "cmp_pos_v": 0.02 * jax.random.normal(ks[14], (DEPTH, CMP_LEN, HEAD_DIM), f32),
        "cmp_w1_v": nrm(ks[15], (DEPTH, CMP_LEN * HEAD_DIM, CMP_HIDDEN), CMP_LEN * HEAD_DIM),
        "cmp_w2_v": nrm(ks[16], (DEPTH, CMP_HIDDEN, HEAD_DIM), CMP_HIDDEN),
        "w_up": nrm(ks[17], (DEPTH, D_MODEL, 2 * D_FF), D_MODEL),
        "conv_w": nrm(ks[18], (DEPTH, CONV_WIDTH, 2 * D_FF), CONV_WIDTH),
        "conv_b": 0.01 * jax.random.normal(ks[19], (DEPTH, 2 * D_FF), f32),
        "w_down": nrm(ks[20], (DEPTH, D_FF, D_MODEL), D_FF),
    }


def reference(x, c, positions, w_ada, b_ada, norm_pre_mix, norm_post_mix, norm_pre_ffn,
              norm_post_ffn, w_in, w_out, cmp_pos_k, cmp_w1_k, cmp_w2_k, cmp_pos_v,
              cmp_w1_v, cmp_w2_v, w_up, conv_w, conv_b, w_down):
    for l in range(DEPTH):
        mod = jnp.einsum('bd,de->be', jax.nn.silu(c), w_ada[l]) + b_ada[l]
        sh_a, sc_a, g_a, sh_f, sc_f, g_f = jnp.split(mod, 6, axis=-1)
        h = rmsnorm(x, norm_pre_mix[l]) * (1.0 + sc_a[:, None]) + sh_a[:, None]
        y = hybrid_mixer(h, positions, w_in[l], w_out[l], cmp_pos_k[l], cmp_w1_k[l], cmp_w2_k[l],
                         cmp_pos_v[l], cmp_w1_v[l], cmp_w2_v[l])
        x = x + g_a[:, None] * rmsnorm(y, norm_post_mix[l])
        h = rmsnorm(x, norm_pre_ffn[l]) * (1.0 + sc_f[:, None]) + sh_f[:, None]
        y = conv_ffn(h, w_up[l], conv_w[l], conv_b[l], w_down[l])
        x = x + g_f[:, None] * rmsnorm(y, norm_post_ffn[l])
    return x
```

```python
import math
from contextlib import ExitStack

import numpy as np
import ml_dtypes

import concourse.bass as bass
import concourse.mybir as mybir
from concourse.bass_utils import run_bass_kernel_spmd

F32 = mybir.dt.float32
F32R = mybir.dt.float32r
BF16 = mybir.dt.bfloat16
I32 = mybir.dt.int32
AF = mybir.ActivationFunctionType
ALU = mybir.AluOpType
AX = mybir.AxisListType

D = 2048
S = 4096
NKT = S // 128
NQT = 9
NQ = NQT * 128
HW = 32
KC = D // 128
DFF = 8192
EPS = 1e-6
SCALE = 128 ** -0.5
NDMA = 40


class Buf:
    __slots__ = ("name", "w", "r")

    def __init__(self, name):
        self.name = name
        self.w = {}
        self.r = {}


class Eng:
    def __init__(self, name, h, semid):
        self.name = name
        self.h = h
        self.semid = semid
        self.cnt = 0
        self.seen = {}


class Sched:
    def __init__(self, nc, es):
        self.nc = nc
        self.sems = []
        self.engs = {}
        for name, h in (("pe", nc.tensor), ("act", nc.scalar), ("dve", nc.vector),
                        ("pool", nc.gpsimd), ("sp", nc.sync)):
            sem = es.enter_context(nc.semaphore("s_" + name))
            self.sems.append(sem)
            self.engs[name] = Eng(name, h, len(self.sems) - 1)
        self.dma_semids = []
        self.dma_vals = []
        for i in range(NDMA):
            sem = es.enter_context(nc.semaphore("d_%d" % i))
            self.sems.append(sem)
            self.dma_semids.append(len(self.sems) - 1)
            self.dma_vals.append(0)
        self.dma_i = 0
        self.n_ins = 0
        self.n_waits = 0
        self.snaps = {}
        self.pending = {}

    def _learn(self, eng, sid, v):
        if eng.seen.get(sid, 0) < v:
            eng.seen[sid] = v
        snap = self.snaps.get((sid, v))
        if snap:
            es = eng.seen
            for s2, v2 in snap.items():
                if es.get(s2, 0) < v2:
                    es[s2] = v2

    def _waits(self, eng, reads, writes):
        need = {}
        own = eng.semid
        for b in reads:
            for sid, v in b.w.items():
                if need.get(sid, 0) < v:
                    need[sid] = v
        for b in writes:
            for sid, v in b.w.items():
                if sid != own and need.get(sid, 0) < v:
                    need[sid] = v
            for sid, v in b.r.items():
                if sid != own and need.get(sid, 0) < v:
                    need[sid] = v
        for sid, v in sorted(need.items(), key=lambda kv: -kv[1]):
            if eng.seen.get(sid, 0) < v:
                eng.h.wait_ge(self.sems[sid], v)
                self.n_ins += 1
                self.n_waits += 1
                self._learn(eng, sid, v)

    def op(self, ename, fn, reads=(), writes=(), sig=True):
        eng = self.engs[ename]
        self._waits(eng, reads, writes)
        ins = fn(eng.h)
        self.n_ins += 1
        if sig:
            eng.cnt += 1
            ins.then_inc(self.sems[eng.semid], 1)
            v = eng.cnt
            self.snaps[(eng.semid, v)] = dict(eng.seen)
        else:
            v = eng.cnt + 1
            self.pending[eng.semid] = True
        sid = eng.semid
        for b in reads:
            if b.r.get(sid, 0) < v:
                b.r[sid] = v
        for b in writes:
            if b.w.get(sid, 0) < v:
                b.w[sid] = v
        return ins

    def dma(self, qname, out, in_, reads=(), writes=()):
        eng = self.engs[qname]
        slot = self.dma_i % NDMA
        self.dma_i += 1
        sid = self.dma_semids[slot]
        prev = self.dma_vals[slot]
        if prev > 0 and eng.seen.get(sid, 0) < prev:
            eng.h.wait_ge(self.sems[sid], prev)
            self.n_waits += 1
            self._learn(eng, sid, prev)
        self._waits(eng, reads, writes)
        ins = eng.h.dma_start(out=out, in_=in_)
        ins.then_inc(self.sems[sid], 16)
        self.n_ins += 1
        v = prev + 16
        self.dma_vals[slot] = v
        self.snaps[(sid, v)] = dict(eng.seen)
        for b in reads:
            b.r[sid] = v
        for b in writes:
            b.w[sid] = v
        return ins

    def barrier(self):
        toks = {}
        for e in self.engs.values():
            if e.cnt:
                toks[e.semid] = e.cnt
        for sid, v in zip(self.dma_semids, self.dma_vals):
            if v:
                toks[sid] = v
        sp = self.engs["sp"]
        for sid, v in sorted(toks.items(), key=lambda kv: -kv[1]):
            if sp.seen.get(sid, 0) < v:
                sp.h.wait_ge(self.sems[sid], v)
                self.n_waits += 1
                self._learn(sp, sid, v)
        slot = self.dma_i % NDMA
        self.dma("sp", self.bar_dst, self.bar_src)
        sid = self.dma_semids[slot]
        v = self.dma_vals[slot]
        for e in self.engs.values():
            if e.seen.get(sid, 0) < v:
                e.h.wait_ge(self.sems[sid], v)
                self.n_waits += 1
                self._learn(e, sid, v)

    def wait_all(self, ename, bufs):
        eng = self.engs[ename]
        self._waits(eng, bufs, ())


class Ring:
    def __init__(self, items):
        self.items = items
        self.i = 0

    def next(self):
        it = self.items[self.i % len(self.items)]
        self.i += 1
        return it


def _sb(nc, es, name, shape, dt):
    t = es.enter_context(nc.sbuf_tensor(name, list(shape), dt))
    return t, Buf(name)


def _ring(nc, es, name, shape, dt, n):
    return Ring([_sb(nc, es, "%s%d" % (name, i), shape, dt) for i in range(n)])


def build_program(dbg=False, stop_after=None):
    nc = bass.Bass("TRN2", target_bir_lowering=False)
    es = ExitStack()

    def din(name, shape, dt=F32):
        return nc.dram_tensor(name, list(shape), dt, kind="ExternalInput").ap()

    def dscr(name, shape, dt):
        return nc.dram_tensor(name, list(shape), dt).ap(), Buf(name)

    xall = din("xall", [S, D])
    xq = din("xq", [NQ, D])
    posall = din("posall", [128, NKT], I32)
    posq = din("posq", [128, NQT], I32)
    qpos_col = din("qpos_col", [128, NQT])
    qpos_row = din("qpos_row", [128, NQ])
    qflag_row = din("qflag_row", [128, NQ])
    c_col = din("c_col", [128, KC])
    w_ada = din("w_ada", [D, 6 * D])
    b_ada = din("b_ada", [1, 6 * D])
    n_pre_mix = din("n_pre_mix", [1, D])
    n_post_mix = din("n_post_mix", [1, D])
    n_pre_ffn = din("n_pre_ffn", [1, D])
    n_post_ffn = din("n_post_ffn", [1, D])
    w_k = din("w_k", [D, 3584])
    w_q = din("w_q", [D, 2048 + 24])
    w_out = din("w_out", [D, D])
    ident_f = din("ident_f", [128, 128])
    ident_b = din("ident_b", [128, 128], BF16)
    invf = din("invf", [128, 64])
    consts = din("consts", [128, 1024])
    aind_in = din("aind", [128, NKT * 128], BF16)
    cmp_w1_k = din("cmp_w1_k", [4096, 256])
    cmp_w2_k = din("cmp_w2_k", [256, 128])
    cmp_w1_v = din("cmp_w1_v", [4096, 256])
    cmp_w2_v = din("cmp_w2_v", [256, 128])
    cpos_k = din("cpos_k", [128, 32])
    cpos_v = din("cpos_v", [128, 32])
    w_up_l = din("w_up_l", [D, 64, 256])
    w_down = din("w_down", [DFF, D])
    cw_l = din("cw_l", [128, 128, 3])
    cb_l = din("cb_l", [128, 128])
    out = nc.dram_tensor("out", [1024, D], F32, kind="ExternalOutput").ap()
    out_buf = Buf("out")
    dbg_aps = {}
    if dbg:
        for nm, shp, dt in (("d_mod", [1, 6 * D], F32), ("d_hT", [128, KC, 256], BF16),
                            ("d_kT", [16, 128, S], BF16), ("d_V", [S, 1536], BF16),
                            ("d_qT", [128, 16, NQ], BF16), ("d_gl", [128, NQT, 24], F32),
                            ("d_oT", [128, 16, NQ], BF16)):
            dbg_aps[nm] = nc.dram_tensor(nm, shp, dt, kind="ExternalOutput").ap()

    kT_d, kT_db = dscr("kT_d", [16, 128, S], BF16)
    V_d, V_db = dscr("V_d", [S, 1536], BF16)

    SC = Sched(nc, es)
    op, dma = SC.op, SC.dma
    SC.bar_dst = nc.dram_tensor("bar_d", [1, 16], F32).ap()[0:1, :]
    SC.bar_src = b_ada[0:1, 0:16]

    banks = []
    for i in range(8):
        t = es.enter_context(nc.psum_tensor("ps%d" % i, [128, 512], F32))
        banks.append((t, Buf("ps%d" % i)))
    pring = Ring(banks)

    P = ExitStack()
    es.enter_context(P)
    mod_d, mod_db = dscr("mod_d", [1, 6 * D], F32)
    idf, idf_b = _sb(nc, P, "idf", [128, 128], F32)
    idb, idb_b = _sb(nc, P, "idb", [128, 128], BF16)
    ones_f, ones_fb = _sb(nc, P, "ones_f", [128, 128], F32)
    cst, cst_b = _sb(nc, P, "cst", [128, 1024], F32)
    dma("sp", idf[:], ident_f[:, :], writes=[idf_b])
    dma("sp", idb[:], ident_b[:, :], writes=[idb_b])
    dma("sp", cst[:], consts[:, :], writes=[cst_b])
    op("pool", lambda e: e.memset(ones_f[:], 1.0), writes=[ones_fb])

    with ExitStack() as s0:
        cc, cc_b = _sb(nc, s0, "cc", [128, KC], F32)
        sT, sT_b = _sb(nc, s0, "sT", [128, KC], BF16)
        brow, brow_b = _sb(nc, s0, "brow", [1, 6 * D], F32)
        mrow = _ring(nc, s0, "mrow", [1, 512], F32, 2)
        war = _ring(nc, s0, "wa", [128, KC, 512], BF16, 3)
        dma("sp", cc[:], c_col[:, :], writes=[cc_b])
        dma("sp", brow[:], b_ada[:, :], writes=[brow_b])
        op("act", lambda e: e.activation(out=sT[:], in_=cc[:], func=AF.Silu), reads=[cc_b], writes=[sT_b])
        for g in range(24):
            wa, wa_b = war.next()
            src = w_ada[:, g * 512:(g + 1) * 512].rearrange("(k p) c -> p k c", p=128)
            for h2 in range(4):
                dma("pool", wa[:, h2 * 4:(h2 + 1) * 4, :], src[:, h2 * 4:(h2 + 1) * 4, :], writes=[wa_b])
            ps, ps_b = pring.next()
            for k in range(KC):
                op("pe", lambda e, k=k: e.matmul(ps[0:1, :], lhsT=sT[:, k:k + 1],
                                                 rhs=wa[:, k, :],
                                                 start=(k == 0), stop=(k == KC - 1)),
                   reads=[sT_b, wa_b], writes=[ps_b], sig=(k == KC - 1))
            mr, mr_b = mrow.next()
            op("dve", lambda e: e.tensor_tensor(out=mr[0:1, :], in0=ps[0:1, :],
                                                in1=brow[0:1, g * 512:(g + 1) * 512], op=ALU.add),
               reads=[ps_b, brow_b], writes=[mr_b])
            dma("sp", mod_d[0:1, g * 512:(g + 1) * 512], mr[0:1, :], reads=[mr_b], writes=[mod_db])
        if dbg:
            SC.barrier()
            dma("sp", dbg_aps["d_mod"][:, :], mod_d[:, :], reads=[mod_db], writes=[out_buf])
        SC.barrier()

    def bcast_mod(dst, dst_b, idx, wrow_ap, plus_one, tmp, tmp_b):
        dma("sp", dst[:], mod_d[0:1, idx * D:(idx + 1) * D].to_broadcast([128, D]), reads=[mod_db], writes=[dst_b])
        if wrow_ap is not None:
            dma("act", tmp[:], wrow_ap.to_broadcast([128, D]), writes=[tmp_b])
            op("dve", lambda e: e.scalar_tensor_tensor(out=dst[:], in0=dst[:], scalar=(1.0 if plus_one else 0.0),
                                                       in1=tmp[:], op0=ALU.add, op1=ALU.mult),
               reads=[dst_b, tmp_b], writes=[dst_b])

    def _norm_tile(src_ap, src_bufs, wm, wm_b, sh, sh_b, dstT, dstT_b, col0, xr, tr, hbr, st):
        x, x_b = xr.next()
        dma("sp", x[:, 0:1024], src_ap[:, 0:1024], reads=src_bufs, writes=[x_b])
        dma("act", x[:, 1024:2048], src_ap[:, 1024:2048], reads=src_bufs, writes=[x_b])
        s4, s4_b = st.next()
        hb, hb_b = hbr.next()
        op("act", lambda e: e.activation(out=hb[:], in_=x[:], func=AF.Square, accum_out=s4[:, 0:1]),
           reads=[x_b], writes=[hb_b, s4_b])
        op("dve", lambda e: e.tensor_scalar(out=s4[:, 1:2], in0=s4[:, 0:1], scalar1=1.0 / D, scalar2=EPS,
                                            op0=ALU.mult, op1=ALU.add), reads=[s4_b], writes=[s4_b])
        op("act", lambda e: e.activation(out=s4[:, 2:3], in_=s4[:, 1:2], func=AF.Sqrt), reads=[s4_b], writes=[s4_b])
        op("dve", lambda e: e.reciprocal(out=s4[:, 3:4], in_=s4[:, 2:3]), reads=[s4_b], writes=[s4_b])
        t, t_b = tr.next()
        op("dve", lambda e: e.scalar_tensor_tensor(out=t[:], in0=x[:], scalar=s4[:, 3:4], in1=wm[:],
                                                   op0=ALU.mult, op1=ALU.mult),
           reads=[x_b, s4_b, wm_b], writes=[t_b])
        op("pool", lambda e: e.tensor_tensor(out=hb[:], in0=t[:], in1=sh[:], op=ALU.add),
           reads=[t_b, sh_b], writes=[hb_b])
        for c4 in range(4):
            ps, ps_b = pring.next()
            psb = ps[:, :].bitcast(BF16)
            for i in range(4):
                k = c4 * 4 + i
                op("pe", lambda e, k=k, i=i: e.transpose(out=psb[:, i * 128:(i + 1) * 128], in_=hb[:, k * 128:(k + 1) * 128],
                                                         identity=idb[:]),
                   reads=[hb_b, idb_b], writes=[ps_b], sig=(i == 3))
            dst = dstT[:, c4 * 4:(c4 + 1) * 4, col0:col0 + 128]
            srcv = psb[:, 0:512].rearrange("p (a b) -> p a b", a=4)
            if c4 % 2 == 0:
                op("act", lambda e: e.copy(out=dst, in_=srcv), reads=[ps_b], writes=[dstT_b])
            else:
                op("dve", lambda e: e.tensor_copy(out=dst, in_=srcv), reads=[ps_b], writes=[dstT_b])

    A = ExitStack()
    qT, qT_b = _sb(nc, A, "qT", [128, 16, NQ], BF16)
    glog, glog_b = _sb(nc, A, "glog", [128, NQT, 24], F32)

    with ExitStack() as S1:
        hT, hT_b = _sb(nc, S1, "hT", [128, KC, NQ], BF16)
        wmod, wmod_b = _sb(nc, S1, "wmod", [128, D], F32)
        shm, shm_b = _sb(nc, S1, "shm", [128, D], F32)
        xr = _ring(nc, S1, "xr", [128, D], F32, 2)
        tr = _ring(nc, S1, "tr", [128, D], F32, 1)
        hbr = _ring(nc, S1, "hbr", [128, D], BF16, 2)
        st = _ring(nc, S1, "st", [128, 4], F32, 3)
        bcast_mod(wmod, wmod_b, 1, n_pre_mix[0:1, :], True, tr.items[0][0], tr.items[0][1])
        bcast_mod(shm, shm_b, 0, None, False, None, None)

        def norm_tile(src_ap, wm, wm_b, sh, sh_b, dstT, dstT_b, col0):
            _norm_tile(src_ap, [], wm, wm_b, sh, sh_b, dstT, dstT_b, col0, xr, tr, hbr, st)

        def _unused():
            x, x_b = xr.next()
            s4, s4_b = st.next()
            hb, hb_b = hbr.next()
            op("act", lambda e: e.activation(out=hb[:], in_=x[:], func=AF.Square, accum_out=s4[:, 0:1]),
               reads=[x_b], writes=[hb_b, s4_b])
            op("dve", lambda e: e.tensor_scalar(out=s4[:, 1:2], in0=s4[:, 0:1], scalar1=1.0 / D, scalar2=EPS,
                                                op0=ALU.mult, op1=ALU.add), reads=[s4_b], writes=[s4_b])
            op("act", lambda e: e.activation(out=s4[:, 2:3], in_=s4[:, 1:2], func=AF.Sqrt), reads=[s4_b], writes=[s4_b])
            op("dve", lambda e: e.reciprocal(out=s4[:, 3:4], in_=s4[:, 2:3]), reads=[s4_b], writes=[s4_b])
            t, t_b = tr.next()
            op("dve", lambda e: e.scalar_tensor_tensor(out=t[:], in0=x[:], scalar=s4[:, 3:4], in1=wm[:],
                                                       op0=ALU.mult, op1=ALU.mult),
               reads=[x_b, s4_b, wm_b], writes=[t_b])
            op("pool", lambda e: e.tensor_tensor(out=hb[:], in0=t[:], in1=sh[:], op=ALU.add),
               reads=[t_b, sh_b], writes=[hb_b])
            for c4 in range(4):
                ps, ps_b = pring.next()
                psb = ps[:, :].bitcast(BF16)
                for i in range(4):
                    k = c4 * 4 + i
                    op("pe", lambda e, k=k, i=i: e.transpose(out=psb[:, i * 128:(i + 1) * 128], in_=hb[:, k * 128:(k + 1) * 128],
                                                             identity=idb[:]),
                       reads=[hb_b, idb_b], writes=[ps_b], sig=(i == 3))
                dst = dstT[:, c4 * 4:(c4 + 1) * 4, col0:col0 + 128]
                srcv = psb[:, 0:512].rearrange("p (a b) -> p a b", a=4)
                if c4 % 2 == 0:
                    op("act", lambda e: e.copy(out=dst, in_=srcv), reads=[ps_b], writes=[dstT_b])
                else:
                    op("dve", lambda e: e.tensor_copy(out=dst, in_=srcv), reads=[ps_b], writes=[dstT_b])

        cosK, cosK_b = _sb(nc, S1, "cosK", [128, NQT, 64], F32)
        sinK, sinK_b = _sb(nc, S1, "sinK", [128, NQT, 64], F32)
        pi_, pi_b = _sb(nc, S1, "posi", [128, NKT + NQT], I32)
        pf, pf_b = _sb(nc, S1, "posf", [128, NKT + NQT], F32)
        ivf, ivf_b = _sb(nc, S1, "ivf", [128, 64], F32)
        angr = _ring(nc, S1, "ang", [128, 64], F32, 6)
        dma("sp", pi_[:, 0:NKT], posall[:, :], writes=[pi_b])
        dma("sp", pi_[:, NKT:NKT + NQT], posq[:, :], writes=[pi_b])
        dma("sp", ivf[:], invf[:, :], writes=[ivf_b])
        op("dve", lambda e: e.tensor_copy(out=pf[:], in_=pi_[:]), reads=[pi_b], writes=[pf_b])
        negpi, negpi_b = _sb(nc, S1, "negpi", [128, 1], F32)
        op("pool", lambda e: e.memset(negpi[:], -math.pi), writes=[negpi_b])

        def rope_tables(tt0, n):
            MAGIC = 12582912.0
            for i in range(n):
                t = tt0 + i
                a, a_b = angr.next()
                op("dve", lambda e: e.tensor_scalar(out=a[:], in0=ivf[:], scalar1=pf[:, t:t + 1], scalar2=None, op0=ALU.mult),
                   reads=[ivf_b, pf_b], writes=[a_b])
                for (shift, dst, dst_b) in ((0.5 * math.pi, cosK, cosK_b), (0.0, sinK, sinK_b)):
                    a1, a1_b = angr.next()
                    a2, a2_b = angr.next()
                    op("dve", lambda e: e.tensor_scalar(out=a1[:], in0=a[:], scalar1=shift, scalar2=None, op0=ALU.add),
                       reads=[a_b], writes=[a1_b])
                    op("dve", lambda e: e.tensor_scalar(out=a2[:], in0=a1[:], scalar1=1.0 / (2 * math.pi), scalar2=MAGIC,
                                                        op0=ALU.mult, op1=ALU.add), reads=[a1_b], writes=[a2_b])
                    op("dve", lambda e: e.tensor_scalar(out=a2[:], in0=a2[:], scalar1=MAGIC, scalar2=None, op0=ALU.subtract),
                       reads=[a2_b], writes=[a2_b])
                    op("dve", lambda e: e.scalar_tensor_tensor(out=a1[:], in0=a2[:], scalar=-2 * math.pi, in1=a1[:],
                                                               op0=ALU.mult, op1=ALU.add), reads=[a2_b, a1_b], writes=[a1_b])
                    op("dve", lambda e: e.tensor_scalar(out=a1[:], in0=a1[:], scalar1=3.14159, scalar2=-3.14159,
                                                        op0=ALU.min, op1=ALU.max), reads=[a1_b], writes=[a1_b])
                    op("act", lambda e: e.activation(out=dst[:, i, :], in_=a1[:], func=AF.Sin),
                       reads=[a1_b], writes=[dst_b])

        wr = _ring(nc, S1, "wr", [128, KC, 512], BF16, 2)
        ta = _ring(nc, S1, "ta", [128, 4, 64], F32, 2)
        tb = _ring(nc, S1, "tb", [128, 4, 64], F32, 2)
        tc_ = _ring(nc, S1, "tc", [128, 4, 64], F32, 2)
        td = _ring(nc, S1, "td", [128, 4, 64], F32, 2)
        krr = _ring(nc, S1, "kr", [128, 4, 128], BF16, 2)
        kst = _ring(nc, S1, "kst", [128, 4, 512], BF16, 2)
        vst = _ring(nc, S1, "vst", [128, 512], BF16, 3)

        def load_w(src_cols, ncols=512):
            w, w_b = wr.next()
            for h2 in range(4):
                dma("pool", w[:, h2 * 4:(h2 + 1) * 4, 0:ncols],
                    src_cols[h2 * 512:(h2 + 1) * 512, :].rearrange("(k p) c -> p k c", p=128), writes=[w_b])
            return w, w_b

        def proj_tile(col0, w, w_b, ncols=512):
            ps, ps_b = pring.next()
            for k in range(KC):
                op("pe", lambda e, k=k: e.matmul(ps[:, 0:ncols], lhsT=hT[:, k, col0:col0 + 128], rhs=w[:, k, 0:ncols],
                                                 start=(k == 0), stop=(k == KC - 1)),
                   reads=[hT_b, w_b], writes=[ps_b], sig=(k == KC - 1))
            return ps, ps_b

        def rope4(ps, ps_b, ti, nrope):
            kr, kr_b = krr.next()
            pv = ps[:, :].rearrange("p (h t d) -> p h t d", h=4, t=2)
            krv = kr[:, :, :].rearrange("p h (t d) -> p h t d", t=2)
            if nrope > 0:
                n = nrope
                cs = cosK[:, ti:ti + 1, :].to_broadcast([128, n, 64])
                sn = sinK[:, ti:ti + 1, :].to_broadcast([128, n, 64])
                a, a_b = ta.next(); b, b_b = tb.next(); c, c_b = tc_.next(); d, d_b = td.next()
                op("dve", lambda e: e.tensor_tensor(out=a[:, 0:n, :], in0=pv[:, 0:n, 0, :], in1=cs, op=ALU.mult),
                   reads=[ps_b, cosK_b], writes=[a_b])
                op("dve", lambda e: e.tensor_tensor(out=b[:, 0:n, :], in0=pv[:, 0:n, 1, :], in1=sn, op=ALU.mult),
                   reads=[ps_b, sinK_b], writes=[b_b])
                op("dve", lambda e: e.tensor_tensor(out=c[:, 0:n, :], in0=pv[:, 0:n, 1, :], in1=cs, op=ALU.mult),
                   reads=[ps_b, cosK_b], writes=[c_b])
                op("dve", lambda e: e.tensor_tensor(out=d[:, 0:n, :], in0=pv[:, 0:n, 0, :], in1=sn, op=ALU.mult),
                   reads=[ps_b, sinK_b], writes=[d_b])
                op("pool", lambda e: e.tensor_tensor(out=krv[:, 0:n, 0, :], in0=a[:, 0:n, :], in1=b[:, 0:n, :], op=ALU.subtract),
                   reads=[a_b, b_b], writes=[kr_b])
                op("pool", lambda e: e.tensor_tensor(out=krv[:, 0:n, 1, :], in0=c[:, 0:n, :], in1=d[:, 0:n, :], op=ALU.add),
                   reads=[c_b, d_b], writes=[kr_b])
            if nrope < 4:
                op("act", lambda e: e.copy(out=kr[:, nrope:4, :], in_=ps[:, nrope * 128:512].rearrange("p (h d) -> p h d", h=4 - nrope)),
                   reads=[ps_b], writes=[kr_b])
            return kr, kr_b

        def transpose4(kr, kr_b, dst, dst_b):
            ps, ps_b = pring.next()
            psb = ps[:, :].bitcast(BF16)
            for i in range(4):
                op("pe", lambda e, i=i: e.transpose(out=psb[:, i * 128:(i + 1) * 128], in_=kr[:, i, :], identity=idb[:]),
                   reads=[kr_b, idb_b], writes=[ps_b], sig=(i == 3))
            op("act", lambda e: e.copy(out=dst, in_=psb[:, 0:512].rearrange("p (a b) -> p a b", a=4)),
               reads=[ps_b], writes=[dst_b])

        for qtr in range(4):
            for t in range(8):
                tg = qtr * 8 + t
                norm_tile(xall[tg * 128:(tg + 1) * 128, :], wmod, wmod_b, shm, shm_b, hT, hT_b, t * 128)
            rope_tables(qtr * 8, 8)
            if dbg and qtr == 0:
                dma("sp", dbg_aps["d_hT"][:, :, 0:128], hT[:, :, 128:256], reads=[hT_b], writes=[out_buf])
            if stop_after == "norm":
                break
            for cg in range(7):
                w, w_b = load_w(w_k[:, cg * 512:(cg + 1) * 512])
                for t in range(8):
                    tg = qtr * 8 + t
                    ps, ps_b = proj_tile(t * 128, w, w_b)
                    if cg < 4:
                        nrope = 4 if cg < 3 else 2
                        kr, kr_b = rope4(ps, ps_b, t, nrope)
                        if t % 4 == 0:
                            ks, ks_b = kst.next()
                        transpose4(kr, kr_b, ks[:, :, (t % 4) * 128:(t % 4 + 1) * 128], ks_b)
                        if t % 4 == 3:
                            t0 = (tg // 4) * 512
                            dma("sp", kT_d[cg * 4:(cg + 1) * 4, :, t0:t0 + 512].rearrange("s d t -> d s t"), ks[:, :, :],
                                reads=[ks_b], writes=[kT_db])
                    else:
                        v, v_b = vst.next()
                        op("act", lambda e: e.copy(out=v[:], in_=ps[:, :]), reads=[ps_b], writes=[v_b])
                        dma("sp", V_d[tg * 128:(tg + 1) * 128, (cg - 4) * 512:(cg - 3) * 512], v[:], reads=[v_b], writes=[V_db])
        for t in range(NQT):
            norm_tile(xq[t * 128:(t + 1) * 128, :], wmod, wmod_b, shm, shm_b, hT, hT_b, t * 128)
        rope_tables(NKT, NQT)
        if dbg:
            dma("sp", dbg_aps["d_hT"][:, :, 128:256], hT[:, :, 0:128], reads=[hT_b], writes=[out_buf])
        if stop_after != "norm":
            for cg in range(4):
                w, w_b = load_w(w_q[:, cg * 512:(cg + 1) * 512])
                for t in range(NQT):
                    ps, ps_b = proj_tile(t * 128, w, w_b)
                    kr, kr_b = rope4(ps, ps_b, t, 4)
                    transpose4(kr, kr_b, qT[:, cg * 4:(cg + 1) * 4, t * 128:(t + 1) * 128], qT_b)
            wg, wg_b = load_w(w_q[:, 2048:2072], ncols=24)
            for t in range(NQT):
                ps, ps_b = proj_tile(t * 128, wg, wg_b, ncols=24)
                op("act", lambda e: e.copy(out=glog[:, t, :], in_=ps[:, 0:24]), reads=[ps_b], writes=[glog_b])
            if dbg:
                SC.barrier()
                dma("sp", dbg_aps["d_kT"][:, :, :], kT_d[:, :, :], reads=[kT_db], writes=[out_buf])
                dma("sp", dbg_aps["d_V"][:, :], V_d[:, :], reads=[V_db], writes=[out_buf])
                dma("sp", dbg_aps["d_qT"][:, :, :], qT[:], reads=[qT_b], writes=[out_buf])
                dma("sp", dbg_aps["d_gl"][:, :, :], glog[:], reads=[glog_b], writes=[out_buf])
        SC.barrier()
    if stop_after in ("norm", "proj"):
        return _finish(nc, es, SC, out_buf)

    pringA = Ring(banks[0:4])
    pringB = Ring(banks[4:8])
    oT_d, oT_db = dscr("oT_d", [16, 128, NQ], BF16)
    x1_d, x1_db = dscr("x1_d", [NQ, D], F32)
    GROUPS = ((0, 512, (0, 1, 2, 3), 16), (512, 512, (4, 5, 6, 7), NKT), (1024, HW, (8,), NKT))
    with ExitStack() as S4:
        qpc, qpc_b = _sb(nc, S4, "qpc", [128, NQT], F32)
        qpr, qpr_b = _sb(nc, S4, "qpr", [128, NQ], F32)
        cm, cm_b = _sb(nc, S4, "cm", [128, NKT, 1056], BF16)
        ones_b, ones_bb = _sb(nc, S4, "ones_b", [128, 128], BF16)
        dma("sp", qpc[:], qpos_col[:, :], writes=[qpc_b])
        dma("sp", qpr[:], qpos_row[:, :], writes=[qpr_b])
        op("pool", lambda e: e.memset(ones_b[:], 1.0), writes=[ones_bb])
        for c in range(NKT):
            op("dve", lambda e, c=c: e.tensor_scalar(out=cm[:, c, :], in0=qpr[:, 0:1056], scalar1=cst[:, c:c + 1], scalar2=None,
                                                     op0=ALU.is_ge), reads=[qpr_b, cst_b], writes=[cm_b])
        kTr = _ring(nc, S4, "kTt", [128, S], BF16, 2)
        Vr = _ring(nc, S4, "Vt", [128, NKT, 129], BF16, 2)
        for (v, v_b) in Vr.items:
            op("pool", lambda e, v=v: e.memset(v[:, :, 128:129], 1.0), writes=[v_b])
        ptr = _ring(nc, S4, "pt", [128, 512], BF16, 5)
        m8r = _ring(nc, S4, "m8", [128, 8], F32, 6)
        ncr = _ring(nc, S4, "ncr", [128, 4], F32, 2)
        obr = _ring(nc, S4, "ob", [128, 128], BF16, 3)
        ostr = _ring(nc, S4, "ost", [128, 128], BF16, 4)
        rvr = _ring(nc, S4, "rv", [128, 4], F32, 6)

        def load_k(kslot):
            kT_t, kT_tb = kTr.next()
            for h4 in range(4):
                dma("sp", kT_t[:, h4 * 1024:(h4 + 1) * 1024], kT_d[kslot, :, h4 * 1024:(h4 + 1) * 1024], reads=[kT_db], writes=[kT_tb])
            return kT_t, kT_tb

        def load_v(vslot):
            V_t, V_tb = Vr.next()
            for h4 in range(4):
                dma("act", V_t[:, h4 * 8:(h4 + 1) * 8, 0:128],
                    V_d[h4 * 1024:(h4 + 1) * 1024, vslot * 128:(vslot + 1) * 128].rearrange("(c p) d -> p c d", p=128),
                    reads=[V_db], writes=[V_tb])
            return V_t, V_tb

        def shift_const(kT_t, kT_tb, qh):
            nct, nct_b = ncr.next()
            m8a, m8a_b = m8r.next()
            m8b, m8b_b = m8r.next()
            for g in range(8):
                sq_, sq_b = ptr.next()
                op("act", lambda e: e.activation(out=sq_[:], in_=kT_t[:, g * 512:(g + 1) * 512], func=AF.Square), reads=[kT_tb], writes=[sq_b])
                ps, ps_b = pringA.next()
                op("pe", lambda e: e.matmul(ps[:, :], lhsT=ones_b[:], rhs=sq_[:], start=True, stop=True),
                   reads=[ones_bb, sq_b], writes=[ps_b])
                op("dve", lambda e: e.tensor_reduce(out=m8a[:, g:g + 1], in_=ps[:, :], axis=AX.X, op=ALU.max),
                   reads=[ps_b], writes=[m8a_b])
            op("dve", lambda e: e.tensor_reduce(out=nct[:, 0:1], in_=m8a[:], axis=AX.X, op=ALU.max), reads=[m8a_b], writes=[nct_b])
            for g, (c0, wd) in enumerate(((0, 512), (512, 512), (1024, 128))):
                sq_, sq_b = ptr.next()
                op("act", lambda e: e.activation(out=sq_[:, 0:wd], in_=qT[:, qh, c0:c0 + wd], func=AF.Square), reads=[qT_b], writes=[sq_b])
                ps, ps_b = pringA.next()
                op("pe", lambda e: e.matmul(ps[:, 0:wd], lhsT=ones_b[:], rhs=sq_[:, 0:wd], start=True, stop=True),
                   reads=[ones_bb, sq_b], writes=[ps_b])
                op("dve", lambda e: e.tensor_reduce(out=m8b[:, g:g + 1], in_=ps[:, 0:wd], axis=AX.X, op=ALU.max),
                   reads=[ps_b], writes=[m8b_b])
            op("dve", lambda e: e.tensor_reduce(out=nct[:, 1:2], in_=m8b[:, 0:3], axis=AX.X, op=ALU.max), reads=[m8b_b], writes=[nct_b])
            op("dve", lambda e: e.tensor_tensor(out=nct[:, 2:3], in0=nct[:, 0:1], in1=nct[:, 1:2], op=ALU.mult), reads=[nct_b], writes=[nct_b])
            op("act", lambda e: e.activation(out=nct[:, 3:4], in_=nct[:, 2:3], func=AF.Sqrt), reads=[nct_b], writes=[nct_b])
            op("dve", lambda e: e.tensor_scalar(out=nct[:, 0:1], in0=nct[:, 3:4], scalar1=-SCALE * 1.02, scalar2=None, op0=ALU.mult),
               reads=[nct_b], writes=[nct_b])
            return nct, nct_b

        def emit_oT(ob, ob_b, rows, slot, t):
            ps, ps_b = pringA.next()
            psb = ps[:, :].bitcast(BF16)
            op("pe", lambda e: e.transpose(out=psb[:, 0:128], in_=ob[:, :], identity=idb[:, :]),
               reads=[ob_b, idb_b], writes=[ps_b])
            ost, ost_b = ostr.next()
            op("act", lambda e: e.copy(out=ost[:, 0:128], in_=psb[:, 0:128]), reads=[ps_b], writes=[ost_b])
            dma("sp", oT_d[slot, :, t * 128:t * 128 + rows], ost[:, 0:rows], reads=[ost_b], writes=[oT_db])

        with ExitStack() as S4a:
            accr = _ring(nc, S4a, "acc", [128, 129], F32, 9)
            smr = _ring(nc, S4a, "sm", [128, 16], F32, 8)
            selr = _ring(nc, S4a, "sel", [128, NQT, 16], F32, 2)
            kmr = _ring(nc, S4a, "km", [128, 16], F32, 2)
            kmhr = _ring(nc, S4a, "kmh", [128, 16], BF16, 2)
            kmlr = _ring(nc, S4a, "kml", [128, 16], BF16, 2)
            for hd in range(8 if stop_after != "nsa_only" else 0):
                kT_t, kT_tb = load_k(hd)
                V_t, V_tb = load_v(hd)
                nct, nct_b = shift_const(kT_t, kT_tb, hd)
                km, km_b = kmr.next(); kmh, kmh_b = kmhr.next(); kml, kml_b = kmlr.next()
                op("dve", lambda e: e.tensor_reduce(out=km[:], in_=kT_t[:, :].rearrange("p (n k) -> p n k", n=16), axis=AX.X, op=ALU.add),
                   reads=[kT_tb], writes=[km_b])
                op("dve", lambda e: e.tensor_scalar(out=km[:], in0=km[:], scalar1=1.0 / 256, scalar2=None, op0=ALU.mult), reads=[km_b], writes=[km_b])
                op("dve", lambda e: e.tensor_copy(out=kmh[:], in_=km[:]), reads=[km_b], writes=[kmh_b])
                op("dve", lambda e: e.tensor_tensor(out=kml[:], in0=km[:], in1=kmh[:], op=ALU.subtract), reads=[km_b, kmh_b], writes=[kml_b])
                sel, sel_b = selr.next()
                for t in range(NQT):
                    ps, ps_b = pringA.next()
                    op("pe", lambda e: e.matmul(ps[:, 0:16], lhsT=qT[:, hd, t * 128:(t + 1) * 128], rhs=kmh[:], start=True, stop=False),
                       reads=[qT_b, kmh_b], writes=[ps_b], sig=False)
                    op("pe", lambda e: e.matmul(ps[:, 0:16], lhsT=qT[:, hd, t * 128:(t + 1) * 128], rhs=kml[:], start=False, stop=True),
                       reads=[qT_b, kml_b], writes=[ps_b])
                    past, past_b = smr.next(); own, own_b = smr.next(); t1, t1_b = smr.next(); gm, gm_b = smr.next()
                    m8, m8_b = m8r.next()
                    qc = qpc[:, t:t + 1]
                    op("dve", lambda e: e.tensor_scalar(out=past[:], in0=cst[:, 32:48], scalar1=qc, scalar2=None, op0=ALU.is_le),
                       reads=[cst_b, qpc_b], writes=[past_b])
                    op("dve", lambda e: e.tensor_scalar(out=own[:], in0=cst[:, 48:64], scalar1=qc, scalar2=None, op0=ALU.is_le),
                       reads=[cst_b, qpc_b], writes=[own_b])
                    op("dve", lambda e: e.tensor_tensor(out=own[:], in0=own[:], in1=past[:], op=ALU.subtract), reads=[own_b, past_b], writes=[own_b])
                    op("dve", lambda e: e.tensor_scalar(out=t1[:], in0=past[:], scalar1=1e30, scalar2=-1e30, op0=ALU.mult, op1=ALU.add),
                       reads=[past_b], writes=[t1_b])
                    op("dve", lambda e: e.tensor_tensor(out=gm[:], in0=ps[:, 0:16], in1=past[:], op=ALU.mult), reads=[ps_b, past_b], writes=[gm_b])
                    op("dve", lambda e: e.tensor_tensor(out=gm[:], in0=gm[:], in1=t1[:], op=ALU.add), reads=[gm_b, t1_b], writes=[gm_b])
                    op("dve", lambda e: e.max(out=m8[:], in_=gm[:]), reads=[gm_b], writes=[m8_b])
                    op("dve", lambda e: e.tensor_scalar(out=t1[:], in0=gm[:], scalar1=m8[:, 2:3], scalar2=None, op0=ALU.is_ge),
                       reads=[gm_b, m8_b, t1_b], writes=[t1_b])
                    op("dve", lambda e: e.tensor_tensor(out=t1[:], in0=t1[:], in1=past[:], op=ALU.mult), reads=[t1_b, past_b], writes=[t1_b])
                    op("dve", lambda e: e.tensor_tensor(out=sel[:, t, :], in0=t1[:], in1=own[:], op=ALU.add), reads=[t1_b, own_b], writes=[sel_b])
                for (g0, wg, tiles, nvis) in GROUPS:
                    rows = 128 if wg == 512 else wg
                    accs = [accr.next() for _ in tiles]
                    for (a, a_b) in accs:
                        op("pool", lambda e, a=a: e.memset(a[:], 0.0), writes=[a_b])
                    for n in range(nvis // 2):
                        pts = []
                        for r in range(2):
                            c = 2 * n + r
                            ps, ps_b = pringA.next()
                            op("pe", lambda e: e.matmul(ps[:, 0:wg], lhsT=kT_t[:, c * 128:(c + 1) * 128], rhs=qT[:, hd, g0:g0 + wg],
                                                        start=True, stop=True), reads=[kT_tb, qT_b], writes=[ps_b])
                            pt, pt_b = ptr.next()
                            op("act", lambda e: e.activation(out=pt[:, 0:wg], in_=ps[:, 0:wg], func=AF.Exp, scale=SCALE, bias=nct[:, 0:1]),
                               reads=[ps_b, nct_b], writes=[pt_b])
                            op("pool", lambda e: e.tensor_tensor(out=pt[:, 0:wg], in0=pt[:, 0:wg], in1=cm[:, c, g0:g0 + wg], op=ALU.mult),
                               reads=[pt_b, cm_b], writes=[pt_b])
                            pts.append((pt, pt_b))
                        for ti, t in enumerate(tiles):
                            ps, ps_b = pringB.next()
                            for r in range(2):
                                pt, pt_b = pts[r]
                                op("pe", lambda e: e.matmul(ps[:, 0:129], lhsT=pt[:, ti * 128:(ti + 1) * 128], rhs=V_t[:, 2 * n + r, :],
                                                            start=(r == 0), stop=(r == 1)), reads=[pt_b, V_tb], writes=[ps_b], sig=(r == 1))
                            a, a_b = accs[ti]
                            op("dve", lambda e: e.scalar_tensor_tensor(out=a[0:rows, :], in0=ps[0:rows, 0:129], scalar=sel[0:rows, t, n:n + 1],
                                                                       in1=a[0:rows, :], op0=ALU.mult, op1=ALU.add),
                               reads=[ps_b, sel_b, a_b], writes=[a_b])
                    for ti, t in enumerate(tiles):
                        a, a_b = accs[ti]
                        rv, rv_b = rvr.next()
                        op("dve", lambda e: e.tensor_scalar(out=rv[0:rows, 0:1], in0=a[0:rows, 128:129], scalar1=1e-30, scalar2=None, op0=ALU.add),
                           reads=[a_b], writes=[rv_b])
                        op("dve", lambda e: e.reciprocal(out=rv[0:rows, 1:2], in_=rv[0:rows, 0:1]), reads=[rv_b], writes=[rv_b])
                        ob, ob_b = obr.next()
                        op("dve", lambda e: e.tensor_scalar(out=ob[0:rows, :], in0=a[0:rows, 0:128], scalar1=rv[0:rows, 1:2], scalar2=None, op0=ALU.mult),
                           reads=[a_b, rv_b], writes=[ob_b])
                        emit_oT(ob, ob_b, rows, hd, t)
            SC.barrier()
        if stop_after == "moba":
            if dbg:
                SC.barrier()
                dma("sp", dbg_aps["d_oT"][:, :, :], oT_d[:, :, :].rearrange("s d t -> d s t"), reads=[oT_db], writes=[out_buf])
            SC.barrier()
            S4.close(); A.close()
            return _finish(nc, es, SC, out_buf)

        with ExitStack() as S4b:
            sg, sg_b = _sb(nc, S4b, "sg", [128, NQT, 24], F32)
            op("act", lambda e: e.activation(out=sg[:], in_=glog[:], func=AF.Sigmoid), reads=[glog_b], writes=[sg_b])
            reg = qT[:, 0:8, :].rearrange("p a b -> p (a b)")
            aind = reg[:, 4096:8192]
            aind_b = Buf("aind_v")
            dma("sp", aind, aind_in[:, :], writes=[aind_b])
            w1slots = [(reg[:, 0:2048].rearrange("p (l h) -> p l h", h=256), Buf("w1a")),
                       (reg[:, 2048:4096].rearrange("p (l h) -> p l h", h=256), Buf("w1b"))]
            w1ring = Ring(w1slots)
            mb, mb_b = _sb(nc, S4b, "mb", [128, NQT, 255], BF16)
            for t in range(NQT):
                op("dve", lambda e: e.tensor_scalar(out=mb[:, t, :], in0=cst[:, 256:511], scalar1=qpc[:, t:t + 1], scalar2=None, op0=ALU.is_le),
                   reads=[cst_b, qpc_b], writes=[mb_b])
                op("dve", lambda e: e.tensor_scalar(out=mb[:, t, :], in0=mb[:, t, :], scalar1=30000.0, scalar2=-30000.0, op0=ALU.mult, op1=ALU.add),
                   reads=[mb_b], writes=[mb_b])
            oacc = [_sb(nc, S4b, "oacc%d" % g, [128, NQT, 128], F32) for g in range(4)]
            selT, selT_b = _sb(nc, S4b, "selT", [128, NQ], BF16)
            op("pool", lambda e: e.memset(selT[:], 0.0), writes=[selT_b])
            hidT, hidT_b = _sb(nc, S4b, "hidT", [128, 2, 256], BF16)
            kcmpT, kcmpT_b = _sb(nc, S4b, "kcmpT", [128, 256], BF16)
            vcmp, vcmp_b = _sb(nc, S4b, "vcmp", [128, 2, 128], BF16)
            w2t, w2t_b = _sb(nc, S4b, "w2t", [128, 2, 128], BF16)
            posT, posT_b = _sb(nc, S4b, "posT", [128, 32], BF16)
            cbr = _ring(nc, S4b, "cb", [128, 1], F32, 2)
            zr = _ring(nc, S4b, "z", [128, 255], F32, 2)
            p32r = _ring(nc, S4b, "p32", [128, 255], F32, 2)
            pbr = _ring(nc, S4b, "pb", [128, 256], BF16, 2)
            pTr = _ring(nc, S4b, "pT", [128, 2, 128], BF16, 2)
            ips = [_sb(nc, S4b, "ipk%d" % t, [128, 260], F32) for t in range(NQT)]
            s64r = _ring(nc, S4b, "s64", [128, 64], F32, 10)
            sbr = _ring(nc, S4b, "sb16", [128, 64], BF16, 2)
            wtr = _ring(nc, S4b, "wt", [128, 512], BF16, 3)

            def compress(src_slot, w1_in, w2_in, pos_in, is_k):
                srcT, srcT_b = load_k(src_slot)
                dma("pool", w2t[:], w2_in.rearrange("(hh p) d -> p hh d", p=128), writes=[w2t_b])
                dma("pool", posT[:], pos_in[:, :], writes=[posT_b])
                for hh in range(2):
                    ps, ps_b = pringA.next()
                    psc, psc_b = pringA.next()
                    for l4 in range(4):
                        w1s, w1s_b = w1ring.next()
                        dma("pool", w1s, w1_in[l4 * 1024:(l4 + 1) * 1024, :].rearrange("(l d) h -> d l h", d=128), writes=[w1s_b])
                        for l8 in range(8):
                            l = l4 * 8 + l8
                            op("pe", lambda e, l=l, l8=l8: e.matmul(ps[:, 0:255], lhsT=w1s[:, l8, hh * 128:(hh + 1) * 128], rhs=srcT[:, l:l + 4065:16],
                                                                    start=(l == 0), stop=(l == 31)), reads=[w1s_b, srcT_b], writes=[ps_b], sig=(l == 31))
                            op("pe", lambda e, l=l, l8=l8: e.matmul(psc[:, 0:1], lhsT=w1s[:, l8, hh * 128:(hh + 1) * 128], rhs=posT[:, l:l + 1],
                                                                    start=(l == 0), stop=(l == 31)), reads=[w1s_b, posT_b], writes=[psc_b], sig=True)
                    cb, cb_b = cbr.next()
                    op("dve", lambda e: e.tensor_copy(out=cb[:], in_=psc[:, 0:1]), reads=[psc_b], writes=[cb_b])
                    op("act", lambda e: e.activation(out=hidT[:, hh, 0:255], in_=ps[:, 0:255], func=AF.Gelu_apprx_tanh, bias=cb[:, 0:1]),
                       reads=[ps_b, cb_b], writes=[hidT_b])
                if is_k:
                    ps, ps_b = pringA.next()
                    for hh in range(2):
                        op("pe", lambda e: e.matmul(ps[:, 0:255], lhsT=w2t[:, hh, :], rhs=hidT[:, hh, 0:255], start=(hh == 0), stop=(hh == 1)),
                           reads=[w2t_b, hidT_b], writes=[ps_b], sig=(hh == 1))
                    op("act", lambda e: e.copy(out=kcmpT[:, 0:255], in_=ps[:, 0:255]), reads=[ps_b], writes=[kcmpT_b])
                else:
                    for i2, rws in ((0, 128), (1, 127)):
                        ps, ps_b = pringA.next()
                        for hh in range(2):
                            op("pe", lambda e: e.matmul(ps[0:rws, 0:128], lhsT=hidT[:, hh, i2 * 128:i2 * 128 + rws], rhs=w2t[:, hh, :],
                                                        start=(hh == 0), stop=(hh == 1)), reads=[w2t_b, hidT_b], writes=[ps_b], sig=(hh == 1))
                        op("act", lambda e: e.copy(out=vcmp[0:rws, i2, :], in_=ps[0:rws, 0:128]), reads=[ps_b], writes=[vcmp_b])

            for kh in range(2):
                compress(8 + kh, cmp_w1_k, cmp_w2_k, cpos_k, True)
                compress(14 + kh, cmp_w1_v, cmp_w2_v, cpos_v, False)
                for t in range(NQT):
                    ip, ip_b = ips[t]
                    op("pool", lambda e, ip=ip: e.memset(ip[:], 0.0), writes=[ip_b])
                for g in range(4):
                    qh = 4 * kh + g
                    oa, oa_b = oacc[g]
                    for t in range(NQT):
                        ip, ip_b = ips[t]
                        ps, ps_b = pringA.next()
                        op("pe", lambda e: e.matmul(ps[:, 0:255], lhsT=qT[:, 8 + qh, t * 128:(t + 1) * 128], rhs=kcmpT[:, 0:255], start=True, stop=True),
                           reads=[qT_b, kcmpT_b], writes=[ps_b])
                        z, z_b = zr.next()
                        op("dve", lambda e: e.scalar_tensor_tensor(out=z[:], in0=ps[:, 0:255], scalar=SCALE, in1=mb[:, t, :], op0=ALU.mult, op1=ALU.add),
                           reads=[ps_b, mb_b], writes=[z_b])
                        rv, rv_b = rvr.next()
                        op("dve", lambda e: e.tensor_reduce(out=rv[:, 0:1], in_=z[:], axis=AX.X, op=ALU.max), reads=[z_b], writes=[rv_b])
                        op("dve", lambda e: e.tensor_scalar(out=rv[:, 1:2], in0=rv[:, 0:1], scalar1=-10000.0, scalar2=-1.0, op0=ALU.max, op1=ALU.mult),
                           reads=[rv_b], writes=[rv_b])
                        p32, p32_b = p32r.next()
                        op("act", lambda e: e.activation(out=p32[:], in_=z[:], func=AF.Exp, bias=rv[:, 1:2], accum_out=rv[:, 2:3]),
                           reads=[z_b, rv_b], writes=[p32_b, rv_b])
                        op("dve", lambda e: e.tensor_scalar(out=rv[:, 3:4], in0=rv[:, 2:3], scalar1=1e-30, scalar2=None, op0=ALU.add),
                           reads=[rv_b], writes=[rv_b])
                        op("dve", lambda e: e.reciprocal(out=rv[:, 0:1], in_=rv[:, 3:4]), reads=[rv_b], writes=[rv_b])
                        op("dve", lambda e: e.scalar_tensor_tensor(out=ip[:, 1:256], in0=p32[:], scalar=rv[:, 0:1], in1=ip[:, 1:256], op0=ALU.mult, op1=ALU.add),
                           reads=[p32_b, rv_b, ip_b], writes=[ip_b])
                        pb, pb_b = pbr.next()
                        op("pool", lambda e: e.tensor_copy(out=pb[:, 0:255], in_=p32[:]), reads=[p32_b], writes=[pb_b])
                        psT, psT_b = pringA.next()
                        psTb = psT[:, :].bitcast(BF16)
                        op("pe", lambda e: e.transpose(out=psTb[:, 0:128], in_=pb[:, 0:128], identity=idb[:]), reads=[pb_b, idb_b], writes=[psT_b], sig=False)
                        op("pe", lambda e: e.transpose(out=psTb[0:127, 128:256], in_=pb[:, 128:255], identity=idb[:]), reads=[pb_b, idb_b], writes=[psT_b])
                        pT, pT_b = pTr.next()
                        op("act", lambda e: e.copy(out=pT[:, 0, :], in_=psTb[:, 0:128]), reads=[psT_b], writes=[pT_b])
                        op("act", lambda e: e.copy(out=pT[0:127, 1, :], in_=psTb[0:127, 128:256]), reads=[psT_b], writes=[pT_b])
                        po, po_b = pringA.next()
                        op("pe", lambda e: e.matmul(po[:, 0:128], lhsT=pT[:, 0, :], rhs=vcmp[:, 0, :], start=True, stop=False),
                           reads=[pT_b, vcmp_b], writes=[po_b], sig=False)
                        op("pe", lambda e: e.matmul(po[:, 0:128], lhsT=pT[0:127, 1, :], rhs=vcmp[0:127, 1, :], start=False, stop=True),
                           reads=[pT_b, vcmp_b], writes=[po_b])
                        op("dve", lambda e: e.tensor_tensor(out=rv[:, 1:2], in0=rv[:, 0:1], in1=sg[:, t, qh:qh + 1], op=ALU.mult),
                           reads=[rv_b, sg_b], writes=[rv_b])
                        op("dve", lambda e: e.tensor_scalar(out=oa[:, t, :], in0=po[:, 0:128], scalar1=rv[:, 1:2], scalar2=None, op0=ALU.mult),
                           reads=[po_b, rv_b], writes=[oa_b])
                for t in range(NQT):
                    ip, ip_b = ips[t]
                    qc = qpc[:, t:t + 1]
                    a1, a1_b = s64r.next(); a2, a2_b = s64r.next(); vl, vl_b = s64r.next(); fc, fc_b = s64r.next(); sc_, sc_b = s64r.next()
                    op("dve", lambda e: e.tensor_tensor(out=a1[:], in0=ip[:, 0:253:4], in1=ip[:, 4:257:4], op=ALU.add), reads=[ip_b], writes=[a1_b])
                    op("dve", lambda e: e.tensor_tensor(out=a2[:], in0=ip[:, 1:254:4], in1=ip[:, 2:255:4], op=ALU.add), reads=[ip_b], writes=[a2_b])
                    op("dve", lambda e: e.tensor_tensor(out=a2[:], in0=a2[:], in1=ip[:, 3:256:4], op=ALU.add), reads=[ip_b, a2_b], writes=[a2_b])
                    op("dve", lambda e: e.scalar_tensor_tensor(out=a1[:], in0=a2[:], scalar=2.0, in1=a1[:], op0=ALU.mult, op1=ALU.add),
                       reads=[a1_b, a2_b], writes=[a1_b])
                    op("dve", lambda e: e.tensor_scalar(out=vl[:], in0=cst[:, 64:128], scalar1=qc, scalar2=None, op0=ALU.is_le),
                       reads=[cst_b, qpc_b], writes=[vl_b])
                    op("dve", lambda e: e.tensor_scalar(out=fc[:], in0=cst[:, 128:192], scalar1=qc, scalar2=None, op0=ALU.is_gt),
                       reads=[cst_b, qpc_b], writes=[fc_b])
                    op("dve", lambda e: e.tensor_tensor(out=fc[:], in0=fc[:], in1=vl[:], op=ALU.mult), reads=[fc_b, vl_b], writes=[fc_b])
                    op("dve", lambda e: e.tensor_tensor(out=fc[:], in0=fc[:], in1=cst[:, 192:256], op=ALU.max), reads=[fc_b, cst_b], writes=[fc_b])
                    op("dve", lambda e: e.tensor_tensor(out=a1[:], in0=a1[:], in1=vl[:], op=ALU.mult), reads=[a1_b, vl_b], writes=[a1_b])
                    op("dve", lambda e: e.scalar_tensor_tensor(out=a1[:], in0=fc[:], scalar=1e9, in1=a1[:], op0=ALU.mult, op1=ALU.add),
                       reads=[a1_b, fc_b], writes=[a1_b])
                    op("dve", lambda e: e.tensor_scalar(out=a2[:], in0=vl[:], scalar1=1e30, scalar2=-1e30, op0=ALU.mult, op1=ALU.add),
                       reads=[vl_b, a2_b], writes=[a2_b])
                    op("dve", lambda e: e.tensor_tensor(out=sc_[:], in0=a1[:], in1=a2[:], op=ALU.add), reads=[a1_b, a2_b], writes=[sc_b])
                    m8a, m8a_b = m8r.next(); m8b, m8b_b = m8r.next()
                    op("dve", lambda e: e.max(out=m8a[:], in_=sc_[:]), reads=[sc_b], writes=[m8a_b])
                    op("dve", lambda e: e.match_replace(out=a1[:], in_to_replace=m8a[:], in_values=sc_[:], imm_value=-3e38),
                       reads=[m8a_b, sc_b, a1_b], writes=[a1_b])
                    op("dve", lambda e: e.max(out=m8b[:], in_=a1[:]), reads=[a1_b], writes=[m8b_b])
                    op("dve", lambda e: e.tensor_scalar(out=a2[:], in0=sc_[:], scalar1=m8b[:, 7:8], scalar2=None, op0=ALU.is_ge),
                       reads=[sc_b, m8b_b, a2_b], writes=[a2_b])
                    op("dve", lambda e: e.tensor_tensor(out=a2[:], in0=a2[:], in1=vl[:], op=ALU.mult), reads=[a2_b, vl_b], writes=[a2_b])
                    sb16, sb16_b = sbr.next()
                    op("dve", lambda e: e.tensor_scalar(out=sb16[:], in0=a2[:], scalar1=30000.0, scalar2=-30000.0, op0=ALU.mult, op1=ALU.add),
                       reads=[a2_b], writes=[sb16_b])
                    ps, ps_b = pringA.next()
                    psb = ps[:, :].bitcast(BF16)
                    op("pe", lambda e: e.transpose(out=psb[0:64, 0:128], in_=sb16[:, :], identity=idb[:]), reads=[sb16_b, idb_b], writes=[ps_b])
                    op("act", lambda e: e.copy(out=selT[0:64, t * 128:(t + 1) * 128], in_=psb[0:64, 0:128]), reads=[ps_b], writes=[selT_b])
                for branch in (1, 2):
                    kslot = (10 + kh) if branch == 1 else (12 + kh)
                    vslot = (8 + kh) if branch == 1 else (10 + kh)
                    kT_t, kT_tb = load_k(kslot)
                    V_t, V_tb = load_v(vslot)
                    for g in range(4):
                        qh = 4 * kh + g
                        oa, oa_b = oacc[g]
                        nct, nct_b = shift_const(kT_t, kT_tb, 8 + qh)
                        for (g0, wg, tiles, nvis) in GROUPS:
                            rows = 128 if wg == 512 else wg
                            accs = [pringB.next() for _ in tiles]
                            for c in range(nvis):
                                ps, ps_b = pringA.next()
                                if branch == 1:
                                    op("pe", lambda e: e.matmul(ps[:, 0:wg], lhsT=kT_t[:, c * 128:(c + 1) * 128], rhs=qT[:, 8 + qh, g0:g0 + wg],
                                                                start=True, stop=False), reads=[kT_tb, qT_b], writes=[ps_b], sig=False)
                                    op("pe", lambda e: e.matmul(ps[:, 0:wg], lhsT=aind[:, c * 128:(c + 1) * 128], rhs=selT[:, g0:g0 + wg],
                                                                start=False, stop=True), reads=[aind_b, selT_b], writes=[ps_b])
                                    msk = cm[:, c, g0:g0 + wg]
                                    msk_bufs = [cm_b]
                                else:
                                    op("pe", lambda e: e.matmul(ps[:, 0:wg], lhsT=kT_t[:, c * 128:(c + 1) * 128], rhs=qT[:, 8 + qh, g0:g0 + wg],
                                                                start=True, stop=True), reads=[kT_tb, qT_b], writes=[ps_b])
                                    if c + 4 < NKT:
                                        wt, wt_b = wtr.next()
                                        op("dve", lambda e: e.tensor_tensor(out=wt[:, 0:wg], in0=cm[:, c, g0:g0 + wg], in1=cm[:, c + 4, g0:g0 + wg],
                                                                            op=ALU.subtract), reads=[cm_b], writes=[wt_b])
                                        msk = wt[:, 0:wg]
                                        msk_bufs = [wt_b]
                                    else:
                                        msk = cm[:, c, g0:g0 + wg]
                                        msk_bufs = [cm_b]
                                pt, pt_b = ptr.next()
                                op("act", lambda e: e.activation(out=pt[:, 0:wg], in_=ps[:, 0:wg], func=AF.Exp, scale=SCALE, bias=nct[:, 0:1]),
                                   reads=[ps_b, nct_b], writes=[pt_b])
                                op("pool", lambda e: e.tensor_tensor(out=pt[:, 0:wg], in0=pt[:, 0:wg], in1=msk, op=ALU.mult),
                                   reads=[pt_b] + msk_bufs, writes=[pt_b])
                                for ti, t in enumerate(tiles):
                                    ac, ac_b = accs[ti]
                                    op("pe", lambda e: e.matmul(ac[:, 0:129], lhsT=pt[:, ti * 128:(ti + 1) * 128], rhs=V_t[:, c, :],
                                                                start=(c == 0), stop=(c == nvis - 1)), reads=[pt_b, V_tb], writes=[ac_b],
                                       sig=(c == nvis - 1))
                            for ti, t in enumerate(tiles):
                                ac, ac_b = accs[ti]
                                rv, rv_b = rvr.next()
                                op("dve", lambda e: e.tensor_scalar(out=rv[0:rows, 0:1], in0=ac[0:rows, 128:129], scalar1=1e-30, scalar2=None, op0=ALU.add),
                                   reads=[ac_b], writes=[rv_b])
                                op("dve", lambda e: e.reciprocal(out=rv[0:rows, 1:2], in_=rv[0:rows, 0:1]), reads=[rv_b], writes=[rv_b])
                                op("dve", lambda e: e.tensor_tensor(out=rv[0:rows, 2:3], in0=rv[0:rows, 1:2], in1=sg[0:rows, t, branch * 8 + qh:branch * 8 + qh + 1],
                                                                    op=ALU.mult), reads=[rv_b, sg_b], writes=[rv_b])
                                op("dve", lambda e: e.scalar_tensor_tensor(out=oa[0:rows, t, :], in0=ac[0:rows, 0:128], scalar=rv[0:rows, 2:3],
                                                                           in1=oa[0:rows, t, :], op0=ALU.mult, op1=ALU.add),
                                   reads=[ac_b, rv_b, oa_b], writes=[oa_b])
                for g in range(4):
                    oa, oa_b = oacc[g]
                    for t in range(NQT):
                        rows = 128 if t < 8 else HW
                        ob, ob_b = obr.next()
                        op("pool", lambda e: e.tensor_copy(out=ob[0:rows, :], in_=oa[0:rows, t, :]), reads=[oa_b], writes=[ob_b])
                        emit_oT(ob, ob_b, rows, 8 + 4 * kh + g, t)
            SC.barrier()
        if dbg:
            SC.barrier()
            dma("sp", dbg_aps["d_oT"][:, :, :], oT_d[:, :, :].rearrange("s d t -> d s t"), reads=[oT_db], writes=[out_buf])
        SC.barrier()
    A.close()
    if stop_after == "attn":
        return _finish(nc, es, SC, out_buf)

    with ExitStack() as S6:
        oTs, oTs_b = _sb(nc, S6, "oTs", [128, 16, NQ], BF16)
        wo, wo_b = _sb(nc, S6, "wo", [128, KC, D], BF16)
        gw, gw_b = _sb(nc, S6, "gw", [128, D], F32)
        tmpw, tmpw_b = _sb(nc, S6, "tmpw", [128, D], F32)
        xr6 = _ring(nc, S6, "x6", [128, D], F32, 2)
        yr6 = _ring(nc, S6, "y6", [128, D], F32, 2)
        st6 = _ring(nc, S6, "st6", [128, 8], F32, 3)
        for h4 in range(4):
            dma("sp", oTs[:, h4 * 4:(h4 + 1) * 4, :], oT_d[h4 * 4:(h4 + 1) * 4, :, :].rearrange("s d t -> d s t"), reads=[oT_db], writes=[oTs_b])
        for h2 in range(8):
            dma("pool", wo[:, h2 * 2:(h2 + 1) * 2, :], w_out[h2 * 256:(h2 + 1) * 256, :].rearrange("(k p) c -> p k c", p=128), writes=[wo_b])
        bcast_mod(gw, gw_b, 2, n_post_mix[0:1, :], False, tmpw, tmpw_b)
        for t in range(NQT):
            x, x_b = xr6.next()
            dma("sp", x[:], xq[t * 128:(t + 1) * 128, :], writes=[x_b])
            y, y_b = yr6.next()
            s8, s8_b = st6.next()
            for g4 in range(4):
                ps, ps_b = pringA.next()
                for e_ in range(16):
                    op("pe", lambda e, e_=e_: e.matmul(ps[:, :], lhsT=oTs[:, e_, t * 128:(t + 1) * 128], rhs=wo[:, e_, g4 * 512:(g4 + 1) * 512],
                                                       start=(e_ == 0), stop=(e_ == 15)), reads=[oTs_b, wo_b], writes=[ps_b], sig=(e_ == 15))
                op("act", lambda e: e.activation(out=y[:, g4 * 512:(g4 + 1) * 512], in_=ps[:, :], func=AF.Square, accum_out=s8[:, g4:g4 + 1]),
                   reads=[ps_b], writes=[y_b, s8_b])
                op("dve", lambda e: e.tensor_copy(out=y[:, g4 * 512:(g4 + 1) * 512], in_=ps[:, :]), reads=[ps_b, y_b], writes=[y_b])
            op("dve", lambda e: e.tensor_reduce(out=s8[:, 4:5], in_=s8[:, 0:4], axis=AX.X, op=ALU.add), reads=[s8_b], writes=[s8_b])
            op("dve", lambda e: e.tensor_scalar(out=s8[:, 5:6], in0=s8[:, 4:5], scalar1=1.0 / D, scalar2=EPS, op0=ALU.mult, op1=ALU.add),
               reads=[s8_b], writes=[s8_b])
            op("act", lambda e: e.activation(out=s8[:, 6:7], in_=s8[:, 5:6], func=AF.Sqrt), reads=[s8_b], writes=[s8_b])
            op("dve", lambda e: e.reciprocal(out=s8[:, 7:8], in_=s8[:, 6:7]), reads=[s8_b], writes=[s8_b])
            op("dve", lambda e: e.scalar_tensor_tensor(out=y[:], in0=y[:], scalar=s8[:, 7:8], in1=gw[:], op0=ALU.mult, op1=ALU.mult),
               reads=[y_b, s8_b, gw_b], writes=[y_b])
            op("pool", lambda e: e.tensor_tensor(out=x[:], in0=x[:], in1=y[:], op=ALU.add), reads=[x_b, y_b], writes=[x_b])
            dma("sp", x1_d[t * 128:(t + 1) * 128, :], x[:], reads=[x_b], writes=[x1_db])
        SC.barrier()

    if stop_after == "p6":
        return _finish(nc, es, SC, out_buf)

    with ExitStack() as S7:
        h2T, h2T_b = _sb(nc, S7, "h2T", [128, KC, NQ], BF16)
        cwt, cwt_b = _sb(nc, S7, "cwt", [128, 128, 3], F32)
        cbt, cbt_b = _sb(nc, S7, "cbt", [128, 128], F32)
        uhs, uhs_b = _sb(nc, S7, "uhs", [128, 64, 2, 4], F32)
        qfl, qfl_b = _sb(nc, S7, "qfl", [128, 4], F32)
        dma("sp", qfl[:], qflag_row[:, 1024:1028], writes=[qfl_b])
        dma("sp", cwt[:], cw_l[:, :, :], writes=[cwt_b])
        dma("sp", cbt[:], cb_l[:, :], writes=[cbt_b])
        with ExitStack() as S7n:
            wmod, wmod_b = _sb(nc, S7n, "wmodf", [128, D], F32)
            shm, shm_b = _sb(nc, S7n, "shmf", [128, D], F32)
            xr = _ring(nc, S7n, "xrf", [128, D], F32, 2)
            tr = _ring(nc, S7n, "trf", [128, D], F32, 1)
            hbr = _ring(nc, S7n, "hbrf", [128, D], BF16, 2)
            st = _ring(nc, S7n, "stf", [128, 4], F32, 3)
            bcast_mod(wmod, wmod_b, 4, n_pre_ffn[0:1, :], True, tr.items[0][0], tr.items[0][1])
            bcast_mod(shm, shm_b, 3, None, False, None, None)
            for t in range(NQT):
                _norm_tile(x1_d[t * 128:(t + 1) * 128, :], [x1_db], wmod, wmod_b, shm, shm_b, h2T, h2T_b, t * 128, xr, tr, hbr, st)
            SC.barrier()
        if stop_after == "p7n":
            S7.close()
            return _finish(nc, es, SC, out_buf)
        gT, gT_b = _sb(nc, S7, "gT", [128, 64, 512], BF16)
        for gi, g0 in enumerate((0, 512)):
            with ExitStack() as S7u:
                wur = _ring(nc, S7u, "wu_%d" % gi, [128, KC, 256], BF16, 2)
                ugr = _ring(nc, S7u, "ug_%d" % gi, [128, 514], F32, 2)
                uvr = _ring(nc, S7u, "uv_%d" % gi, [128, 514], F32, 2)
                cgr = _ring(nc, S7u, "cg_%d" % gi, [128, 512], F32, 2)
                cvr = _ring(nc, S7u, "cv_%d" % gi, [128, 512], F32, 2)
                ggr = _ring(nc, S7u, "gg_%d" % gi, [128, 512], F32, 2)
                uhtr = _ring(nc, S7u, "uht_%d" % gi, [128, 256], F32, 2)
                for i in range(64):
                    wu, wu_b = wur.next()
                    for h4 in range(4):
                        dma("pool", wu[:, h4 * 4:(h4 + 1) * 4, :],
                            w_up_l[h4 * 512:(h4 + 1) * 512, i, :].rearrange("(k p) c -> p k c", p=128), writes=[wu_b])
                    if gi == 0:
                        ph, ph_b = pringB.next()
                        for k in range(KC):
                            op("pe", lambda e, k=k: e.matmul(ph[:, 0:256], lhsT=h2T[:, k, 1024:1152], rhs=wu[:, k, :],
                                                             start=(k == 0), stop=(k == KC - 1)), reads=[wu_b, h2T_b], writes=[ph_b], sig=(k == KC - 1))
                        uht, uht_b = uhtr.next()
                        op("act", lambda e: e.copy(out=uht[:], in_=ph[:, 0:256]), reads=[ph_b], writes=[uht_b])
                        pt2, pt2_b = pringB.next()
                        for half in range(2):
                            op("pe", lambda e, half=half: e.transpose(out=pt2[:, half * 128:(half + 1) * 128], in_=uht[:, half * 128:(half + 1) * 128],
                                                                      identity=idf[:]), reads=[uht_b, idf_b], writes=[pt2_b], sig=(half == 1))
                        for half in range(2):
                            op("dve", lambda e, half=half: e.tensor_tensor(out=uhs[:, i, half, :], in0=pt2[:, half * 128:half * 128 + 4],
                                                                           in1=qfl[:, 0:4], op=ALU.mult), reads=[pt2_b, qfl_b], writes=[uhs_b])
                    ug, ug_b = ugr.next()
                    uv, uv_b = uvr.next()
                    for half, (u, u_b) in enumerate(((ug, ug_b), (uv, uv_b))):
                        ps, ps_b = pringA.next()
                        for k in range(KC):
                            op("pe", lambda e, k=k: e.matmul(ps[:, :], lhsT=wu[:, k, half * 128:(half + 1) * 128], rhs=h2T[:, k, g0:g0 + 512],
                                                             start=(k == 0), stop=(k == KC - 1)), reads=[wu_b, h2T_b], writes=[ps_b], sig=(k == KC - 1))
                        if half == 0:
                            op("act", lambda e: e.copy(out=u[:, 2:514], in_=ps[:, :]), reads=[ps_b], writes=[u_b])
                        else:
                            op("act", lambda e: e.copy(out=u[:, 2:514], in_=ps[:, :]), reads=[ps_b], writes=[u_b])
                        op("pool", lambda e: e.tensor_copy(out=u[:, 0:2], in_=uhs[:, i, half, 2 * gi:2 * gi + 2]), reads=[uhs_b], writes=[u_b])
                    cg, cg_b = cgr.next()
                    cv, cv_b = cvr.next()
                    for half, (u, u_b, cdst, cdst_b) in enumerate(((ug, ug_b, cg, cg_b), (uv, uv_b, cv, cv_b))):
                        ch = half * 64 + i
                        op("dve", lambda e: e.tensor_scalar(out=cdst[:], in0=u[:, 2:514], scalar1=cwt[:, ch, 2:3], scalar2=cbt[:, ch:ch + 1],
                                                            op0=ALU.mult, op1=ALU.add), reads=[u_b, cwt_b, cbt_b], writes=[cdst_b])
                        op("dve", lambda e: e.scalar_tensor_tensor(out=cdst[:], in0=u[:, 1:513], scalar=cwt[:, ch, 1:2], in1=cdst[:],
                                                                    op0=ALU.mult, op1=ALU.add), reads=[u_b, cwt_b, cdst_b], writes=[cdst_b])
                        op("dve", lambda e: e.scalar_tensor_tensor(out=cdst[:], in0=u[:, 0:512], scalar=cwt[:, ch, 0:1], in1=cdst[:],
                                                                    op0=ALU.mult, op1=ALU.add), reads=[u_b, cwt_b, cdst_b], writes=[cdst_b])
                    gg, gg_b = ggr.next()
                    op("act", lambda e: e.activation(out=gg[:], in_=cg[:], func=AF.Gelu_apprx_tanh), reads=[cg_b], writes=[gg_b])
                    op("dve", lambda e: e.tensor_tensor(out=gT[:, i, :], in0=gg[:], in1=cv[:], op=ALU.mult), reads=[gg_b, cv_b], writes=[gT_b])
                SC.barrier()
            if stop_after == "p7u":
                S7.close()
                return _finish(nc, es, SC, out_buf)
            with ExitStack() as S7d:
                wdr = _ring(nc, S7d, "wd_%d" % gi, [128, 8, 512], BF16, 3)
                y2, y2_b = _sb(nc, S7d, "y2_%d" % gi, [128, 4, D], F32)
                gwf, gwf_b = _sb(nc, S7d, "gwf_%d" % gi, [128, D], F32)
                tmpf, tmpf_b = _sb(nc, S7d, "tmpf_%d" % gi, [128, D], F32)
                x1r = _ring(nc, S7d, "x1t_%d" % gi, [128, D], F32, 2)
                ssq, ssq_b = _sb(nc, S7d, "ssq_%d" % gi, [128, 32], F32)
                bcast_mod(gwf, gwf_b, 5, n_post_ffn[0:1, :], False, tmpf, tmpf_b)
                for d4 in range(4):
                    accs = [pringB.next() for _ in range(4)]
                    for s8i in range(8):
                        wd, wd_b = wdr.next()
                        for h2 in range(2):
                            dma("pool", wd[:, h2 * 4:(h2 + 1) * 4, :],
                                w_down[(s8i * 8 + h2 * 4) * 128:(s8i * 8 + h2 * 4 + 4) * 128, d4 * 512:(d4 + 1) * 512].rearrange("(c p) m -> p c m", p=128),
                                writes=[wd_b])
                        for c in range(8):
                            fc_ = s8i * 8 + c
                            for tt in range(4):
                                ac, ac_b = accs[tt]
                                op("pe", lambda e: e.matmul(ac[:, :], lhsT=gT[:, fc_, tt * 128:(tt + 1) * 128], rhs=wd[:, c, :],
                                                            start=(fc_ == 0), stop=(fc_ == 63)), reads=[gT_b, wd_b], writes=[ac_b], sig=(fc_ == 63 or tt == 3))
                    for tt in range(4):
                        ac, ac_b = accs[tt]
                        op("act", lambda e: e.activation(out=y2[:, tt, d4 * 512:(d4 + 1) * 512], in_=ac[:, :], func=AF.Square,
                                                         accum_out=ssq[:, tt * 8 + d4:tt * 8 + d4 + 1]),
                           reads=[ac_b], writes=[y2_b, ssq_b])
                        op("dve", lambda e: e.tensor_copy(out=y2[:, tt, d4 * 512:(d4 + 1) * 512], in_=ac[:, :]), reads=[ac_b, y2_b], writes=[y2_b])
                for tt in range(4):
                    b8 = tt * 8
                    op("dve", lambda e: e.tensor_reduce(out=ssq[:, b8 + 4:b8 + 5], in_=ssq[:, b8:b8 + 4], axis=AX.X, op=ALU.add), reads=[ssq_b], writes=[ssq_b])
                    op("dve", lambda e: e.tensor_scalar(out=ssq[:, b8 + 5:b8 + 6], in0=ssq[:, b8 + 4:b8 + 5], scalar1=1.0 / D, scalar2=EPS, op0=ALU.mult, op1=ALU.add),
                       reads=[ssq_b], writes=[ssq_b])
                    op("act", lambda e: e.activation(out=ssq[:, b8 + 6:b8 + 7], in_=ssq[:, b8 + 5:b8 + 6], func=AF.Sqrt), reads=[ssq_b], writes=[ssq_b])
                    op("dve", lambda e: e.reciprocal(out=ssq[:, b8 + 7:b8 + 8], in_=ssq[:, b8 + 6:b8 + 7]), reads=[ssq_b], writes=[ssq_b])
                    x1, x1_b = x1r.next()
                    row0 = g0 + tt * 128
                    dma("sp", x1[:], x1_d[row0:row0 + 128, :], reads=[x1_db], writes=[x1_b])
                    op("dve", lambda e: e.scalar_tensor_tensor(out=y2[:, tt, :], in0=y2[:, tt, :], scalar=ssq[:, tt * 8 + 7:tt * 8 + 8], in1=gwf[:],
                                                               op0=ALU.mult, op1=ALU.mult), reads=[y2_b, ssq_b, gwf_b], writes=[y2_b])
                    op("pool", lambda e: e.tensor_tensor(out=x1[:], in0=x1[:], in1=y2[:, tt, :], op=ALU.add), reads=[x1_b, y2_b], writes=[x1_b])
                    dma("sp", out[row0:row0 + 128, :], x1[:], reads=[x1_b], writes=[out_buf])
                SC.barrier()
            if stop_after == "p7d":
                S7.close()
                return _finish(nc, es, SC, out_buf)

    return _finish(nc, es, SC, out_buf)


def _finish(nc, es, SC, out_buf):
    SC.wait_all("sp", [out_buf])
    SC.barrier()
    es.close()
    return nc


def _qtok(j):
    ga, gb = j, 4 + j
    own = np.concatenate([np.arange(512 * ga, 512 * ga + 512), np.arange(512 * gb, 512 * gb + 512)])
    halo = np.full(128, 512 * gb - 1)
    flag = np.ones(NQ, np.float32)
    halo[2] = 512 * gb - 2
    halo[3] = 512 * gb - 1
    if ga > 0:
        halo[0] = 512 * ga - 2
        halo[1] = 512 * ga - 1
    else:
        halo[0] = 0
        halo[1] = 0
        flag[1024:1026] = 0.0
    return np.concatenate([own, halo]), flag


def make_in_maps(inp):
    x = np.asarray(inp["x"], np.float32)
    c = np.asarray(inp["c"], np.float32)
    pos = np.asarray(inp["positions"], np.int32)
    w_in = np.asarray(inp["w_in"], np.float32)[0]
    mq, mk, mv, nq = w_in[:, 0:1024], w_in[:, 1024:2048], w_in[:, 2048:3072], w_in[:, 3072:4096]
    kc, vc, ks, vs, kw, vw = [w_in[:, 4096 + 256 * i: 4096 + 256 * (i + 1)] for i in range(6)]
    ng = w_in[:, 5632:5656]
    w_k = np.ascontiguousarray(np.concatenate([mk, kc, ks, kw, vc, mv, vs, vw], axis=1))
    w_q = np.ascontiguousarray(np.concatenate([mq, nq, ng], axis=1))
    half = 64
    inv_freq = (10000.0 ** (-np.arange(half, dtype=np.float32) / half)).astype(np.float32)
    consts = np.zeros((128, 1024), np.float32)
    consts[:, 0:32] = 128.0 * np.arange(32)[None, :] + np.arange(128)[:, None]
    consts[:, 32:48] = 256.0 * (np.arange(16)[None, :] + 1)
    consts[:, 48:64] = 256.0 * np.arange(16)[None, :]
    consts[:, 64:128] = 64.0 * np.arange(64)[None, :]
    consts[:, 128:192] = 64.0 * (np.arange(64)[None, :] + 2)
    consts[:, 192] = 1.0
    consts[:, 256:511] = 16.0 * np.arange(255)[None, :] + 31.0
    aind = np.zeros((128, NKT * 128), np.float32)
    aind[np.arange(NKT * 128) // 64, np.arange(NKT * 128)] = 1.0
    w_up = np.asarray(inp["w_up"], np.float32)[0]
    w_up_l = np.ascontiguousarray(np.stack([w_up[:, :DFF].reshape(D, 64, 128), w_up[:, DFF:].reshape(D, 64, 128)], axis=2).reshape(D, 64, 256))
    conv_w = np.asarray(inp["conv_w"], np.float32)[0]
    conv_b = np.asarray(inp["conv_b"], np.float32)[0]
    cw_l = np.ascontiguousarray(conv_w.reshape(3, 128, 128).transpose(2, 1, 0))
    cb_l = np.ascontiguousarray(conv_b.reshape(128, 128).T)
    shared = {
        "w_ada": np.ascontiguousarray(inp["w_ada"][0], dtype=np.float32),
        "b_ada": np.ascontiguousarray(inp["b_ada"], dtype=np.float32).reshape(1, -1),
        "n_pre_mix": np.asarray(inp["norm_pre_mix"], np.float32).reshape(1, -1),
        "n_post_mix": np.asarray(inp["norm_post_mix"], np.float32).reshape(1, -1),
        "n_pre_ffn": np.asarray(inp["norm_pre_ffn"], np.float32).reshape(1, -1),
        "n_post_ffn": np.asarray(inp["norm_post_ffn"], np.float32).reshape(1, -1),
        "w_k": w_k, "w_q": w_q,
        "w_out": np.ascontiguousarray(inp["w_out"][0], dtype=np.float32),
        "ident_f": np.eye(128, dtype=np.float32),
        "ident_b": np.eye(128, dtype=np.float32).astype(ml_dtypes.bfloat16),
        "invf": np.ascontiguousarray(np.broadcast_to(inv_freq[None, :], (128, 64))),
        "consts": consts,
        "aind": aind.astype(ml_dtypes.bfloat16),
        "cmp_w1_k": np.ascontiguousarray(inp["cmp_w1_k"][0], dtype=np.float32),
        "cmp_w2_k": np.ascontiguousarray(inp["cmp_w2_k"][0], dtype=np.float32),
        "cmp_w1_v": np.ascontiguousarray(inp["cmp_w1_v"][0], dtype=np.float32),
        "cmp_w2_v": np.ascontiguousarray(inp["cmp_w2_v"][0], dtype=np.float32),
        "cpos_k": np.ascontiguousarray(np.asarray(inp["cmp_pos_k"], np.float32)[0].T),
        "cpos_v": np.ascontiguousarray(np.asarray(inp["cmp_pos_v"], np.float32)[0].T),
        "w_up_l": w_up_l,
        "w_down": np.ascontiguousarray(inp["w_down"][0], dtype=np.float32),
        "cw_l": cw_l, "cb_l": cb_l,
    }
    maps = []
    for core in range(8):
        b, j = core // 4, core % 4
        qt, flag = _qtok(j)
        m = dict(shared)
        m["xall"] = np.ascontiguousarray(x[b])
        m["xq"] = np.ascontiguousarray(x[b][qt])
        m["posall"] = np.ascontiguousarray(pos[b].reshape(NKT, 128).T)
        m["posq"] = np.ascontiguousarray(pos[b][qt].reshape(NQT, 128).T)
        qf = qt.astype(np.float32)
        m["qpos_col"] = np.ascontiguousarray(qf.reshape(NQT, 128).T)
        m["qpos_row"] = np.ascontiguousarray(np.broadcast_to(qf[None, :], (128, NQ)))
        m["qflag_row"] = np.ascontiguousarray(np.broadcast_to(flag[None, :], (128, NQ)))
        m["c_col"] = np.ascontiguousarray(c[b].reshape(KC, 128).T)
        maps.append(m)
    return maps


def kernel(**inputs):
    nc = build_program()
    maps = make_in_maps(inputs)
    res = run_bass_kernel_spmd(nc, maps, core_ids=list(range(8)))
    outp = np.zeros((2, S, D), np.float32)
    for core in range(8):
        b, j = core // 4, core % 4
        o = res.results[core]["out"]
        outp[b, 512 * j:512 * j + 512] = o[0:512]
        outp[b, 512 * (4 + j):512 * (4 + j) + 512] = o[512:1024]
    return outp
```

```python
import math
from contextlib import ExitStack

import numpy as np
import ml_dtypes

import concourse.bass as bass
import concourse.mybir as mybir
from concourse.bass_utils import run_bass_kernel_spmd

F32 = mybir.dt.float32
F32R = mybir.dt.float32r
BF16 = mybir.dt.bfloat16
I32 = mybir.dt.int32
AF = mybir.ActivationFunctionType
ALU = mybir.AluOpType
AX = mybir.AxisListType

D = 2048
S = 4096
NKT = S // 128
NQT = 9
NQ = NQT * 128
HW = 32
KC = D // 128
DFF = 8192
EPS = 1e-6
SCALE = 128 ** -0.5
NDMA = 40


class Buf:
    __slots__ = ("name", "w", "r")

    def __init__(self, name):
        self.name = name
        self.w = {}
        self.r = {}


class Eng:
    def __init__(self, name, h, semid):
        self.name = name
        self.h = h
        self.semid = semid
        self.cnt = 0
        self.seen = {}


class Sched:
    def __init__(self, nc, es):
        self.nc = nc
        self.sems = []
        self.engs = {}
        for name, h in (("pe", nc.tensor), ("act", nc.scalar), ("dve", nc.vector),
                        ("pool", nc.gpsimd), ("sp", nc.sync)):
            sem = es.enter_context(nc.semaphore("s_" + name))
            self.sems.append(sem)
            self.engs[name] = Eng(name, h, len(self.sems) - 1)
        self.dma_semids = []
        self.dma_vals = []
        for i in range(NDMA):
            sem = es.enter_context(nc.semaphore("d_%d" % i))
            self.sems.append(sem)
            self.dma_semids.append(len(self.sems) - 1)
            self.dma_vals.append(0)
        self.dma_i = 0
        self.n_ins = 0
        self.n_waits = 0
        self.snaps = {}
        self.pending = {}

    def _learn(self, eng, sid, v):
        if eng.seen.get(sid, 0) < v:
            eng.seen[sid] = v
        snap = self.snaps.get((sid, v))
        if snap:
            es = eng.seen
            for s2, v2 in snap.items():
                if es.get(s2, 0) < v2:
                    es[s2] = v2

    def _waits(self, eng, reads, writes):
        need = {}
        own = eng.semid
        for b in reads:
            for sid, v in b.w.items():
                if need.get(sid, 0) < v:
                    need[sid] = v
        for b in writes:
            for sid, v in b.w.items():
                if sid != own and need.get(sid, 0) < v:
                    need[sid] = v
            for sid, v in b.r.items():
                if sid != own and need.get(sid, 0) < v:
                    need[sid] = v
        for sid, v in sorted(need.items(), key=lambda kv: -kv[1]):
            if eng.seen.get(sid, 0) < v:
                eng.h.wait_ge(self.sems[sid], v)
                self.n_ins += 1
                self.n_waits += 1
                self._learn(eng, sid, v)

    def op(self, ename, fn, reads=(), writes=(), sig=True):
        eng = self.engs[ename]
        self._waits(eng, reads, writes)
        ins = fn(eng.h)
        self.n_ins += 1
        if sig:
            eng.cnt += 1
            ins.then_inc(self.sems[eng.semid], 1)
            v = eng.cnt
            self.snaps[(eng.semid, v)] = dict(eng.seen)
        else:
            v = eng.cnt + 1
            self.pending[eng.semid] = True
        sid = eng.semid
        for b in reads:
            if b.r.get(sid, 0) < v:
                b.r[sid] = v
        for b in writes:
            if b.w.get(sid, 0) < v:
                b.w[sid] = v
        return ins

    def dma(self, qname, out, in_, reads=(), writes=()):
        eng = self.engs[qname]
        slot = self.dma_i % NDMA
        self.dma_i += 1
        sid = self.dma_semids[slot]
        prev = self.dma_vals[slot]
        if prev > 0 and eng.seen.get(sid, 0) < prev:
            eng.h.wait_ge(self.sems[sid], prev)
            self.n_waits += 1
            self._learn(eng, sid, prev)
        self._waits(eng, reads, writes)
        ins = eng.h.dma_start(out=out, in_=in_)
        ins.then_inc(self.sems[sid], 16)
        self.n_ins += 1
        v = prev + 16
        self.dma_vals[slot] = v
        self.snaps[(sid, v)] = dict(eng.seen)
        for b in reads:
            b.r[sid] = v
        for b in writes:
            b.w[sid] = v
        return ins

    def barrier(self):
        toks = {}
        for e in self.engs.values():
            if e.cnt:
                toks[e.semid] = e.cnt
        for sid, v in zip(self.dma_semids, self.dma_vals):
            if v:
                toks[sid] = v
        sp = self.engs["sp"]
        for sid, v in sorted(toks.items(), key=lambda kv: -kv[1]):
            if sp.seen.get(sid, 0) < v:
                sp.h.wait_ge(self.sems[sid], v)
                self.n_waits += 1
                self._learn(sp, sid, v)
        slot = self.dma_i % NDMA
        self.dma("sp", self.bar_dst, self.bar_src)
        sid = self.dma_semids[slot]
        v = self.dma_vals[slot]
        for e in self.engs.values():
            if e.seen.get(sid, 0) < v:
                e.h.wait_ge(self.sems[sid], v)
                self.n_waits += 1
                self._learn(e, sid, v)

    def wait_all(self, ename, bufs):
        eng = self.engs[ename]
        self._waits(eng, bufs, ())


class Ring:
    def __init__(self, items):
        self.items = items
        self.i = 0

    def next(self):
        it = self.items[self.i % len(self.items)]
        self.i += 1
        return it


def _sb(nc, es, name, shape, dt):
    t = es.enter_context(nc.sbuf_tensor(name, list(shape), dt))
    return t, Buf(name)


def _ring(nc, es, name, shape, dt, n):
    return Ring([_sb(nc, es, "%s%d" % (name, i), shape, dt) for i in range(n)])


def build_program(dbg=False, stop_after=None):
    nc = bass.Bass("TRN2", target_bir_lowering=False)
    es = ExitStack()

    def din(name, shape, dt=F32):
        return nc.dram_tensor(name, list(shape), dt, kind="ExternalInput").ap()

    def dscr(name, shape, dt):
        return nc.dram_tensor(name, list(shape), dt).ap(), Buf(name)

    xall = din("xall", [S, D])
    xq = din("xq", [NQ, D])
    posall = din("posall", [128, NKT], I32)
    posq = din("posq", [128, NQT], I32)
    qpos_col = din("qpos_col", [128, NQT])
    qpos_row = din("qpos_row", [128, NQ])
    qflag_row = din("qflag_row", [128, NQ])
    c_col = din("c_col", [128, KC])
    w_ada = din("w_ada", [24, 128, KC, 512])
    b_ada = din("b_ada", [1, 6 * D])
    n_pre_mix = din("n_pre_mix", [1, D])
    n_post_mix = din("n_post_mix", [1, D])
    n_pre_ffn = din("n_pre_ffn", [1, D])
    n_post_ffn = din("n_post_ffn", [1, D])
    w_k = din("w_k", [7, 128, KC, 512])
    w_q = din("w_q", [4, 128, KC, 512])
    w_g = din("w_g", [128, KC, 24])
    w_out = din("w_out", [128, KC, D])
    ident_f = din("ident_f", [128, 128])
    ident_b = din("ident_b", [128, 128], BF16)
    invf = din("invf", [128, 64])
    consts = din("consts", [128, 1024])
    aind_in = din("aind", [128, NKT * 128], BF16)
    cmp_w1_k = din("cmp_w1_k", [4096, 256])
    cmp_w2_k = din("cmp_w2_k", [256, 128])
    cmp_w1_v = din("cmp_w1_v", [4096, 256])
    cmp_w2_v = din("cmp_w2_v", [256, 128])
    cpos_k = din("cpos_k", [128, 32])
    cpos_v = din("cpos_v", [128, 32])
    w_up_l = din("w_up_l", [64, 128, KC, 256])
    w_down = din("w_down", [4, 8, 128, 8, 512])
    cw_l = din("cw_l", [128, 128, 3])
    cb_l = din("cb_l", [128, 128])
    out = nc.dram_tensor("out", [1024, D], F32, kind="ExternalOutput").ap()
    out_buf = Buf("out")
    dbg_aps = {}
    if dbg:
        for nm, shp, dt in (("d_mod", [1, 6 * D], F32), ("d_hT", [128, KC, 256], BF16),
                            ("d_kT", [16, 128, S], BF16), ("d_V", [S, 1536], BF16),
                            ("d_qT", [128, 16, NQ], BF16), ("d_gl", [128, NQT, 24], F32),
                            ("d_oT", [128, 16, NQ], BF16)):
            dbg_aps[nm] = nc.dram_tensor(nm, shp, dt, kind="ExternalOutput").ap()

    kT_d, kT_db = dscr("kT_d", [16, 128, S], BF16)
    V_d, V_db = dscr("V_d", [S, 1536], BF16)

    SC = Sched(nc, es)
    op, dma = SC.op, SC.dma
    SC.bar_dst = nc.dram_tensor("bar_d", [1, 16], F32).ap()[0:1, :]
    SC.bar_src = b_ada[0:1, 0:16]

    banks = []
    for i in range(8):
        t = es.enter_context(nc.psum_tensor("ps%d" % i, [128, 512], F32))
        banks.append((t, Buf("ps%d" % i)))
    pring = Ring(banks)

    P = ExitStack()
    es.enter_context(P)
    mod_d, mod_db = dscr("mod_d", [1, 6 * D], F32)
    idf, idf_b = _sb(nc, P, "idf", [128, 128], F32)
    idb, idb_b = _sb(nc, P, "idb", [128, 128], BF16)
    ones_f, ones_fb = _sb(nc, P, "ones_f", [128, 128], F32)
    cst, cst_b = _sb(nc, P, "cst", [128, 1024], F32)
    dma("sp", idf[:], ident_f[:, :], writes=[idf_b])
    dma("sp", idb[:], ident_b[:, :], writes=[idb_b])
    dma("sp", cst[:], consts[:, :], writes=[cst_b])
    op("pool", lambda e: e.memset(ones_f[:], 1.0), writes=[ones_fb])

    with ExitStack() as s0:
        cc, cc_b = _sb(nc, s0, "cc", [128, KC], F32)
        sT, sT_b = _sb(nc, s0, "sT", [128, KC], BF16)
        brow, brow_b = _sb(nc, s0, "brow", [1, 6 * D], F32)
        mrow = _ring(nc, s0, "mrow", [1, 512], F32, 2)
        war = _ring(nc, s0, "wa", [128, KC, 512], BF16, 3)
        dma("sp", cc[:], c_col[:, :], writes=[cc_b])
        dma("sp", brow[:], b_ada[:, :], writes=[brow_b])
        op("act", lambda e: e.activation(out=sT[:], in_=cc[:], func=AF.Silu), reads=[cc_b], writes=[sT_b])
        for g in range(24):
            wa, wa_b = war.next()
            for h2 in range(2):
                dma("pool", wa[:, h2 * 8:(h2 + 1) * 8, :], w_ada[g, :, h2 * 8:(h2 + 1) * 8, :], writes=[wa_b])
            ps, ps_b = pring.next()
            for k in range(KC):
                op("pe", lambda e, k=k: e.matmul(ps[0:1, :], lhsT=sT[:, k:k + 1],
                                                 rhs=wa[:, k, :],
                                                 start=(k == 0), stop=(k == KC - 1)),
                   reads=[sT_b, wa_b], writes=[ps_b], sig=(k == KC - 1))
            mr, mr_b = mrow.next()
            op("dve", lambda e: e.tensor_tensor(out=mr[0:1, :], in0=ps[0:1, :],
                                                in1=brow[0:1, g * 512:(g + 1) * 512], op=ALU.add),
               reads=[ps_b, brow_b], writes=[mr_b])
            dma("sp", mod_d[0:1, g * 512:(g + 1) * 512], mr[0:1, :], reads=[mr_b], writes=[mod_db])
        if dbg:
            SC.barrier()
            dma("sp", dbg_aps["d_mod"][:, :], mod_d[:, :], reads=[mod_db], writes=[out_buf])
        SC.barrier()

    def bcast_mod(dst, dst_b, idx, wrow_ap, plus_one, tmp, tmp_b):
        dma("sp", dst[:], mod_d[0:1, idx * D:(idx + 1) * D].to_broadcast([128, D]), reads=[mod_db], writes=[dst_b])
        if wrow_ap is not None:
            dma("act", tmp[:], wrow_ap.to_broadcast([128, D]), writes=[tmp_b])
            op("dve", lambda e: e.scalar_tensor_tensor(out=dst[:], in0=dst[:], scalar=(1.0 if plus_one else 0.0),
                                                       in1=tmp[:], op0=ALU.add, op1=ALU.mult),
               reads=[dst_b, tmp_b], writes=[dst_b])

    def _norm_tile(src_ap, src_bufs, wm, wm_b, sh, sh_b, dstT, dstT_b, col0, xr, tr, hbr, st):
        x, x_b = xr.next()
        dma("sp", x[:, 0:1024], src_ap[:, 0:1024], reads=src_bufs, writes=[x_b])
        dma("act", x[:, 1024:2048], src_ap[:, 1024:2048], reads=src_bufs, writes=[x_b])
        s4, s4_b = st.next()
        hb, hb_b = hbr.next()
        op("act", lambda e: e.activation(out=hb[:], in_=x[:], func=AF.Square, accum_out=s4[:, 0:1]),
           reads=[x_b], writes=[hb_b, s4_b])
        op("dve", lambda e: e.tensor_scalar(out=s4[:, 1:2], in0=s4[:, 0:1], scalar1=1.0 / D, scalar2=EPS,
                                            op0=ALU.mult, op1=ALU.add), reads=[s4_b], writes=[s4_b])
        op("act", lambda e: e.activation(out=s4[:, 2:3], in_=s4[:, 1:2], func=AF.Sqrt), reads=[s4_b], writes=[s4_b])
        op("dve", lambda e: e.reciprocal(out=s4[:, 3:4], in_=s4[:, 2:3]), reads=[s4_b], writes=[s4_b])
        t, t_b = tr.next()
        op("dve", lambda e: e.scalar_tensor_tensor(out=t[:], in0=x[:], scalar=s4[:, 3:4], in1=wm[:],
                                                   op0=ALU.mult, op1=ALU.mult),
           reads=[x_b, s4_b, wm_b], writes=[t_b])
        op("pool", lambda e: e.tensor_tensor(out=hb[:], in0=t[:], in1=sh[:], op=ALU.add),
           reads=[t_b, sh_b], writes=[hb_b])
        for c4 in range(4):
            ps, ps_b = pring.next()
            psb = ps[:, :].bitcast(BF16)
            for i in range(4):
                k = c4 * 4 + i
                op("pe", lambda e, k=k, i=i: e.transpose(out=psb[:, i * 128:(i + 1) * 128], in_=hb[:, k * 128:(k + 1) * 128],
                                                         identity=idb[:]),
                   reads=[hb_b, idb_b], writes=[ps_b], sig=(i == 3))
            dst = dstT[:, c4 * 4:(c4 + 1) * 4, col0:col0 + 128]
            srcv = psb[:, 0:512].rearrange("p (a b) -> p a b", a=4)
            if c4 % 2 == 0:
                op("act", lambda e: e.copy(out=dst, in_=srcv), reads=[ps_b], writes=[dstT_b])
            else:
                op("dve", lambda e: e.tensor_copy(out=dst, in_=srcv), reads=[ps_b], writes=[dstT_b])

    A = ExitStack()
    qT, qT_b = _sb(nc, A, "qT", [128, 16, NQ], BF16)
    glog, glog_b = _sb(nc, A, "glog", [128, NQT, 24], F32)

    with ExitStack() as S1:
        hT, hT_b = _sb(nc, S1, "hT", [128, KC, NQ], BF16)
        wmod, wmod_b = _sb(nc, S1, "wmod", [128, D], F32)
        shm, shm_b = _sb(nc, S1, "shm", [128, D], F32)
        xr = _ring(nc, S1, "xr", [128, D], F32, 2)
        tr = _ring(nc, S1, "tr", [128, D], F32, 1)
        hbr = _ring(nc, S1, "hbr", [128, D], BF16, 2)
        st = _ring(nc, S1, "st", [128, 4], F32, 3)
        bcast_mod(wmod, wmod_b, 1, n_pre_mix[0:1, :], True, tr.items[0][0], tr.items[0][1])
        bcast_mod(shm, shm_b, 0, None, False, None, None)

        def norm_tile(src_ap, wm, wm_b, sh, sh_b, dstT, dstT_b, col0):
            _norm_tile(src_ap, [], wm, wm_b, sh, sh_b, dstT, dstT_b, col0, xr, tr, hbr, st)

        def _unused():
            x, x_b = xr.next()
            s4, s4_b = st.next()
            hb, hb_b = hbr.next()
            op("act", lambda e: e.activation(out=hb[:], in_=x[:], func=AF.Square, accum_out=s4[:, 0:1]),
               reads=[x_b], writes=[hb_b, s4_b])
            op("dve", lambda e: e.tensor_scalar(out=s4[:, 1:2], in0=s4[:, 0:1], scalar1=1.0 / D, scalar2=EPS,
                                                op0=ALU.mult, op1=ALU.add), reads=[s4_b], writes=[s4_b])
            op("act", lambda e: e.activation(out=s4[:, 2:3], in_=s4[:, 1:2], func=AF.Sqrt), reads=[s4_b], writes=[s4_b])
            op("dve", lambda e: e.reciprocal(out=s4[:, 3:4], in_=s4[:, 2:3]), reads=[s4_b], writes=[s4_b])
            t, t_b = tr.next()
            op("dve", lambda e: e.scalar_tensor_tensor(out=t[:], in0=x[:], scalar=s4[:, 3:4], in1=wm[:],
                                                       op0=ALU.mult, op1=ALU.mult),
               reads=[x_b, s4_b, wm_b], writes=[t_b])
            op("pool", lambda e: e.tensor_tensor(out=hb[:], in0=t[:], in1=sh[:], op=ALU.add),
               reads=[t_b, sh_b], writes=[hb_b])
            for c4 in range(4):
                ps, ps_b = pring.next()
                psb = ps[:, :].bitcast(BF16)
                for i in range(4):
                    k = c4 * 4 + i
                    op("pe", lambda e, k=k, i=i: e.transpose(out=psb[:, i * 128:(i + 1) * 128], in_=hb[:, k * 128:(k + 1) * 128],
                                                             identity=idb[:]),
                       reads=[hb_b, idb_b], writes=[ps_b], sig=(i == 3))
                dst = dstT[:, c4 * 4:(c4 + 1) * 4, col0:col0 + 128]
                srcv = psb[:, 0:512].rearrange("p (a b) -> p a b", a=4)
                if c4 % 2 == 0:
                    op("act", lambda e: e.copy(out=dst, in_=srcv), reads=[ps_b], writes=[dstT_b])
                else:
                    op("dve", lambda e: e.tensor_copy(out=dst, in_=srcv), reads=[ps_b], writes=[dstT_b])

        cosK, cosK_b = _sb(nc, S1, "cosK", [128, NQT, 64], F32)
        sinK, sinK_b = _sb(nc, S1, "sinK", [128, NQT, 64], F32)
        pi_, pi_b = _sb(nc, S1, "posi", [128, NKT + NQT], I32)
        pf, pf_b = _sb(nc, S1, "posf", [128, NKT + NQT], F32)
        ivf, ivf_b = _sb(nc, S1, "ivf", [128, 64], F32)
        angr = _ring(nc, S1, "ang", [128, 64], F32, 6)
        dma("sp", pi_[:, 0:NKT], posall[:, :], writes=[pi_b])
        dma("sp", pi_[:, NKT:NKT + NQT], posq[:, :], writes=[pi_b])
        dma("sp", ivf[:], invf[:, :], writes=[ivf_b])
        op("dve", lambda e: e.tensor_copy(out=pf[:], in_=pi_[:]), reads=[pi_b], writes=[pf_b])
        negpi, negpi_b = _sb(nc, S1, "negpi", [128, 1], F32)
        op("pool", lambda e: e.memset(negpi[:], -math.pi), writes=[negpi_b])

        def rope_tables(tt0, n):
            MAGIC = 12582912.0
            for i in range(n):
                t = tt0 + i
                a, a_b = angr.next()
                op("dve", lambda e: e.tensor_scalar(out=a[:], in0=ivf[:], scalar1=pf[:, t:t + 1], scalar2=None, op0=ALU.mult),
                   reads=[ivf_b, pf_b], writes=[a_b])
                for (shift, dst, dst_b) in ((0.5 * math.pi, cosK, cosK_b), (0.0, sinK, sinK_b)):
                    a1, a1_b = angr.next()
                    a2, a2_b = angr.next()
                    op("dve", lambda e: e.tensor_scalar(out=a1[:], in0=a[:], scalar1=shift, scalar2=None, op0=ALU.add),
                       reads=[a_b], writes=[a1_b])
                    op("dve", lambda e: e.tensor_scalar(out=a2[:], in0=a1[:], scalar1=1.0 / (2 * math.pi), scalar2=MAGIC,
                                                        op0=ALU.mult, op1=ALU.add), reads=[a1_b], writes=[a2_b])
                    op("dve", lambda e: e.tensor_scalar(out=a2[:], in0=a2[:], scalar1=MAGIC, scalar2=None, op0=ALU.subtract),
                       reads=[a2_b], writes=[a2_b])
                    op("dve", lambda e: e.scalar_tensor_tensor(out=a1[:], in0=a2[:], scalar=-2 * math.pi, in1=a1[:],
                                                               op0=ALU.mult, op1=ALU.add), reads=[a2_b, a1_b], writes=[a1_b])
                    op("dve", lambda e: e.tensor_scalar(out=a1[:], in0=a1[:], scalar1=3.14159, scalar2=-3.14159,
                                                        op0=ALU.min, op1=ALU.max), reads=[a1_b], writes=[a1_b])
                    op("act", lambda e: e.activation(out=dst[:, i, :], in_=a1[:], func=AF.Sin),
                       reads=[a1_b], writes=[dst_b])

        wr = _ring(nc, S1, "wr", [128, KC, 512], BF16, 2)
        ta = _ring(nc, S1, "ta", [128, 4, 64], F32, 2)
        tb = _ring(nc, S1, "tb", [128, 4, 64], F32, 2)
        tc_ = _ring(nc, S1, "tc", [128, 4, 64], F32, 2)
        td = _ring(nc, S1, "td", [128, 4, 64], F32, 2)
        krr = _ring(nc, S1, "kr", [128, 4, 128], BF16, 2)
        kst = _ring(nc, S1, "kst", [128, 4, 512], BF16, 2)
        vst = _ring(nc, S1, "vst", [128, 512], BF16, 3)

        def load_w(src3, ncols=512):
            w, w_b = wr.next()
            for h2 in range(2):
                dma("pool", w[:, h2 * 8:(h2 + 1) * 8, 0:ncols], src3[:, h2 * 8:(h2 + 1) * 8, :], writes=[w_b])
            return w, w_b

        def proj_tile(col0, w, w_b, ncols=512):
            ps, ps_b = pring.next()
            for k in range(KC):
                op("pe", lambda e, k=k: e.matmul(ps[:, 0:ncols], lhsT=hT[:, k, col0:col0 + 128], rhs=w[:, k, 0:ncols],
                                                 start=(k == 0), stop=(k == KC - 1)),
                   reads=[hT_b, w_b], writes=[ps_b], sig=(k == KC - 1))
            return ps, ps_b

        def rope4(ps, ps_b, ti, nrope):
            kr, kr_b = krr.next()
            pv = ps[:, :].rearrange("p (h t d) -> p h t d", h=4, t=2)
            krv = kr[:, :, :].rearrange("p h (t d) -> p h t d", t=2)
            if nrope > 0:
                n = nrope
                cs = cosK[:, ti:ti + 1, :].to_broadcast([128, n, 64])
                sn = sinK[:, ti:ti + 1, :].to_broadcast([128, n, 64])
                a, a_b = ta.next(); b, b_b = tb.next(); c, c_b = tc_.next(); d, d_b = td.next()
                op("dve", lambda e: e.tensor_tensor(out=a[:, 0:n, :], in0=pv[:, 0:n, 0, :], in1=cs, op=ALU.mult),
                   reads=[ps_b, cosK_b], writes=[a_b])
                op("dve", lambda e: e.tensor_tensor(out=b[:, 0:n, :], in0=pv[:, 0:n, 1, :], in1=sn, op=ALU.mult),
                   reads=[ps_b, sinK_b], writes=[b_b])
                op("dve", lambda e: e.tensor_tensor(out=c[:, 0:n, :], in0=pv[:, 0:n, 1, :], in1=cs, op=ALU.mult),
                   reads=[ps_b, cosK_b], writes=[c_b])
                op("dve", lambda e: e.tensor_tensor(out=d[:, 0:n, :], in0=pv[:, 0:n, 0, :], in1=sn, op=ALU.mult),
                   reads=[ps_b, sinK_b], writes=[d_b])
                op("pool", lambda e: e.tensor_tensor(out=krv[:, 0:n, 0, :], in0=a[:, 0:n, :], in1=b[:, 0:n, :], op=ALU.subtract),
                   reads=[a_b, b_b], writes=[kr_b])
                op("pool", lambda e: e.tensor_tensor(out=krv[:, 0:n, 1, :], in0=c[:, 0:n, :], in1=d[:, 0:n, :], op=ALU.add),
                   reads=[c_b, d_b], writes=[kr_b])
            if nrope < 4:
                op("act", lambda e: e.copy(out=kr[:, nrope:4, :], in_=ps[:, nrope * 128:512].rearrange("p (h d) -> p h d", h=4 - nrope)),
                   reads=[ps_b], writes=[kr_b])
            return kr, kr_b

        def transpose4(kr, kr_b, dst, dst_b):
            ps, ps_b = pring.next()
            psb = ps[:, :].bitcast(BF16)
            for i in range(4):
                op("pe", lambda e, i=i: e.transpose(out=psb[:, i * 128:(i + 1) * 128], in_=kr[:, i, :], identity=idb[:]),
                   reads=[kr_b, idb_b], writes=[ps_b], sig=(i == 3))
            op("act", lambda e: e.copy(out=dst, in_=psb[:, 0:512].rearrange("p (a b) -> p a b", a=4)),
               reads=[ps_b], writes=[dst_b])

        for qtr in range(4):
            for t in range(8):
                tg = qtr * 8 + t
                norm_tile(xall[tg * 128:(tg + 1) * 128, :], wmod, wmod_b, shm, shm_b, hT, hT_b, t * 128)
            rope_tables(qtr * 8, 8)
            if dbg and qtr == 0:
                dma("sp", dbg_aps["d_hT"][:, :, 0:128], hT[:, :, 128:256], reads=[hT_b], writes=[out_buf])
            if stop_after == "norm":
                break
            for cg in range(7):
                w, w_b = load_w(w_k[cg])
                for t in range(8):
                    tg = qtr * 8 + t
                    ps, ps_b = proj_tile(t * 128, w, w_b)
                    if cg < 4:
                        nrope = 4 if cg < 3 else 2
                        kr, kr_b = rope4(ps, ps_b, t, nrope)
                        if t % 4 == 0:
                            ks, ks_b = kst.next()
                        transpose4(kr, kr_b, ks[:, :, (t % 4) * 128:(t % 4 + 1) * 128], ks_b)
                        if t % 4 == 3:
                            t0 = (tg // 4) * 512
                            dma("sp", kT_d[cg * 4:(cg + 1) * 4, :, t0:t0 + 512].rearrange("s d t -> d s t"), ks[:, :, :],
                                reads=[ks_b], writes=[kT_db])
                    else:
                        v, v_b = vst.next()
                        op("act", lambda e: e.copy(out=v[:], in_=ps[:, :]), reads=[ps_b], writes=[v_b])
                        dma("sp", V_d[tg * 128:(tg + 1) * 128, (cg - 4) * 512:(cg - 3) * 512], v[:], reads=[v_b], writes=[V_db])
        for t in range(NQT):
            norm_tile(xq[t * 128:(t + 1) * 128, :], wmod, wmod_b, shm, shm_b, hT, hT_b, t * 128)
        rope_tables(NKT, NQT)
        if dbg:
            dma("sp", dbg_aps["d_hT"][:, :, 128:256], hT[:, :, 0:128], reads=[hT_b], writes=[out_buf])
        if stop_after != "norm":
            for cg in range(4):
                w, w_b = load_w(w_q[cg])
                for t in range(NQT):
                    ps, ps_b = proj_tile(t * 128, w, w_b)
                    kr, kr_b = rope4(ps, ps_b, t, 4)
                    transpose4(kr, kr_b, qT[:, cg * 4:(cg + 1) * 4, t * 128:(t + 1) * 128], qT_b)
            wg, wg_b = load_w(w_g, ncols=24)
            for t in range(NQT):
                ps, ps_b = proj_tile(t * 128, wg, wg_b, ncols=24)
                op("act", lambda e: e.copy(out=glog[:, t, :], in_=ps[:, 0:24]), reads=[ps_b], writes=[glog_b])
            if dbg:
                SC.barrier()
                dma("sp", dbg_aps["d_kT"][:, :, :], kT_d[:, :, :], reads=[kT_db], writes=[out_buf])
                dma("sp", dbg_aps["d_V"][:, :], V_d[:, :], reads=[V_db], writes=[out_buf])
                dma("sp", dbg_aps["d_qT"][:, :, :], qT[:], reads=[qT_b], writes=[out_buf])
                dma("sp", dbg_aps["d_gl"][:, :, :], glog[:], reads=[glog_b], writes=[out_buf])
        SC.barrier()
    if stop_after in ("norm", "proj"):
        return _finish(nc, es, SC, out_buf)

    pringA = Ring(banks[0:4])
    pringB = Ring(banks[4:8])
    oT_d, oT_db = dscr("oT_d", [16, 128, NQ], BF16)
    x1_d, x1_db = dscr("x1_d", [NQ, D], F32)
    GROUPS = ((0, 512, (0, 1, 2, 3), 16), (512, 512, (4, 5, 6, 7), NKT), (1024, HW, (8,), 28))
    with ExitStack() as S4:
        qpc, qpc_b = _sb(nc, S4, "qpc", [128, NQT], F32)
        qpr, qpr_b = _sb(nc, S4, "qpr", [128, NQ], F32)
        cm, cm_b = _sb(nc, S4, "cm", [128, NKT, 1056], BF16)
        ones_b, ones_bb = _sb(nc, S4, "ones_b", [128, 128], BF16)
        dma("sp", qpc[:], qpos_col[:, :], writes=[qpc_b])
        dma("sp", qpr[:], qpos_row[:, :], writes=[qpr_b])
        op("pool", lambda e: e.memset(ones_b[:], 1.0), writes=[ones_bb])
        for c in range(NKT):
            op("dve", lambda e, c=c: e.tensor_scalar(out=cm[:, c, :], in0=qpr[:, 0:1056], scalar1=cst[:, c:c + 1], scalar2=None,
                                                     op0=ALU.is_ge), reads=[qpr_b, cst_b], writes=[cm_b])
        kTr = _ring(nc, S4, "kTt", [128, S], BF16, 2)
        Vr = _ring(nc, S4, "Vt", [128, NKT, 129], BF16, 2)
        for (v, v_b) in Vr.items:
            op("pool", lambda e, v=v: e.memset(v[:, :, 128:129], 1.0), writes=[v_b])
        ptr = _ring(nc, S4, "pt", [128, 512], BF16, 5)
        m8r = _ring(nc, S4, "m8", [128, 8], F32, 6)
        ncr = _ring(nc, S4, "ncr", [128, 4], F32, 2)
        obr = _ring(nc, S4, "ob", [128, 128], BF16, 3)
        ostr = _ring(nc, S4, "ost", [128, 128], BF16, 4)
        rvr = _ring(nc, S4, "rv", [128, 4], F32, 6)

        def load_k(kslot):
            kT_t, kT_tb = kTr.next()
            for h4 in range(4):
                dma("sp", kT_t[:, h4 * 1024:(h4 + 1) * 1024], kT_d[kslot, :, h4 * 1024:(h4 + 1) * 1024], reads=[kT_db], writes=[kT_tb])
            return kT_t, kT_tb

        def load_v(vslot):
            V_t, V_tb = Vr.next()
            for h4 in range(4):
                dma("act", V_t[:, h4 * 8:(h4 + 1) * 8, 0:128],
                    V_d[h4 * 1024:(h4 + 1) * 1024, vslot * 128:(vslot + 1) * 128].rearrange("(c p) d -> p c d", p=128),
                    reads=[V_db], writes=[V_tb])
            return V_t, V_tb

        def shift_const(kT_t, kT_tb, qh):
            nct, nct_b = ncr.next()
            m8a, m8a_b = m8r.next()
            m8b, m8b_b = m8r.next()
            for g in range(8):
                sq_, sq_b = ptr.next()
                op("act", lambda e: e.activation(out=sq_[:], in_=kT_t[:, g * 512:(g + 1) * 512], func=AF.Square), reads=[kT_tb], writes=[sq_b])
                ps, ps_b = pringA.next()
                op("pe", lambda e: e.matmul(ps[:, :], lhsT=ones_b[:], rhs=sq_[:], start=True, stop=True),
                   reads=[ones_bb, sq_b], writes=[ps_b])
                op("dve", lambda e: e.tensor_reduce(out=m8a[:, g:g + 1], in_=ps[:, :], axis=AX.X, op=ALU.max),
                   reads=[ps_b], writes=[m8a_b])
            op("dve", lambda e: e.tensor_reduce(out=nct[:, 0:1], in_=m8a[:], axis=AX.X, op=ALU.max), reads=[m8a_b], writes=[nct_b])
            for g, (c0, wd) in enumerate(((0, 512), (512, 512), (1024, 128))):
                sq_, sq_b = ptr.next()
                op("act", lambda e: e.activation(out=sq_[:, 0:wd], in_=qT[:, qh, c0:c0 + wd], func=AF.Square), reads=[qT_b], writes=[sq_b])
                ps, ps_b = pringA.next()
                op("pe", lambda e: e.matmul(ps[:, 0:wd], lhsT=ones_b[:], rhs=sq_[:, 0:wd], start=True, stop=True),
                   reads=[ones_bb, sq_b], writes=[ps_b])
                op("dve", lambda e: e.tensor_reduce(out=m8b[:, g:g + 1], in_=ps[:, 0:wd], axis=AX.X, op=ALU.max),
                   reads=[ps_b], writes=[m8b_b])
            op("dve", lambda e: e.tensor_reduce(out=nct[:, 1:2], in_=m8b[:, 0:3], axis=AX.X, op=ALU.max), reads=[m8b_b], writes=[nct_b])
            op("dve", lambda e: e.tensor_tensor(out=nct[:, 2:3], in0=nct[:, 0:1], in1=nct[:, 1:2], op=ALU.mult), reads=[nct_b], writes=[nct_b])
            op("act", lambda e: e.activation(out=nct[:, 3:4], in_=nct[:, 2:3], func=AF.Sqrt), reads=[nct_b], writes=[nct_b])
            op("dve", lambda e: e.tensor_scalar(out=nct[:, 0:1], in0=nct[:, 3:4], scalar1=-SCALE * 1.02, scalar2=None, op0=ALU.mult),
               reads=[nct_b], writes=[nct_b])
            return nct, nct_b

        def emit_oT(ob, ob_b, rows, slot, t):
            ps, ps_b = pringA.next()
            psb = ps[:, :].bitcast(BF16)
            op("pe", lambda e: e.transpose(out=psb[:, 0:128], in_=ob[:, :], identity=idb[:, :]),
               reads=[ob_b, idb_b], writes=[ps_b])
            ost, ost_b = ostr.next()
            op("act", lambda e: e.copy(out=ost[:, 0:128], in_=psb[:, 0:128]), reads=[ps_b], writes=[ost_b])
            dma("sp", oT_d[slot, :, t * 128:t * 128 + rows], ost[:, 0:rows], reads=[ost_b], writes=[oT_db])

        with ExitStack() as S4a:
            accr = _ring(nc, S4a, "acc", [128, 129], F32, 9)
            smr = _ring(nc, S4a, "sm", [128, 16], F32, 8)
            selr = _ring(nc, S4a, "sel", [128, NQT, 16], F32, 2)
            kmr = _ring(nc, S4a, "km", [128, 16], F32, 2)
            kmhr = _ring(nc, S4a, "kmh", [128, 16], BF16, 2)
            kmlr = _ring(nc, S4a, "kml", [128, 16], BF16, 2)
            for hd in range(8 if stop_after != "nsa_only" else 0):
                kT_t, kT_tb = load_k(hd)
                V_t, V_tb = load_v(hd)
                nct, nct_b = shift_const(kT_t, kT_tb, hd)
                km, km_b = kmr.next(); kmh, kmh_b = kmhr.next(); kml, kml_b = kmlr.next()
                op("dve", lambda e: e.tensor_reduce(out=km[:], in_=kT_t[:, :].rearrange("p (n k) -> p n k", n=16), axis=AX.X, op=ALU.add),
                   reads=[kT_tb], writes=[km_b])
                op("dve", lambda e: e.tensor_scalar(out=km[:], in0=km[:], scalar1=1.0 / 256, scalar2=None, op0=ALU.mult), reads=[km_b], writes=[km_b])
                op("dve", lambda e: e.tensor_copy(out=kmh[:], in_=km[:]), reads=[km_b], writes=[kmh_b])
                op("dve", lambda e: e.tensor_tensor(out=kml[:], in0=km[:], in1=kmh[:], op=ALU.subtract), reads=[km_b, kmh_b], writes=[kml_b])
                sel, sel_b = selr.next()
                for t in range(NQT):
                    ps, ps_b = pringA.next()
                    op("pe", lambda e: e.matmul(ps[:, 0:16], lhsT=qT[:, hd, t * 128:(t + 1) * 128], rhs=kmh[:], start=True, stop=False),
                       reads=[qT_b, kmh_b], writes=[ps_b], sig=False)
                    op("pe", lambda e: e.matmul(ps[:, 0:16], lhsT=qT[:, hd, t * 128:(t + 1) * 128], rhs=kml[:], start=False, stop=True),
                       reads=[qT_b, kml_b], writes=[ps_b])
                    past, past_b = smr.next(); own, own_b = smr.next(); t1, t1_b = smr.next(); gm, gm_b = smr.next()
                    m8, m8_b = m8r.next()
                    qc = qpc[:, t:t + 1]
                    op("dve", lambda e: e.tensor_scalar(out=past[:], in0=cst[:, 32:48], scalar1=qc, scalar2=None, op0=ALU.is_le),
                       reads=[cst_b, qpc_b], writes=[past_b])
                    op("dve", lambda e: e.tensor_scalar(out=own[:], in0=cst[:, 48:64], scalar1=qc, scalar2=None, op0=ALU.is_le),
                       reads=[cst_b, qpc_b], writes=[own_b])
                    op("dve", lambda e: e.tensor_tensor(out=own[:], in0=own[:], in1=past[:], op=ALU.subtract), reads=[own_b, past_b], writes=[own_b])
                    op("dve", lambda e: e.tensor_scalar(out=t1[:], in0=past[:], scalar1=1e30, scalar2=-1e30, op0=ALU.mult, op1=ALU.add),
                       reads=[past_b], writes=[t1_b])
                    op("dve", lambda e: e.tensor_tensor(out=gm[:], in0=ps[:, 0:16], in1=past[:], op=ALU.mult), reads=[ps_b, past_b], writes=[gm_b])
                    op("dve", lambda e: e.tensor_tensor(out=gm[:], in0=gm[:], in1=t1[:], op=ALU.add), reads=[gm_b, t1_b], writes=[gm_b])
                    op("dve", lambda e: e.max(out=m8[:], in_=gm[:]), reads=[gm_b], writes=[m8_b])
                    op("dve", lambda e: e.tensor_scalar(out=t1[:], in0=gm[:], scalar1=m8[:, 2:3], scalar2=None, op0=ALU.is_ge),
                       reads=[gm_b, m8_b, t1_b], writes=[t1_b])
                    op("dve", lambda e: e.tensor_tensor(out=t1[:], in0=t1[:], in1=past[:], op=ALU.mult), reads=[t1_b, past_b], writes=[t1_b])
                    op("dve", lambda e: e.tensor_tensor(out=sel[:, t, :], in0=t1[:], in1=own[:], op=ALU.add), reads=[t1_b, own_b], writes=[sel_b])
                for (g0, wg, tiles, nvis) in GROUPS:
                    rows = 128 if wg == 512 else wg
                    accs = [accr.next() for _ in tiles]
                    for (a, a_b) in accs:
                        op("pool", lambda e, a=a: e.memset(a[:], 0.0), writes=[a_b])
                    for n in range(nvis // 2):
                        pts = []
                        for r in range(2):
                            c = 2 * n + r
                            ps, ps_b = pringA.next()
                            op("pe", lambda e: e.matmul(ps[:, 0:wg], lhsT=kT_t[:, c * 128:(c + 1) * 128], rhs=qT[:, hd, g0:g0 + wg],
                                                        start=True, stop=True), reads=[kT_tb, qT_b], writes=[ps_b])
                            pt, pt_b = ptr.next()
                            op("act", lambda e: e.activation(out=pt[:, 0:wg], in_=ps[:, 0:wg], func=AF.Exp, scale=SCALE, bias=nct[:, 0:1]),
                               reads=[ps_b, nct_b], writes=[pt_b])
                            op("pool", lambda e: e.tensor_tensor(out=pt[:, 0:wg], in0=pt[:, 0:wg], in1=cm[:, c, g0:g0 + wg], op=ALU.mult),
                               reads=[pt_b, cm_b], writes=[pt_b])
                            pts.append((pt, pt_b))
                        for ti, t in enumerate(tiles):
                            ps, ps_b = pringB.next()
                            for r in range(2):
                                pt, pt_b = pts[r]
                                op("pe", lambda e: e.matmul(ps[:, 0:129], lhsT=pt[:, ti * 128:(ti + 1) * 128], rhs=V_t[:, 2 * n + r, :],
                                                            start=(r == 0), stop=(r == 1)), reads=[pt_b, V_tb], writes=[ps_b], sig=(r == 1))
                            a, a_b = accs[ti]
                            op("dve", lambda e: e.scalar_tensor_tensor(out=a[0:rows, :], in0=ps[0:rows, 0:129], scalar=sel[0:rows, t, n:n + 1],
                                                                       in1=a[0:rows, :], op0=ALU.mult, op1=ALU.add),
                               reads=[ps_b, sel_b, a_b], writes=[a_b])
                    for ti, t in enumerate(tiles):
                        a, a_b = accs[ti]
                        rv, rv_b = rvr.next()
                        op("dve", lambda e: e.tensor_scalar(out=rv[0:rows, 0:1], in0=a[0:rows, 128:129], scalar1=1e-30, scalar2=None, op0=ALU.add),
                           reads=[a_b], writes=[rv_b])
                        op("dve", lambda e: e.reciprocal(out=rv[0:rows, 1:2], in_=rv[0:rows, 0:1]), reads=[rv_b], writes=[rv_b])
                        ob, ob_b = obr.next()
                        op("dve", lambda e: e.tensor_scalar(out=ob[0:rows, :], in0=a[0:rows, 0:128], scalar1=rv[0:rows, 1:2], scalar2=None, op0=ALU.mult),
                           reads=[a_b, rv_b], writes=[ob_b])
                        emit_oT(ob, ob_b, rows, hd, t)
            SC.barrier()
        if stop_after == "moba":
            if dbg:
                SC.barrier()
                dma("sp", dbg_aps["d_oT"][:, :, :], oT_d[:, :, :].rearrange("s d t -> d s t"), reads=[oT_db], writes=[out_buf])
            SC.barrier()
            S4.close(); A.close()
            return _finish(nc, es, SC, out_buf)

        with ExitStack() as S4b:
            sg, sg_b = _sb(nc, S4b, "sg", [128, NQT, 24], F32)
            op("act", lambda e: e.activation(out=sg[:], in_=glog[:], func=AF.Sigmoid), reads=[glog_b], writes=[sg_b])
            reg = qT[:, 0:8, :].rearrange("p a b -> p (a b)")
            aind = reg[:, 4096:8192]
            aind_b = Buf("aind_v")
            dma("sp", aind, aind_in[:, :], writes=[aind_b])
            w1slots = [(reg[:, 0:2048].rearrange("p (l h) -> p l h", h=256), Buf("w1a")),
                       (reg[:, 2048:4096].rearrange("p (l h) -> p l h", h=256), Buf("w1b"))]
            w1ring = Ring(w1slots)
            mb, mb_b = _sb(nc, S4b, "mb", [128, NQT, 255], BF16)
            for t in range(NQT):
                op("dve", lambda e: e.tensor_scalar(out=mb[:, t, :], in0=cst[:, 256:511], scalar1=qpc[:, t:t + 1], scalar2=None, op0=ALU.is_le),
                   reads=[cst_b, qpc_b], writes=[mb_b])
                op("dve", lambda e: e.tensor_scalar(out=mb[:, t, :], in0=mb[:, t, :], scalar1=30000.0, scalar2=-30000.0, op0=ALU.mult, op1=ALU.add),
                   reads=[mb_b], writes=[mb_b])
            oacc = [_sb(nc, S4b, "oacc%d" % g, [128, NQT, 128], F32) for g in range(4)]
            selT, selT_b = _sb(nc, S4b, "selT", [128, NQ], BF16)
            op("pool", lambda e: e.memset(selT[:], 0.0), writes=[selT_b])
            hidT, hidT_b = _sb(nc, S4b, "hidT", [128, 2, 256], BF16)
            kcmpT, kcmpT_b = _sb(nc, S4b, "kcmpT", [128, 256], BF16)
            vcmp, vcmp_b = _sb(nc, S4b, "vcmp", [128, 2, 128], BF16)
            w2t, w2t_b = _sb(nc, S4b, "w2t", [128, 2, 128], BF16)
            posT, posT_b = _sb(nc, S4b, "posT", [128, 32], BF16)
            cbr = _ring(nc, S4b, "cb", [128, 1], F32, 2)
            zr = _ring(nc, S4b, "z", [128, 255], F32, 2)
            p32r = _ring(nc, S4b, "p32", [128, 255], F32, 2)
            pbr = _ring(nc, S4b, "pb", [128, 256], BF16, 2)
            pTr = _ring(nc, S4b, "pT", [128, 2, 128], BF16, 2)
            ips = [_sb(nc, S4b, "ipk%d" % t, [128, 260], F32) for t in range(NQT)]
            s64r = _ring(nc, S4b, "s64", [128, 64], F32, 10)
            sbr = _ring(nc, S4b, "sb16", [128, 64], BF16, 2)
            wtr = _ring(nc, S4b, "wt", [128, 512], BF16, 3)

            def compress(src_slot, w1_in, w2_in, pos_in, is_k):
                srcT, srcT_b = load_k(src_slot)
                dma("pool", w2t[:], w2_in.rearrange("(hh p) d -> p hh d", p=128), writes=[w2t_b])
                dma("pool", posT[:], pos_in[:, :], writes=[posT_b])
                for hh in range(2):
                    ps, ps_b = pringA.next()
                    psc, psc_b = pringA.next()
                    for l4 in range(4):
                        w1s, w1s_b = w1ring.next()
                        dma("pool", w1s, w1_in[l4 * 1024:(l4 + 1) * 1024, :].rearrange("(l d) h -> d l h", d=128), writes=[w1s_b])
                        for l8 in range(8):
                            l = l4 * 8 + l8
                            op("pe", lambda e, l=l, l8=l8: e.matmul(ps[:, 0:255], lhsT=w1s[:, l8, hh * 128:(hh + 1) * 128], rhs=srcT[:, l:l + 4065:16],
                                                                    start=(l == 0), stop=(l == 31)), reads=[w1s_b, srcT_b], writes=[ps_b], sig=(l == 31))
                            op("pe", lambda e, l=l, l8=l8: e.matmul(psc[:, 0:1], lhsT=w1s[:, l8, hh * 128:(hh + 1) * 128], rhs=posT[:, l:l + 1],
                                                                    start=(l == 0), stop=(l == 31)), reads=[w1s_b, posT_b], writes=[psc_b], sig=True)
                    cb, cb_b = cbr.next()
                    op("dve", lambda e: e.tensor_copy(out=cb[:], in_=psc[:, 0:1]), reads=[psc_b], writes=[cb_b])
                    op("act", lambda e: e.activation(out=hidT[:, hh, 0:255], in_=ps[:, 0:255], func=AF.Gelu_apprx_tanh, bias=cb[:, 0:1]),
                       reads=[ps_b, cb_b], writes=[hidT_b])
                if is_k:
                    ps, ps_b = pringA.next()
                    for hh in range(2):
                        op("pe", lambda e: e.matmul(ps[:, 0:255], lhsT=w2t[:, hh, :], rhs=hidT[:, hh, 0:255], start=(hh == 0), stop=(hh == 1)),
                           reads=[w2t_b, hidT_b], writes=[ps_b], sig=(hh == 1))
                    op("act", lambda e: e.copy(out=kcmpT[:, 0:255], in_=ps[:, 0:255]), reads=[ps_b], writes=[kcmpT_b])
                else:
                    for i2, rws in ((0, 128), (1, 127)):
                        ps, ps_b = pringA.next()
                        for hh in range(2):
                            op("pe", lambda e: e.matmul(ps[0:rws, 0:128], lhsT=hidT[:, hh, i2 * 128:i2 * 128 + rws], rhs=w2t[:, hh, :],
                                                        start=(hh == 0), stop=(hh == 1)), reads=[w2t_b, hidT_b], writes=[ps_b], sig=(hh == 1))
                        op("act", lambda e: e.copy(out=vcmp[0:rws, i2, :], in_=ps[0:rws, 0:128]), reads=[ps_b], writes=[vcmp_b])

            for kh in range(2):
                compress(8 + kh, cmp_w1_k, cmp_w2_k, cpos_k, True)
                compress(14 + kh, cmp_w1_v, cmp_w2_v, cpos_v, False)
                for t in range(NQT):
                    ip, ip_b = ips[t]
                    op("pool", lambda e, ip=ip: e.memset(ip[:], 0.0), writes=[ip_b])
                for g in range(4):
                    qh = 4 * kh + g
                    oa, oa_b = oacc[g]
                    for t in range(NQT):
                        ip, ip_b = ips[t]
                        ps, ps_b = pringA.next()
                        op("pe", lambda e: e.matmul(ps[:, 0:255], lhsT=qT[:, 8 + qh, t * 128:(t + 1) * 128], rhs=kcmpT[:, 0:255], start=True, stop=True),
                           reads=[qT_b, kcmpT_b], writes=[ps_b])
                        z, z_b = zr.next()
                        op("dve", lambda e: e.scalar_tensor_tensor(out=z[:], in0=ps[:, 0:255], scalar=SCALE, in1=mb[:, t, :], op0=ALU.mult, op1=ALU.add),
                           reads=[ps_b, mb_b], writes=[z_b])
                        rv, rv_b = rvr.next()
                        op("dve", lambda e: e.tensor_reduce(out=rv[:, 0:1], in_=z[:], axis=AX.X, op=ALU.max), reads=[z_b], writes=[rv_b])
                        op("dve", lambda e: e.tensor_scalar(out=rv[:, 1:2], in0=rv[:, 0:1], scalar1=-10000.0, scalar2=-1.0, op0=ALU.max, op1=ALU.mult),
                           reads=[rv_b], writes=[rv_b])
                        p32, p32_b = p32r.next()
                        op("act", lambda e: e.activation(out=p32[:], in_=z[:], func=AF.Exp, bias=rv[:, 1:2], accum_out=rv[:, 2:3]),
                           reads=[z_b, rv_b], writes=[p32_b, rv_b])
                        op("dve", lambda e: e.tensor_scalar(out=rv[:, 3:4], in0=rv[:, 2:3], scalar1=1e-30, scalar2=None, op0=ALU.add),
                           reads=[rv_b], writes=[rv_b])
                        op("dve", lambda e: e.reciprocal(out=rv[:, 0:1], in_=rv[:, 3:4]), reads=[rv_b], writes=[rv_b])
                        op("dve", lambda e: e.scalar_tensor_tensor(out=ip[:, 1:256], in0=p32[:], scalar=rv[:, 0:1], in1=ip[:, 1:256], op0=ALU.mult, op1=ALU.add),
                           reads=[p32_b, rv_b, ip_b], writes=[ip_b])
                        pb, pb_b = pbr.next()
                        op("pool", lambda e: e.tensor_copy(out=pb[:, 0:255], in_=p32[:]), reads=[p32_b], writes=[pb_b])
                        psT, psT_b = pringA.next()
                        psTb = psT[:, :].bitcast(BF16)
                        op("pe", lambda e: e.transpose(out=psTb[:, 0:128], in_=pb[:, 0:128], identity=idb[:]), reads=[pb_b, idb_b], writes=[psT_b], sig=False)
                        op("pe", lambda e: e.transpose(out=psTb[0:127, 128:256], in_=pb[:, 128:255], identity=idb[:]), reads=[pb_b, idb_b], writes=[psT_b])
                        pT, pT_b = pTr.next()
                        op("act", lambda e: e.copy(out=pT[:, 0, :], in_=psTb[:, 0:128]), reads=[psT_b], writes=[pT_b])
                        op("act", lambda e: e.copy(out=pT[0:127, 1, :], in_=psTb[0:127, 128:256]), reads=[psT_b], writes=[pT_b])
                        po, po_b = pringA.next()
                        op("pe", lambda e: e.matmul(po[:, 0:128], lhsT=pT[:, 0, :], rhs=vcmp[:, 0, :], start=True, stop=False),
                           reads=[pT_b, vcmp_b], writes=[po_b], sig=False)
                        op("pe", lambda e: e.matmul(po[:, 0:128], lhsT=pT[0:127, 1, :], rhs=vcmp[0:127, 1, :], start=False, stop=True),
                           reads=[pT_b, vcmp_b], writes=[po_b])
                        op("dve", lambda e: e.tensor_tensor(out=rv[:, 1:2], in0=rv[:, 0:1], in1=sg[:, t, qh:qh + 1], op=ALU.mult),
                           reads=[rv_b, sg_b], writes=[rv_b])
                        op("dve", lambda e: e.tensor_scalar(out=oa[:, t, :], in0=po[:, 0:128], scalar1=rv[:, 1:2], scalar2=None, op0=ALU.mult),
                           reads=[po_b, rv_b], writes=[oa_b])
                for t in range(NQT):
                    ip, ip_b = ips[t]
                    qc = qpc[:, t:t + 1]
                    a1, a1_b = s64r.next(); a2, a2_b = s64r.next(); vl, vl_b = s64r.next(); fc, fc_b = s64r.next(); sc_, sc_b = s64r.next()
                    op("dve", lambda e: e.tensor_tensor(out=a1[:], in0=ip[:, 0:253:4], in1=ip[:, 4:257:4], op=ALU.add), reads=[ip_b], writes=[a1_b])
                    op("dve", lambda e: e.tensor_tensor(out=a2[:], in0=ip[:, 1:254:4], in1=ip[:, 2:255:4], op=ALU.add), reads=[ip_b], writes=[a2_b])
                    op("dve", lambda e: e.tensor_tensor(out=a2[:], in0=a2[:], in1=ip[:, 3:256:4], op=ALU.add), reads=[ip_b, a2_b], writes=[a2_b])
                    op("dve", lambda e: e.scalar_tensor_tensor(out=a1[:], in0=a2[:], scalar=2.0, in1=a1[:], op0=ALU.mult, op1=ALU.add),
                       reads=[a1_b, a2_b], writes=[a1_b])
                    op("dve", lambda e: e.tensor_scalar(out=vl[:], in0=cst[:, 64:128], scalar1=qc, scalar2=None, op0=ALU.is_le),
                       reads=[cst_b, qpc_b], writes=[vl_b])
                    op("dve", lambda e: e.tensor_scalar(out=fc[:], in0=cst[:, 128:192], scalar1=qc, scalar2=None, op0=ALU.is_gt),
                       reads=[cst_b, qpc_b], writes=[fc_b])
                    op("dve", lambda e: e.tensor_tensor(out=fc[:], in0=fc[:], in1=vl[:], op=ALU.mult), reads=[fc_b, vl_b], writes=[fc_b])
                    op("dve", lambda e: e.tensor_tensor(out=fc[:], in0=fc[:], in1=cst[:, 192:256], op=ALU.max), reads=[fc_b, cst_b], writes=[fc_b])
                    op("dve", lambda e: e.tensor_tensor(out=a1[:], in0=a1[:], in1=vl[:], op=ALU.mult), reads=[a1_b, vl_b], writes=[a1_b])
                    op("dve", lambda e: e.scalar_tensor_tensor(out=a1[:], in0=fc[:], scalar=1e9, in1=a1[:], op0=ALU.mult, op1=ALU.add),
                       reads=[a1_b, fc_b], writes=[a1_b])
                    op("dve", lambda e: e.tensor_scalar(out=a2[:], in0=vl[:], scalar1=1e30, scalar2=-1e30, op0=ALU.mult, op1=ALU.add),
                       reads=[vl_b, a2_b], writes=[a2_b])
                    op("dve", lambda e: e.tensor_tensor(out=sc_[:], in0=a1[:], in1=a2[:], op=ALU.add), reads=[a1_b, a2_b], writes=[sc_b])
                    m8a, m8a_b = m8r.next(); m8b, m8b_b = m8r.next()
                    op("dve", lambda e: e.max(out=m8a[:], in_=sc_[:]), reads=[sc_b], writes=[m8a_b])
                    op("dve", lambda e: e.match_replace(out=a1[:], in_to_replace=m8a[:], in_values=sc_[:], imm_value=-3e38),
                       reads=[m8a_b, sc_b, a1_b], writes=[a1_b])
                    op("dve", lambda e: e.max(out=m8b[:], in_=a1[:]), reads=[a1_b], writes=[m8b_b])
                    op("dve", lambda e: e.tensor_scalar(out=a2[:], in0=sc_[:], scalar1=m8b[:, 7:8], scalar2=None, op0=ALU.is_ge),
                       reads=[sc_b, m8b_b, a2_b], writes=[a2_b])
                    op("dve", lambda e: e.tensor_tensor(out=a2[:], in0=a2[:], in1=vl[:], op=ALU.mult), reads=[a2_b, vl_b], writes=[a2_b])
                    sb16, sb16_b = sbr.next()
                    op("dve", lambda e: e.tensor_scalar(out=sb16[:], in0=a2[:], scalar1=30000.0, scalar2=-30000.0, op0=ALU.mult, op1=ALU.add),
                       reads=[a2_b], writes=[sb16_b])
                    ps, ps_b = pringA.next()
                    psb = ps[:, :].bitcast(BF16)
                    op("pe", lambda e: e.transpose(out=psb[0:64, 0:128], in_=sb16[:, :], identity=idb[:]), reads=[sb16_b, idb_b], writes=[ps_b])
                    op("act", lambda e: e.copy(out=selT[0:64, t * 128:(t + 1) * 128], in_=psb[0:64, 0:128]), reads=[ps_b], writes=[selT_b])
                for branch in (1, 2):
                    kslot = (10 + kh) if branch == 1 else (12 + kh)
                    vslot = (8 + kh) if branch == 1 else (10 + kh)
                    kT_t, kT_tb = load_k(kslot)
                    V_t, V_tb = load_v(vslot)
                    for g in range(4):
                        qh = 4 * kh + g
                        oa, oa_b = oacc[g]
                        nct, nct_b = shift_const(kT_t, kT_tb, 8 + qh)
                        for (g0, wg, tiles, nvis) in GROUPS:
                            rows = 128 if wg == 512 else wg
                            accs = [pringB.next() for _ in tiles]
                            c0 = 12 if (branch == 2 and g0 == 512) else 0
                            for c in range(c0, nvis):
                                ps, ps_b = pringA.next()
                                if branch == 1:
                                    op("pe", lambda e: e.matmul(ps[:, 0:wg], lhsT=kT_t[:, c * 128:(c + 1) * 128], rhs=qT[:, 8 + qh, g0:g0 + wg],
                                                                start=True, stop=False), reads=[kT_tb, qT_b], writes=[ps_b], sig=False)
                                    op("pe", lambda e: e.matmul(ps[:, 0:wg], lhsT=aind[:, c * 128:(c + 1) * 128], rhs=selT[:, g0:g0 + wg],
                                                                start=False, stop=True), reads=[aind_b, selT_b], writes=[ps_b])
                                    msk = cm[:, c, g0:g0 + wg]
                                    msk_bufs = [cm_b]
                                else:
                                    op("pe", lambda e: e.matmul(ps[:, 0:wg], lhsT=kT_t[:, c * 128:(c + 1) * 128], rhs=qT[:, 8 + qh, g0:g0 + wg],
                                                                start=True, stop=True), reads=[kT_tb, qT_b], writes=[ps_b])
                                    if c + 4 < NKT:
                                        wt, wt_b = wtr.next()
                                        op("dve", lambda e: e.tensor_tensor(out=wt[:, 0:wg], in0=cm[:, c, g0:g0 + wg], in1=cm[:, c + 4, g0:g0 + wg],
                                                                            op=ALU.subtract), reads=[cm_b], writes=[wt_b])
                                        msk = wt[:, 0:wg]
                                        msk_bufs = [wt_b]
                                    else:
                                        msk = cm[:, c, g0:g0 + wg]
                                        msk_bufs = [cm_b]
                                pt, pt_b = ptr.next()
                                op("act", lambda e: e.activation(out=pt[:, 0:wg], in_=ps[:, 0:wg], func=AF.Exp, scale=SCALE, bias=nct[:, 0:1]),
                                   reads=[ps_b, nct_b], writes=[pt_b])
                                op("pool", lambda e: e.tensor_tensor(out=pt[:, 0:wg], in0=pt[:, 0:wg], in1=msk, op=ALU.mult),
                                   reads=[pt_b] + msk_bufs, writes=[pt_b])
                                for ti, t in enumerate(tiles):
                                    ac, ac_b = accs[ti]
                                    op("pe", lambda e: e.matmul(ac[:, 0:129], lhsT=pt[:, ti * 128:(ti + 1) * 128], rhs=V_t[:, c, :],
                                                                start=(c == c0), stop=(c == nvis - 1)), reads=[pt_b, V_tb], writes=[ac_b],
                                       sig=(c == nvis - 1))
                            for ti, t in enumerate(tiles):
                                ac, ac_b = accs[ti]
                                rv, rv_b = rvr.next()
                                op("dve", lambda e: e.tensor_scalar(out=rv[0:rows, 0:1], in0=ac[0:rows, 128:129], scalar1=1e-30, scalar2=None, op0=ALU.add),
                                   reads=[ac_b], writes=[rv_b])
                                op("dve", lambda e: e.reciprocal(out=rv[0:rows, 1:2], in_=rv[0:rows, 0:1]), reads=[rv_b], writes=[rv_b])
                                op("dve", lambda e: e.tensor_tensor(out=rv[0:rows, 2:3], in0=rv[0:rows, 1:2], in1=sg[0:rows, t, branch * 8 + qh:branch * 8 + qh + 1],
                                                                    op=ALU.mult), reads=[rv_b, sg_b], writes=[rv_b])
                                op("dve", lambda e: e.scalar_tensor_tensor(out=oa[0:rows, t, :], in0=ac[0:rows, 0:128], scalar=rv[0:rows, 2:3],
                                                                           in1=oa[0:rows, t, :], op0=ALU.mult, op1=ALU.add),
                                   reads=[ac_b, rv_b, oa_b], writes=[oa_b])
                for g in range(4):
                    oa, oa_b = oacc[g]
                    for t in range(NQT):
                        rows = 128 if t < 8 else HW
                        ob, ob_b = obr.next()
                        op("pool", lambda e: e.tensor_copy(out=ob[0:rows, :], in_=oa[0:rows, t, :]), reads=[oa_b], writes=[ob_b])
                        emit_oT(ob, ob_b, rows, 8 + 4 * kh + g, t)
            SC.barrier()
        if dbg:
            SC.barrier()
            dma("sp", dbg_aps["d_oT"][:, :, :], oT_d[:, :, :].rearrange("s d t -> d s t"), reads=[oT_db], writes=[out_buf])
        SC.barrier()
    A.close()
    if stop_after == "attn":
        return _finish(nc, es, SC, out_buf)

    with ExitStack() as S6:
        oTs, oTs_b = _sb(nc, S6, "oTs", [128, 16, NQ], BF16)
        wo, wo_b = _sb(nc, S6, "wo", [128, KC, D], BF16)
        gw, gw_b = _sb(nc, S6, "gw", [128, D], F32)
        tmpw, tmpw_b = _sb(nc, S6, "tmpw", [128, D], F32)
        xr6 = _ring(nc, S6, "x6", [128, D], F32, 2)
        yr6 = _ring(nc, S6, "y6", [128, D], F32, 2)
        st6 = _ring(nc, S6, "st6", [128, 8], F32, 3)
        for h4 in range(4):
            dma("sp", oTs[:, h4 * 4:(h4 + 1) * 4, :], oT_d[h4 * 4:(h4 + 1) * 4, :, :].rearrange("s d t -> d s t"), reads=[oT_db], writes=[oTs_b])
        for h2 in range(4):
            dma("pool", wo[:, h2 * 4:(h2 + 1) * 4, :], w_out[:, h2 * 4:(h2 + 1) * 4, :], writes=[wo_b])
        bcast_mod(gw, gw_b, 2, n_post_mix[0:1, :], False, tmpw, tmpw_b)
        for t in range(NQT):
            x, x_b = xr6.next()
            dma("sp", x[:], xq[t * 128:(t + 1) * 128, :], writes=[x_b])
            y, y_b = yr6.next()
            s8, s8_b = st6.next()
            for g4 in range(4):
                ps, ps_b = pringA.next()
                for e_ in range(16):
                    op("pe", lambda e, e_=e_: e.matmul(ps[:, :], lhsT=oTs[:, e_, t * 128:(t + 1) * 128], rhs=wo[:, e_, g4 * 512:(g4 + 1) * 512],
                                                       start=(e_ == 0), stop=(e_ == 15)), reads=[oTs_b, wo_b], writes=[ps_b], sig=(e_ == 15))
                op("act", lambda e: e.activation(out=y[:, g4 * 512:(g4 + 1) * 512], in_=ps[:, :], func=AF.Square, accum_out=s8[:, g4:g4 + 1]),
                   reads=[ps_b], writes=[y_b, s8_b])
                op("dve", lambda e: e.tensor_copy(out=y[:, g4 * 512:(g4 + 1) * 512], in_=ps[:, :]), reads=[ps_b, y_b], writes=[y_b])
            op("dve", lambda e: e.tensor_reduce(out=s8[:, 4:5], in_=s8[:, 0:4], axis=AX.X, op=ALU.add), reads=[s8_b], writes=[s8_b])
            op("dve", lambda e: e.tensor_scalar(out=s8[:, 5:6], in0=s8[:, 4:5], scalar1=1.0 / D, scalar2=EPS, op0=ALU.mult, op1=ALU.add),
               reads=[s8_b], writes=[s8_b])
            op("act", lambda e: e.activation(out=s8[:, 6:7], in_=s8[:, 5:6], func=AF.Sqrt), reads=[s8_b], writes=[s8_b])
            op("dve", lambda e: e.reciprocal(out=s8[:, 7:8], in_=s8[:, 6:7]), reads=[s8_b], writes=[s8_b])
            op("dve", lambda e: e.scalar_tensor_tensor(out=y[:], in0=y[:], scalar=s8[:, 7:8], in1=gw[:], op0=ALU.mult, op1=ALU.mult),
               reads=[y_b, s8_b, gw_b], writes=[y_b])
            op("pool", lambda e: e.tensor_tensor(out=x[:], in0=x[:], in1=y[:], op=ALU.add), reads=[x_b, y_b], writes=[x_b])
            dma("sp", x1_d[t * 128:(t + 1) * 128, :], x[:], reads=[x_b], writes=[x1_db])
        SC.barrier()

    if stop_after == "p6":
        return _finish(nc, es, SC, out_buf)

    with ExitStack() as S7:
        h2T, h2T_b = _sb(nc, S7, "h2T", [128, KC, NQ], BF16)
        cwt, cwt_b = _sb(nc, S7, "cwt", [128, 128, 3], F32)
        cbt, cbt_b = _sb(nc, S7, "cbt", [128, 128], F32)
        uhs, uhs_b = _sb(nc, S7, "uhs", [128, 64, 2, 4], F32)
        qfl, qfl_b = _sb(nc, S7, "qfl", [128, 4], F32)
        dma("sp", qfl[:], qflag_row[:, 1024:1028], writes=[qfl_b])
        dma("sp", cwt[:], cw_l[:, :, :], writes=[cwt_b])
        dma("sp", cbt[:], cb_l[:, :], writes=[cbt_b])
        with ExitStack() as S7n:
            wmod, wmod_b = _sb(nc, S7n, "wmodf", [128, D], F32)
            shm, shm_b = _sb(nc, S7n, "shmf", [128, D], F32)
            xr = _ring(nc, S7n, "xrf", [128, D], F32, 2)
            tr = _ring(nc, S7n, "trf", [128, D], F32, 1)
            hbr = _ring(nc, S7n, "hbrf", [128, D], BF16, 2)
            st = _ring(nc, S7n, "stf", [128, 4], F32, 3)
            bcast_mod(wmod, wmod_b, 4, n_pre_ffn[0:1, :], True, tr.items[0][0], tr.items[0][1])
            bcast_mod(shm, shm_b, 3, None, False, None, None)
            for t in range(NQT):
                _norm_tile(x1_d[t * 128:(t + 1) * 128, :], [x1_db], wmod, wmod_b, shm, shm_b, h2T, h2T_b, t * 128, xr, tr, hbr, st)
            SC.barrier()
        if stop_after == "p7n":
            S7.close()
            return _finish(nc, es, SC, out_buf)
        gT, gT_b = _sb(nc, S7, "gT", [128, 64, 512], BF16)
        for gi, g0 in enumerate((0, 512)):
            with ExitStack() as S7u:
                wur = _ring(nc, S7u, "wu_%d" % gi, [128, KC, 256], BF16, 2)
                ugr = _ring(nc, S7u, "ug_%d" % gi, [128, 514], F32, 2)
                uvr = _ring(nc, S7u, "uv_%d" % gi, [128, 514], F32, 2)
                cgr = _ring(nc, S7u, "cg_%d" % gi, [128, 512], F32, 2)
                cvr = _ring(nc, S7u, "cv_%d" % gi, [128, 512], F32, 2)
                ggr = _ring(nc, S7u, "gg_%d" % gi, [128, 512], F32, 2)
                uhtr = _ring(nc, S7u, "uht_%d" % gi, [128, 256], F32, 2)
                for i in range(64):
                    wu, wu_b = wur.next()
                    for h4 in range(2):
                        dma("pool", wu[:, h4 * 8:(h4 + 1) * 8, :], w_up_l[i, :, h4 * 8:(h4 + 1) * 8, :], writes=[wu_b])
                    if gi == 0:
                        ph, ph_b = pringB.next()
                        for k in range(KC):
                            op("pe", lambda e, k=k: e.matmul(ph[:, 0:256], lhsT=h2T[:, k, 1024:1152], rhs=wu[:, k, :],
                                                             start=(k == 0), stop=(k == KC - 1)), reads=[wu_b, h2T_b], writes=[ph_b], sig=(k == KC - 1))
                        uht, uht_b = uhtr.next()
                        op("act", lambda e: e.copy(out=uht[:], in_=ph[:, 0:256]), reads=[ph_b], writes=[uht_b])
                        pt2, pt2_b = pringB.next()
                        for half in range(2):
                            op("pe", lambda e, half=half: e.transpose(out=pt2[:, half * 128:(half + 1) * 128], in_=uht[:, half * 128:(half + 1) * 128],
                                                                      identity=idf[:]), reads=[uht_b, idf_b], writes=[pt2_b], sig=(half == 1))
                        for half in range(2):
                            op("dve", lambda e, half=half: e.tensor_tensor(out=uhs[:, i, half, :], in0=pt2[:, half * 128:half * 128 + 4],
                                                                           in1=qfl[:, 0:4], op=ALU.mult), reads=[pt2_b, qfl_b], writes=[uhs_b])
                    ug, ug_b = ugr.next()
                    uv, uv_b = uvr.next()
                    for half, (u, u_b) in enumerate(((ug, ug_b), (uv, uv_b))):
                        ps, ps_b = pringA.next()
                        for k in range(KC):
                            op("pe", lambda e, k=k: e.matmul(ps[:, :], lhsT=wu[:, k, half * 128:(half + 1) * 128], rhs=h2T[:, k, g0:g0 + 512],
                                                             start=(k == 0), stop=(k == KC - 1)), reads=[wu_b, h2T_b], writes=[ps_b], sig=(k == KC - 1))
                        if half == 0:
                            op("act", lambda e: e.copy(out=u[:, 2:514], in_=ps[:, :]), reads=[ps_b], writes=[u_b])
                        else:
                            op("act", lambda e: e.copy(out=u[:, 2:514], in_=ps[:, :]), reads=[ps_b], writes=[u_b])
                        op("pool", lambda e: e.tensor_copy(out=u[:, 0:2], in_=uhs[:, i, half, 2 * gi:2 * gi + 2]), reads=[uhs_b], writes=[u_b])
                    cg, cg_b = cgr.next()
                    cv, cv_b = cvr.next()
                    for half, (u, u_b, cdst, cdst_b) in enumerate(((ug, ug_b, cg, cg_b), (uv, uv_b, cv, cv_b))):
                        ch = half * 64 + i
                        op("dve", lambda e: e.tensor_scalar(out=cdst[:], in0=u[:, 2:514], scalar1=cwt[:, ch, 2:3], scalar2=cbt[:, ch:ch + 1],
                                                            op0=ALU.mult, op1=ALU.add), reads=[u_b, cwt_b, cbt_b], writes=[cdst_b])
                        op("dve", lambda e: e.scalar_tensor_tensor(out=cdst[:], in0=u[:, 1:513], scalar=cwt[:, ch, 1:2], in1=cdst[:],
                                                                    op0=ALU.mult, op1=ALU.add), reads=[u_b, cwt_b, cdst_b], writes=[cdst_b])
                        op("dve", lambda e: e.scalar_tensor_tensor(out=cdst[:], in0=u[:, 0:512], scalar=cwt[:, ch, 0:1], in1=cdst[:],
                                                                    op0=ALU.mult, op1=ALU.add), reads=[u_b, cwt_b, cdst_b], writes=[cdst_b])
                    gg, gg_b = ggr.next()
                    op("act", lambda e: e.activation(out=gg[:], in_=cg[:], func=AF.Gelu_apprx_tanh), reads=[cg_b], writes=[gg_b])
                    op("dve", lambda e: e.tensor_tensor(out=gT[:, i, :], in0=gg[:], in1=cv[:], op=ALU.mult), reads=[gg_b, cv_b], writes=[gT_b])
                SC.barrier()
            if stop_after == "p7u":
                S7.close()
                return _finish(nc, es, SC, out_buf)
            with ExitStack() as S7d:
                wdr = _ring(nc, S7d, "wd_%d" % gi, [128, 8, 512], BF16, 3)
                y2, y2_b = _sb(nc, S7d, "y2_%d" % gi, [128, 4, D], F32)
                gwf, gwf_b = _sb(nc, S7d, "gwf_%d" % gi, [128, D], F32)
                tmpf, tmpf_b = _sb(nc, S7d, "tmpf_%d" % gi, [128, D], F32)
                x1r = _ring(nc, S7d, "x1t_%d" % gi, [128, D], F32, 2)
                ssq, ssq_b = _sb(nc, S7d, "ssq_%d" % gi, [128, 32], F32)
                bcast_mod(gwf, gwf_b, 5, n_post_ffn[0:1, :], False, tmpf, tmpf_b)
                for d4 in range(4):
                    accs = [pringB.next() for _ in range(4)]
                    for s8i in range(8):
                        wd, wd_b = wdr.next()
                        for h2 in range(2):
                            dma("pool", wd[:, h2 * 4:(h2 + 1) * 4, :], w_down[d4, s8i, :, h2 * 4:(h2 + 1) * 4, :], writes=[wd_b])
                        for c in range(8):
                            fc_ = s8i * 8 + c
                            for tt in range(4):
                                ac, ac_b = accs[tt]
                                op("pe", lambda e: e.matmul(ac[:, :], lhsT=gT[:, fc_, tt * 128:(tt + 1) * 128], rhs=wd[:, c, :],
                                                            start=(fc_ == 0), stop=(fc_ == 63)), reads=[gT_b, wd_b], writes=[ac_b], sig=(fc_ == 63 or tt == 3))
                    for tt in range(4):
                        ac, ac_b = accs[tt]
                        op("act", lambda e: e.activation(out=y2[:, tt, d4 * 512:(d4 + 1) * 512], in_=ac[:, :], func=AF.Square,
                                                         accum_out=ssq[:, tt * 8 + d4:tt * 8 + d4 + 1]),
                           reads=[ac_b], writes=[y2_b, ssq_b])
                        op("dve", lambda e: e.tensor_copy(out=y2[:, tt, d4 * 512:(d4 + 1) * 512], in_=ac[:, :]), reads=[ac_b, y2_b], writes=[y2_b])
                for tt in range(4):
                    b8 = tt * 8
                    op("dve", lambda e: e.tensor_reduce(out=ssq[:, b8 + 4:b8 + 5], in_=ssq[:, b8:b8 + 4], axis=AX.X, op=ALU.add), reads=[ssq_b], writes=[ssq_b])
                    op("dve", lambda e: e.tensor_scalar(out=ssq[:, b8 + 5:b8 + 6], in0=ssq[:, b8 + 4:b8 + 5], scalar1=1.0 / D, scalar2=EPS, op0=ALU.mult, op1=ALU.add),
                       reads=[ssq_b], writes=[ssq_b])
                    op("act", lambda e: e.activation(out=ssq[:, b8 + 6:b8 + 7], in_=ssq[:, b8 + 5:b8 + 6], func=AF.Sqrt), reads=[ssq_b], writes=[ssq_b])
                    op("dve", lambda e: e.reciprocal(out=ssq[:, b8 + 7:b8 + 8], in_=ssq[:, b8 + 6:b8 + 7]), reads=[ssq_b], writes=[ssq_b])
                    x1, x1_b = x1r.next()
                    row0 = g0 + tt * 128
                    dma("sp", x1[:], x1_d[row0:row0 + 128, :], reads=[x1_db], writes=[x1_b])
                    op("dve", lambda e: e.scalar_tensor_tensor(out=y2[:, tt, :], in0=y2[:, tt, :], scalar=ssq[:, tt * 8 + 7:tt * 8 + 8], in1=gwf[:],
                                                               op0=ALU.mult, op1=ALU.mult), reads=[y2_b, ssq_b, gwf_b], writes=[y2_b])
                    op("pool", lambda e: e.tensor_tensor(out=x1[:], in0=x1[:], in1=y2[:, tt, :], op=ALU.add), reads=[x1_b, y2_b], writes=[x1_b])
                    dma("sp", out[row0:row0 + 128, :], x1[:], reads=[x1_b], writes=[out_buf])
                SC.barrier()
            if stop_after == "p7d":
                S7.close()
                return _finish(nc, es, SC, out_buf)

    return _finish(nc, es, SC, out_buf)


def _finish(nc, es, SC, out_buf):
    SC.wait_all("sp", [out_buf])
    SC.barrier()
    es.close()
    return nc


def _qtok(j):
    ga, gb = j, 4 + j
    own = np.concatenate([np.arange(512 * ga, 512 * ga + 512), np.arange(512 * gb, 512 * gb + 512)])
    halo = np.full(128, 512 * gb - 1)
    flag = np.ones(NQ, np.float32)
    halo[2] = 512 * gb - 2
    halo[3] = 512 * gb - 1
    if ga > 0:
        halo[0] = 512 * ga - 2
        halo[1] = 512 * ga - 1
    else:
        halo[0] = 0
        halo[1] = 0
        flag[1024:1026] = 0.0
    return np.concatenate([own, halo]), flag


def make_in_maps(inp):
    x = np.asarray(inp["x"], np.float32)
    c = np.asarray(inp["c"], np.float32)
    pos = np.asarray(inp["positions"], np.int32)
    w_in = np.asarray(inp["w_in"], np.float32)[0]
    mq, mk, mv, nq = w_in[:, 0:1024], w_in[:, 1024:2048], w_in[:, 2048:3072], w_in[:, 3072:4096]
    kc, vc, ks, vs, kw, vw = [w_in[:, 4096 + 256 * i: 4096 + 256 * (i + 1)] for i in range(6)]
    ng = w_in[:, 5632:5656]
    w_k = np.concatenate([mk, kc, ks, kw, vc, mv, vs, vw], axis=1)
    w_k = np.ascontiguousarray(w_k.reshape(KC, 128, 7, 512).transpose(2, 1, 0, 3))
    w_q = np.concatenate([mq, nq], axis=1)
    w_q = np.ascontiguousarray(w_q.reshape(KC, 128, 4, 512).transpose(2, 1, 0, 3))
    w_g = np.ascontiguousarray(ng.reshape(KC, 128, 24).transpose(1, 0, 2))
    half = 64
    inv_freq = (10000.0 ** (-np.arange(half, dtype=np.float32) / half)).astype(np.float32)
    consts = np.zeros((128, 1024), np.float32)
    consts[:, 0:32] = 128.0 * np.arange(32)[None, :] + np.arange(128)[:, None]
    consts[:, 32:48] = 256.0 * (np.arange(16)[None, :] + 1)
    consts[:, 48:64] = 256.0 * np.arange(16)[None, :]
    consts[:, 64:128] = 64.0 * np.arange(64)[None, :]
    consts[:, 128:192] = 64.0 * (np.arange(64)[None, :] + 2)
    consts[:, 192] = 1.0
    consts[:, 256:511] = 16.0 * np.arange(255)[None, :] + 31.0
    aind = np.zeros((128, NKT * 128), np.float32)
    aind[np.arange(NKT * 128) // 64, np.arange(NKT * 128)] = 1.0
    w_up = np.asarray(inp["w_up"], np.float32)[0]
    w_up_l = np.stack([w_up[:, :DFF].reshape(D, 64, 128), w_up[:, DFF:].reshape(D, 64, 128)], axis=2).reshape(KC, 128, 64, 256)
    w_up_l = np.ascontiguousarray(w_up_l.transpose(2, 1, 0, 3))
    w_down_l = np.ascontiguousarray(np.asarray(inp["w_down"], np.float32)[0].reshape(8, 8, 128, 4, 512).transpose(3, 0, 2, 1, 4))
    w_ada_l = np.ascontiguousarray(np.asarray(inp["w_ada"], np.float32)[0].reshape(KC, 128, 24, 512).transpose(2, 1, 0, 3))
    w_out_l = np.ascontiguousarray(np.asarray(inp["w_out"], np.float32)[0].reshape(KC, 128, D).transpose(1, 0, 2))
    conv_w = np.asarray(inp["conv_w"], np.float32)[0]
    conv_b = np.asarray(inp["conv_b"], np.float32)[0]
    cw_l = np.ascontiguousarray(conv_w.reshape(3, 128, 128).transpose(2, 1, 0))
    cb_l = np.ascontiguousarray(conv_b.reshape(128, 128).T)
    shared = {
        "w_ada": w_ada_l,
        "b_ada": np.ascontiguousarray(inp["b_ada"], dtype=np.float32).reshape(1, -1),
        "n_pre_mix": np.asarray(inp["norm_pre_mix"], np.float32).reshape(1, -1),
        "n_post_mix": np.asarray(inp["norm_post_mix"], np.float32).reshape(1, -1),
        "n_pre_ffn": np.asarray(inp["norm_pre_ffn"], np.float32).reshape(1, -1),
        "n_post_ffn": np.asarray(inp["norm_post_ffn"], np.float32).reshape(1, -1),
        "w_k": w_k, "w_q": w_q, "w_g": w_g,
        "w_out": w_out_l,
        "ident_f": np.eye(128, dtype=np.float32),
        "ident_b": np.eye(128, dtype=np.float32).astype(ml_dtypes.bfloat16),
        "invf": np.ascontiguousarray(np.broadcast_to(inv_freq[None, :], (128, 64))),
        "consts": consts,
        "aind": aind.astype(ml_dtypes.bfloat16),
        "cmp_w1_k": np.ascontiguousarray(inp["cmp_w1_k"][0], dtype=np.float32),
        "cmp_w2_k": np.ascontiguousarray(inp["cmp_w2_k"][0], dtype=np.float32),
        "cmp_w1_v": np.ascontiguousarray(inp["cmp_w1_v"][0], dtype=np.float32),
        "cmp_w2_v": np.ascontiguousarray(inp["cmp_w2_v"][0], dtype=np.float32),
        "cpos_k": np.ascontiguousarray(np.asarray(inp["cmp_pos_k"], np.float32)[0].T),
        "cpos_v": np.ascontiguousarray(np.asarray(inp["cmp_pos_v"], np.float32)[0].T),
        "w_up_l": w_up_l,
        "w_down": w_down_l,
        "cw_l": cw_l, "cb_l": cb_l,
    }
    maps = []
    for core in range(8):
        b, j = core // 4, core % 4
        qt, flag = _qtok(j)
        m = dict(shared)
        m["xall"] = np.ascontiguousarray(x[b])
        m["xq"] = np.ascontiguousarray(x[b][qt])
        m["posall"] = np.ascontiguousarray(pos[b].reshape(NKT, 128).T)
        m["posq"] = np.ascontiguousarray(pos[b][qt].reshape(NQT, 128).T)
        qf = qt.astype(np.float32)
        m["qpos_col"] = np.ascontiguousarray(qf.reshape(NQT, 128).T)
        m["qpos_row"] = np.ascontiguousarray(np.broadcast_to(qf[None, :], (128, NQ)))
        m["qflag_row"] = np.ascontiguousarray(np.broadcast_to(flag[None, :], (128, NQ)))
        m["c_col"] = np.ascontiguousarray(c[b].reshape(KC, 128).T)
        maps.append(m)
    return maps


def kernel(**inputs):
    nc = build_program()
    maps = make_in_maps(inputs)
    res = run_bass_kernel_spmd(nc, maps, core_ids=list(range(8)))
    outp = np.zeros((2, S, D), np.float32)
    for core in range(8):
        b, j = core // 4, core % 4
        o = res.results[core]["out"]
        outp[b, 512 * j:512 * j + 512] = o[0:512]
        outp[b, 512 * (4 + j):512 * (4 + j) + 512] = o[512:1024]
    return outp
```

```python
import math
from contextlib import ExitStack

import numpy as np
import ml_dtypes

import concourse.bass as bass
import concourse.mybir as mybir
from concourse.bass_utils import run_bass_kernel_spmd

F32 = mybir.dt.float32
F32R = mybir.dt.float32r
BF16 = mybir.dt.bfloat16
I32 = mybir.dt.int32
AF = mybir.ActivationFunctionType
ALU = mybir.AluOpType
AX = mybir.AxisListType

D = 2048
S = 4096
NKT = S // 128
NQT = 9
NQ = NQT * 128
HW = 32
KC = D // 128
DFF = 8192
EPS = 1e-6
SCALE = 128 ** -0.5
NDMA = 40


class Buf:
    __slots__ = ("name", "w", "r")

    def __init__(self, name):
        self.name = name
        self.w = {}
        self.r = {}


class Eng:
    def __init__(self, name, h, semid):
        self.name = name
        self.h = h
        self.semid = semid
        self.cnt = 0
        self.seen = {}


class Sched:
    def __init__(self, nc, es):
        self.nc = nc
        self.sems = []
        self.engs = {}
        for name, h in (("pe", nc.tensor), ("act", nc.scalar), ("dve", nc.vector),
                        ("pool", nc.gpsimd), ("sp", nc.sync)):
            sem = es.enter_context(nc.semaphore("s_" + name))
            self.sems.append(sem)
            self.engs[name] = Eng(name, h, len(self.sems) - 1)
        self.dma_semids = []
        self.dma_vals = []
        for i in range(NDMA):
            sem = es.enter_context(nc.semaphore("d_%d" % i))
            self.sems.append(sem)
            self.dma_semids.append(len(self.sems) - 1)
            self.dma_vals.append(0)
        self.dma_i = 0
        self.n_ins = 0
        self.n_waits = 0
        self.snaps = {}
        self.pending = {}

    def _learn(self, eng, sid, v):
        if eng.seen.get(sid, 0) < v:
            eng.seen[sid] = v
        snap = self.snaps.get((sid, v))
        if snap:
            es = eng.seen
            for s2, v2 in snap.items():
                if es.get(s2, 0) < v2:
                    es[s2] = v2

    def _waits(self, eng, reads, writes):
        need = {}
        own = eng.semid
        for b in reads:
            for sid, v in b.w.items():
                if need.get(sid, 0) < v:
                    need[sid] = v
        for b in writes:
            for sid, v in b.w.items():
                if sid != own and need.get(sid, 0) < v:
                    need[sid] = v
            for sid, v in b.r.items():
                if sid != own and need.get(sid, 0) < v:
                    need[sid] = v
        for sid, v in sorted(need.items(), key=lambda kv: -kv[1]):
            if eng.seen.get(sid, 0) < v:
                eng.h.wait_ge(self.sems[sid], v)
                self.n_ins += 1
                self.n_waits += 1
                self._learn(eng, sid, v)

    def op(self, ename, fn, reads=(), writes=(), sig=True):
        eng = self.engs[ename]
        self._waits(eng, reads, writes)
        ins = fn(eng.h)
        self.n_ins += 1
        if sig:
            eng.cnt += 1
            ins.then_inc(self.sems[eng.semid], 1)
            v = eng.cnt
            self.snaps[(eng.semid, v)] = dict(eng.seen)
        else:
            v = eng.cnt + 1
            self.pending[eng.semid] = True
        sid = eng.semid
        for b in reads:
            if b.r.get(sid, 0) < v:
                b.r[sid] = v
        for b in writes:
            if b.w.get(sid, 0) < v:
                b.w[sid] = v
        return ins

    def dma(self, qname, out, in_, reads=(), writes=()):
        eng = self.engs[qname]
        slot = self.dma_i % NDMA
        self.dma_i += 1
        sid = self.dma_semids[slot]
        prev = self.dma_vals[slot]
        if prev > 0 and eng.seen.get(sid, 0) < prev:
            eng.h.wait_ge(self.sems[sid], prev)
            self.n_waits += 1
            self._learn(eng, sid, prev)
        self._waits(eng, reads, writes)
        ins = eng.h.dma_start(out=out, in_=in_)
        ins.then_inc(self.sems[sid], 16)
        self.n_ins += 1
        v = prev + 16
        self.dma_vals[slot] = v
        self.snaps[(sid, v)] = dict(eng.seen)
        for b in reads:
            b.r[sid] = v
        for b in writes:
            b.w[sid] = v
        return ins

    def barrier(self):
        toks = {}
        for e in self.engs.values():
            if e.cnt:
                toks[e.semid] = e.cnt
        for sid, v in zip(self.dma_semids, self.dma_vals):
            if v:
                toks[sid] = v
        sp = self.engs["sp"]
        for sid, v in sorted(toks.items(), key=lambda kv: -kv[1]):
            if sp.seen.get(sid, 0) < v:
                sp.h.wait_ge(self.sems[sid], v)
                self.n_waits += 1
                self._learn(sp, sid, v)
        slot = self.dma_i % NDMA
        self.dma("sp", self.bar_dst, self.bar_src)
        sid = self.dma_semids[slot]
        v = self.dma_vals[slot]
        for e in self.engs.values():
            if e.seen.get(sid, 0) < v:
                e.h.wait_ge(self.sems[sid], v)
                self.n_waits += 1
                self._learn(e, sid, v)

    def wait_all(self, ename, bufs):
        eng = self.engs[ename]
        self._waits(eng, bufs, ())


class Ring:
    def __init__(self, items):
        self.items = items
        self.i = 0

    def next(self):
        it = self.items[self.i % len(self.items)]
        self.i += 1
        return it


def _sb(nc, es, name, shape, dt):
    t = es.enter_context(nc.sbuf_tensor(name, list(shape), dt))
    return t, Buf(name)


def _ring(nc, es, name, shape, dt, n):
    return Ring([_sb(nc, es, "%s%d" % (name, i), shape, dt) for i in range(n)])


def build_program(dbg=False, stop_after=None):
    nc = bass.Bass("TRN2", target_bir_lowering=False)
    es = ExitStack()

    def din(name, shape, dt=F32):
        return nc.dram_tensor(name, list(shape), dt, kind="ExternalInput").ap()

    def dscr(name, shape, dt):
        return nc.dram_tensor(name, list(shape), dt).ap(), Buf(name)

    xall = din("xall", [S, D])
    xq = din("xq", [NQ, D])
    posall = din("posall", [128, NKT], I32)
    posq = din("posq", [128, NQT], I32)
    qpos_col = din("qpos_col", [128, NQT])
    qpos_row = din("qpos_row", [128, NQ])
    qflag_row = din("qflag_row", [128, NQ])
    c_col = din("c_col", [128, KC])
    w_ada = din("w_ada", [24, 128, KC, 512])
    b_ada = din("b_ada", [1, 6 * D])
    n_pre_mix = din("n_pre_mix", [1, D])
    n_post_mix = din("n_post_mix", [1, D])
    n_pre_ffn = din("n_pre_ffn", [1, D])
    n_post_ffn = din("n_post_ffn", [1, D])
    w_k = din("w_k", [7, 128, KC, 512])
    w_q = din("w_q", [4, 128, KC, 512])
    w_g = din("w_g", [128, KC, 24])
    w_out = din("w_out", [128, KC, D])
    ident_f = din("ident_f", [128, 128])
    ident_b = din("ident_b", [128, 128], BF16)
    invf = din("invf", [128, 64])
    consts = din("consts", [128, 1024])
    aind_in = din("aind", [128, NKT * 128], BF16)
    cmp_w1_k = din("cmp_w1_k", [4096, 256])
    cmp_w2_k = din("cmp_w2_k", [256, 128])
    cmp_w1_v = din("cmp_w1_v", [4096, 256])
    cmp_w2_v = din("cmp_w2_v", [256, 128])
    cpos_k = din("cpos_k", [128, 32])
    cpos_v = din("cpos_v", [128, 32])
    w_up_l = din("w_up_l", [64, 128, KC, 256])
    w_down = din("w_down", [4, 8, 128, 8, 512])
    cw_l = din("cw_l", [128, 128, 3])
    cb_l = din("cb_l", [128, 128])
    out = nc.dram_tensor("out", [1024, D], F32, kind="ExternalOutput").ap()
    out_buf = Buf("out")
    dbg_aps = {}
    if dbg:
        for nm, shp, dt in (("d_mod", [1, 6 * D], F32), ("d_hT", [128, KC, 256], BF16),
                            ("d_kT", [16, 128, S], BF16), ("d_V", [S, 1536], BF16),
                            ("d_qT", [128, 16, NQ], BF16), ("d_gl", [128, NQT, 24], F32),
                            ("d_oT", [128, 16, NQ], BF16)):
            dbg_aps[nm] = nc.dram_tensor(nm, shp, dt, kind="ExternalOutput").ap()

    kT_d, kT_db = dscr("kT_d", [16, 128, S], BF16)
    V_d, V_db = dscr("V_d", [S, 1536], BF16)

    SC = Sched(nc, es)
    op, dma = SC.op, SC.dma
    SC.bar_dst = nc.dram_tensor("bar_d", [1, 16], F32).ap()[0:1, :]
    SC.bar_src = b_ada[0:1, 0:16]

    banks = []
    for i in range(8):
        t = es.enter_context(nc.psum_tensor("ps%d" % i, [128, 512], F32))
        banks.append((t, Buf("ps%d" % i)))
    pring = Ring(banks)

    P = ExitStack()
    es.enter_context(P)
    mod_d, mod_db = dscr("mod_d", [1, 6 * D], F32)
    idf, idf_b = _sb(nc, P, "idf", [128, 128], F32)
    idb, idb_b = _sb(nc, P, "idb", [128, 128], BF16)
    ones_f, ones_fb = _sb(nc, P, "ones_f", [128, 128], F32)
    cst, cst_b = _sb(nc, P, "cst", [128, 1024], F32)
    dma("sp", idf[:], ident_f[:, :], writes=[idf_b])
    dma("sp", idb[:], ident_b[:, :], writes=[idb_b])
    dma("sp", cst[:], consts[:, :], writes=[cst_b])
    op("pool", lambda e: e.memset(ones_f[:], 1.0), writes=[ones_fb])

    with ExitStack() as s0:
        cc, cc_b = _sb(nc, s0, "cc", [128, KC], F32)
        sT, sT_b = _sb(nc, s0, "sT", [128, KC], BF16)
        brow, brow_b = _sb(nc, s0, "brow", [1, 6 * D], F32)
        mrow = _ring(nc, s0, "mrow", [1, 512], F32, 2)
        war = _ring(nc, s0, "wa", [128, KC, 512], BF16, 3)
        dma("sp", cc[:], c_col[:, :], writes=[cc_b])
        dma("sp", brow[:], b_ada[:, :], writes=[brow_b])
        op("act", lambda e: e.activation(out=sT[:], in_=cc[:], func=AF.Silu), reads=[cc_b], writes=[sT_b])
        for g in range(24):
            wa, wa_b = war.next()
            for h2 in range(2):
                dma("pool", wa[:, h2 * 8:(h2 + 1) * 8, :], w_ada[g, :, h2 * 8:(h2 + 1) * 8, :], writes=[wa_b])
            ps, ps_b = pring.next()
            for k in range(KC):
                op("pe", lambda e, k=k: e.matmul(ps[0:1, :], lhsT=sT[:, k:k + 1],
                                                 rhs=wa[:, k, :],
                                                 start=(k == 0), stop=(k == KC - 1)),
                   reads=[sT_b, wa_b], writes=[ps_b], sig=(k == KC - 1))
            mr, mr_b = mrow.next()
            op("dve", lambda e: e.tensor_tensor(out=mr[0:1, :], in0=ps[0:1, :],
                                                in1=brow[0:1, g * 512:(g + 1) * 512], op=ALU.add),
               reads=[ps_b, brow_b], writes=[mr_b])
            dma("sp", mod_d[0:1, g * 512:(g + 1) * 512], mr[0:1, :], reads=[mr_b], writes=[mod_db])
        if dbg:
            SC.barrier()
            dma("sp", dbg_aps["d_mod"][:, :], mod_d[:, :], reads=[mod_db], writes=[out_buf])
        SC.barrier()

    def bcast_mod(dst, dst_b, idx, wrow_ap, plus_one, tmp, tmp_b):
        dma("sp", dst[:], mod_d[0:1, idx * D:(idx + 1) * D].to_broadcast([128, D]), reads=[mod_db], writes=[dst_b])
        if wrow_ap is not None:
            dma("act", tmp[:], wrow_ap.to_broadcast([128, D]), writes=[tmp_b])
            op("dve", lambda e: e.scalar_tensor_tensor(out=dst[:], in0=dst[:], scalar=(1.0 if plus_one else 0.0),
                                                       in1=tmp[:], op0=ALU.add, op1=ALU.mult),
               reads=[dst_b, tmp_b], writes=[dst_b])

    def _norm_tile(src_ap, src_bufs, wm, wm_b, sh, sh_b, dstT, dstT_b, col0, xr, tr, hbr, st):
        x, x_b = xr.next()
        dma("sp", x[:, 0:1024], src_ap[:, 0:1024], reads=src_bufs, writes=[x_b])
        dma("act", x[:, 1024:2048], src_ap[:, 1024:2048], reads=src_bufs, writes=[x_b])
        s4, s4_b = st.next()
        hb, hb_b = hbr.next()
        op("act", lambda e: e.activation(out=hb[:], in_=x[:], func=AF.Square, accum_out=s4[:, 0:1]),
           reads=[x_b], writes=[hb_b, s4_b])
        op("dve", lambda e: e.tensor_scalar(out=s4[:, 1:2], in0=s4[:, 0:1], scalar1=1.0 / D, scalar2=EPS,
                                            op0=ALU.mult, op1=ALU.add), reads=[s4_b], writes=[s4_b])
        op("act", lambda e: e.activation(out=s4[:, 2:3], in_=s4[:, 1:2], func=AF.Sqrt), reads=[s4_b], writes=[s4_b])
        op("dve", lambda e: e.reciprocal(out=s4[:, 3:4], in_=s4[:, 2:3]), reads=[s4_b], writes=[s4_b])
        t, t_b = tr.next()
        op("dve", lambda e: e.scalar_tensor_tensor(out=t[:], in0=x[:], scalar=s4[:, 3:4], in1=wm[:],
                                                   op0=ALU.mult, op1=ALU.mult),
           reads=[x_b, s4_b, wm_b], writes=[t_b])
        op("pool", lambda e: e.tensor_tensor(out=hb[:], in0=t[:], in1=sh[:], op=ALU.add),
           reads=[t_b, sh_b], writes=[hb_b])
        for c4 in range(4):
            ps, ps_b = pring.next()
            psb = ps[:, :].bitcast(BF16)
            for i in range(4):
                k = c4 * 4 + i
                op("pe", lambda e, k=k, i=i: e.transpose(out=psb[:, i * 128:(i + 1) * 128], in_=hb[:, k * 128:(k + 1) * 128],
                                                         identity=idb[:]),
                   reads=[hb_b, idb_b], writes=[ps_b], sig=(i == 3))
            dst = dstT[:, c4 * 4:(c4 + 1) * 4, col0:col0 + 128]
            srcv = psb[:, 0:512].rearrange("p (a b) -> p a b", a=4)
            if c4 % 2 == 0:
                op("act", lambda e: e.copy(out=dst, in_=srcv), reads=[ps_b], writes=[dstT_b])
            else:
                op("dve", lambda e: e.tensor_copy(out=dst, in_=srcv), reads=[ps_b], writes=[dstT_b])

    A = ExitStack()
    qT, qT_b = _sb(nc, A, "qT", [128, 16, NQ], BF16)
    glog, glog_b = _sb(nc, A, "glog", [128, NQT, 24], F32)

    with ExitStack() as S1:
        hT, hT_b = _sb(nc, S1, "hT", [128, KC, NQ], BF16)
        wmod, wmod_b = _sb(nc, S1, "wmod", [128, D], F32)
        shm, shm_b = _sb(nc, S1, "shm", [128, D], F32)
        xr = _ring(nc, S1, "xr", [128, D], F32, 2)
        tr = _ring(nc, S1, "tr", [128, D], F32, 1)
        hbr = _ring(nc, S1, "hbr", [128, D], BF16, 2)
        st = _ring(nc, S1, "st", [128, 4], F32, 3)
        bcast_mod(wmod, wmod_b, 1, n_pre_mix[0:1, :], True, tr.items[0][0], tr.items[0][1])
        bcast_mod(shm, shm_b, 0, None, False, None, None)

        def norm_tile(src_ap, wm, wm_b, sh, sh_b, dstT, dstT_b, col0):
            _norm_tile(src_ap, [], wm, wm_b, sh, sh_b, dstT, dstT_b, col0, xr, tr, hbr, st)

        def _unused():
            x, x_b = xr.next()
            s4, s4_b = st.next()
            hb, hb_b = hbr.next()
            op("act", lambda e: e.activation(out=hb[:], in_=x[:], func=AF.Square, accum_out=s4[:, 0:1]),
               reads=[x_b], writes=[hb_b, s4_b])
            op("dve", lambda e: e.tensor_scalar(out=s4[:, 1:2], in0=s4[:, 0:1], scalar1=1.0 / D, scalar2=EPS,
                                                op0=ALU.mult, op1=ALU.add), reads=[s4_b], writes=[s4_b])
            op("act", lambda e: e.activation(out=s4[:, 2:3], in_=s4[:, 1:2], func=AF.Sqrt), reads=[s4_b], writes=[s4_b])
            op("dve", lambda e: e.reciprocal(out=s4[:, 3:4], in_=s4[:, 2:3]), reads=[s4_b], writes=[s4_b])
            t, t_b = tr.next()
            op("dve", lambda e: e.scalar_tensor_tensor(out=t[:], in0=x[:], scalar=s4[:, 3:4], in1=wm[:],
                                                       op0=ALU.mult, op1=ALU.mult),
               reads=[x_b, s4_b, wm_b], writes=[t_b])
            op("pool", lambda e: e.tensor_tensor(out=hb[:], in0=t[:], in1=sh[:], op=ALU.add),
               reads=[t_b, sh_b], writes=[hb_b])
            for c4 in range(4):
                ps, ps_b = pring.next()
                psb = ps[:, :].bitcast(BF16)
                for i in range(4):
                    k = c4 * 4 + i
                    op("pe", lambda e, k=k, i=i: e.transpose(out=psb[:, i * 128:(i + 1) * 128], in_=hb[:, k * 128:(k + 1) * 128],
                                                             identity=idb[:]),
                       reads=[hb_b, idb_b], writes=[ps_b], sig=(i == 3))
                dst = dstT[:, c4 * 4:(c4 + 1) * 4, col0:col0 + 128]
                srcv = psb[:, 0:512].rearrange("p (a b) -> p a b", a=4)
                if c4 % 2 == 0:
                    op("act", lambda e: e.copy(out=dst, in_=srcv), reads=[ps_b], writes=[dstT_b])
                else:
                    op("dve", lambda e: e.tensor_copy(out=dst, in_=srcv), reads=[ps_b], writes=[dstT_b])

        cosK, cosK_b = _sb(nc, S1, "cosK", [128, NQT, 64], F32)
        sinK, sinK_b = _sb(nc, S1, "sinK", [128, NQT, 64], F32)
        pi_, pi_b = _sb(nc, S1, "posi", [128, NKT + NQT], I32)
        pf, pf_b = _sb(nc, S1, "posf", [128, NKT + NQT], F32)
        ivf, ivf_b = _sb(nc, S1, "ivf", [128, 64], F32)
        angr = _ring(nc, S1, "ang", [128, 64], F32, 6)
        dma("sp", pi_[:, 0:NKT], posall[:, :], writes=[pi_b])
        dma("sp", pi_[:, NKT:NKT + NQT], posq[:, :], writes=[pi_b])
        dma("sp", ivf[:], invf[:, :], writes=[ivf_b])
        op("dve", lambda e: e.tensor_copy(out=pf[:], in_=pi_[:]), reads=[pi_b], writes=[pf_b])
        negpi, negpi_b = _sb(nc, S1, "negpi", [128, 1], F32)
        op("pool", lambda e: e.memset(negpi[:], -math.pi), writes=[negpi_b])

        def rope_tables(tt0, n):
            MAGIC = 12582912.0
            for i in range(n):
                t = tt0 + i
                a, a_b = angr.next()
                op("dve", lambda e: e.tensor_scalar(out=a[:], in0=ivf[:], scalar1=pf[:, t:t + 1], scalar2=None, op0=ALU.mult),
                   reads=[ivf_b, pf_b], writes=[a_b])
                for (shift, dst, dst_b) in ((0.5 * math.pi, cosK, cosK_b), (0.0, sinK, sinK_b)):
                    a1, a1_b = angr.next()
                    a2, a2_b = angr.next()
                    op("dve", lambda e: e.tensor_scalar(out=a1[:], in0=a[:], scalar1=shift, scalar2=None, op0=ALU.add),
                       reads=[a_b], writes=[a1_b])
                    op("dve", lambda e: e.tensor_scalar(out=a2[:], in0=a1[:], scalar1=1.0 / (2 * math.pi), scalar2=MAGIC,
                                                        op0=ALU.mult, op1=ALU.add), reads=[a1_b], writes=[a2_b])
                    op("dve", lambda e: e.tensor_scalar(out=a2[:], in0=a2[:], scalar1=MAGIC, scalar2=None, op0=ALU.subtract),
                       reads=[a2_b], writes=[a2_b])
                    op("dve", lambda e: e.scalar_tensor_tensor(out=a1[:], in0=a2[:], scalar=-2 * math.pi, in1=a1[:],
                                                               op0=ALU.mult, op1=ALU.add), reads=[a2_b, a1_b], writes=[a1_b])
                    op("dve", lambda e: e.tensor_scalar(out=a1[:], in0=a1[:], scalar1=3.14159, scalar2=-3.14159,
                                                        op0=ALU.min, op1=ALU.max), reads=[a1_b], writes=[a1_b])
                    op("act", lambda e: e.activation(out=dst[:, i, :], in_=a1[:], func=AF.Sin),
                       reads=[a1_b], writes=[dst_b])

        wr = _ring(nc, S1, "wr", [128, KC, 512], BF16, 2)
        ta = _ring(nc, S1, "ta", [128, 4, 64], F32, 2)
        tb = _ring(nc, S1, "tb", [128, 4, 64], F32, 2)
        tc_ = _ring(nc, S1, "tc", [128, 4, 64], F32, 2)
        td = _ring(nc, S1, "td", [128, 4, 64], F32, 2)
        krr = _ring(nc, S1, "kr", [128, 4, 128], BF16, 2)
        kst = _ring(nc, S1, "kst", [128, 4, 512], BF16, 2)
        vst = _ring(nc, S1, "vst", [128, 512], BF16, 3)

        def load_w(src3, ncols=512):
            w, w_b = wr.next()
            for h2 in range(2):
                dma("pool", w[:, h2 * 8:(h2 + 1) * 8, 0:ncols], src3[:, h2 * 8:(h2 + 1) * 8, :], writes=[w_b])
            return w, w_b

        def proj_tile(col0, w, w_b, ncols=512):
            ps, ps_b = pring.next()
            for k in range(KC):
                op("pe", lambda e, k=k: e.matmul(ps[:, 0:ncols], lhsT=hT[:, k, col0:col0 + 128], rhs=w[:, k, 0:ncols],
                                                 start=(k == 0), stop=(k == KC - 1)),
                   reads=[hT_b, w_b], writes=[ps_b], sig=(k == KC - 1))
            return ps, ps_b

        def rope4(ps, ps_b, ti, nrope):
            kr, kr_b = krr.next()
            pv = ps[:, :].rearrange("p (h t d) -> p h t d", h=4, t=2)
            krv = kr[:, :, :].rearrange("p h (t d) -> p h t d", t=2)
            if nrope > 0:
                n = nrope
                cs = cosK[:, ti:ti + 1, :].to_broadcast([128, n, 64])
                sn = sinK[:, ti:ti + 1, :].to_broadcast([128, n, 64])
                a, a_b = ta.next(); b, b_b = tb.next(); c, c_b = tc_.next(); d, d_b = td.next()
                op("dve", lambda e: e.tensor_tensor(out=a[:, 0:n, :], in0=pv[:, 0:n, 0, :], in1=cs, op=ALU.mult),
                   reads=[ps_b, cosK_b], writes=[a_b])
                op("dve", lambda e: e.tensor_tensor(out=b[:, 0:n, :], in0=pv[:, 0:n, 1, :], in1=sn, op=ALU.mult),
                   reads=[ps_b, sinK_b], writes=[b_b])
                op("dve", lambda e: e.tensor_tensor(out=c[:, 0:n, :], in0=pv[:, 0:n, 1, :], in1=cs, op=ALU.mult),
                   reads=[ps_b, cosK_b], writes=[c_b])
                op("dve", lambda e: e.tensor_tensor(out=d[:, 0:n, :], in0=pv[:, 0:n, 0, :], in1=sn, op=ALU.mult),
                   reads=[ps_b, sinK_b], writes=[d_b])
                op("pool", lambda e: e.tensor_tensor(out=krv[:, 0:n, 0, :], in0=a[:, 0:n, :], in1=b[:, 0:n, :], op=ALU.subtract),
                   reads=[a_b, b_b], writes=[kr_b])
                op("pool", lambda e: e.tensor_tensor(out=krv[:, 0:n, 1, :], in0=c[:, 0:n, :], in1=d[:, 0:n, :], op=ALU.add),
                   reads=[c_b, d_b], writes=[kr_b])
            if nrope < 4:
                op("act", lambda e: e.copy(out=kr[:, nrope:4, :], in_=ps[:, nrope * 128:512].rearrange("p (h d) -> p h d", h=4 - nrope)),
                   reads=[ps_b], writes=[kr_b])
            return kr, kr_b

        def transpose4(kr, kr_b, dst, dst_b):
            ps, ps_b = pring.next()
            psb = ps[:, :].bitcast(BF16)
            for i in range(4):
                op("pe", lambda e, i=i: e.transpose(out=psb[:, i * 128:(i + 1) * 128], in_=kr[:, i, :], identity=idb[:]),
                   reads=[kr_b, idb_b], writes=[ps_b], sig=(i == 3))
            op("act", lambda e: e.copy(out=dst, in_=psb[:, 0:512].rearrange("p (a b) -> p a b", a=4)),
               reads=[ps_b], writes=[dst_b])

        for qtr in range(4):
            for t in range(8):
                tg = qtr * 8 + t
                norm_tile(xall[tg * 128:(tg + 1) * 128, :], wmod, wmod_b, shm, shm_b, hT, hT_b, t * 128)
            rope_tables(qtr * 8, 8)
            if dbg and qtr == 0:
                dma("sp", dbg_aps["d_hT"][:, :, 0:128], hT[:, :, 128:256], reads=[hT_b], writes=[out_buf])
            if stop_after == "norm":
                break
            for cg in range(7):
                w, w_b = load_w(w_k[cg])
                for t in range(8):
                    tg = qtr * 8 + t
                    ps, ps_b = proj_tile(t * 128, w, w_b)
                    if cg < 4:
                        nrope = 4 if cg < 3 else 2
                        kr, kr_b = rope4(ps, ps_b, t, nrope)
                        if t % 4 == 0:
                            ks, ks_b = kst.next()
                        transpose4(kr, kr_b, ks[:, :, (t % 4) * 128:(t % 4 + 1) * 128], ks_b)
                        if t % 4 == 3:
                            t0 = (tg // 4) * 512
                            dma("sp", kT_d[cg * 4:(cg + 1) * 4, :, t0:t0 + 512].rearrange("s d t -> d s t"), ks[:, :, :],
                                reads=[ks_b], writes=[kT_db])
                    else:
                        v, v_b = vst.next()
                        op("act", lambda e: e.copy(out=v[:], in_=ps[:, :]), reads=[ps_b], writes=[v_b])
                        dma("sp", V_d[tg * 128:(tg + 1) * 128, (cg - 4) * 512:(cg - 3) * 512], v[:], reads=[v_b], writes=[V_db])
        for t in range(NQT):
            norm_tile(xq[t * 128:(t + 1) * 128, :], wmod, wmod_b, shm, shm_b, hT, hT_b, t * 128)
        rope_tables(NKT, NQT)
        if dbg:
            dma("sp", dbg_aps["d_hT"][:, :, 128:256], hT[:, :, 0:128], reads=[hT_b], writes=[out_buf])
        if stop_after != "norm":
            for cg in range(4):
                w, w_b = load_w(w_q[cg])
                for t in range(NQT):
                    ps, ps_b = proj_tile(t * 128, w, w_b)
                    kr, kr_b = rope4(ps, ps_b, t, 4)
                    transpose4(kr, kr_b, qT[:, cg * 4:(cg + 1) * 4, t * 128:(t + 1) * 128], qT_b)
            wg, wg_b = load_w(w_g, ncols=24)
            for t in range(NQT):
                ps, ps_b = proj_tile(t * 128, wg, wg_b, ncols=24)
                op("act", lambda e: e.copy(out=glog[:, t, :], in_=ps[:, 0:24]), reads=[ps_b], writes=[glog_b])
            if dbg:
                SC.barrier()
                dma("sp", dbg_aps["d_kT"][:, :, :], kT_d[:, :, :], reads=[kT_db], writes=[out_buf])
                dma("sp", dbg_aps["d_V"][:, :], V_d[:, :], reads=[V_db], writes=[out_buf])
                dma("sp", dbg_aps["d_qT"][:, :, :], qT[:], reads=[qT_b], writes=[out_buf])
                dma("sp", dbg_aps["d_gl"][:, :, :], glog[:], reads=[glog_b], writes=[out_buf])
        SC.barrier()
    if stop_after in ("norm", "proj"):
        return _finish(nc, es, SC, out_buf)

    pringA = Ring(banks[0:4])
    pringB = Ring(banks[4:8])
    oT_d, oT_db = dscr("oT_d", [16, 128, NQ], BF16)
    x1_d, x1_db = dscr("x1_d", [NQ, D], F32)
    GROUPS = ((0, 512, (0, 1, 2, 3), 16), (512, 512, (4, 5, 6, 7), NKT), (1024, HW, (8,), 28))
    with ExitStack() as S4:
        qpc, qpc_b = _sb(nc, S4, "qpc", [128, NQT], F32)
        qpr, qpr_b = _sb(nc, S4, "qpr", [128, NQ], F32)
        cm, cm_b = _sb(nc, S4, "cm", [128, NKT, 1056], BF16)
        ones_b, ones_bb = _sb(nc, S4, "ones_b", [128, 128], BF16)
        dma("sp", qpc[:], qpos_col[:, :], writes=[qpc_b])
        dma("sp", qpr[:], qpos_row[:, :], writes=[qpr_b])
        op("pool", lambda e: e.memset(ones_b[:], 1.0), writes=[ones_bb])
        for c in range(NKT):
            op("dve", lambda e, c=c: e.tensor_scalar(out=cm[:, c, :], in0=qpr[:, 0:1056], scalar1=cst[:, c:c + 1], scalar2=None,
                                                     op0=ALU.is_ge), reads=[qpr_b, cst_b], writes=[cm_b])
        kTr = _ring(nc, S4, "kTt", [128, S], BF16, 2)
        Vr = _ring(nc, S4, "Vt", [128, NKT, 129], BF16, 2)
        for (v, v_b) in Vr.items:
            op("pool", lambda e, v=v: e.memset(v[:, :, 128:129], 1.0), writes=[v_b])
        ptr = _ring(nc, S4, "pt", [128, 512], BF16, 7)
        m8r = _ring(nc, S4, "m8", [128, 8], F32, 6)
        ncr = _ring(nc, S4, "ncr", [128, 4], F32, 2)
        obr = _ring(nc, S4, "ob", [128, 128], BF16, 3)
        ostr = _ring(nc, S4, "ost", [128, 128], BF16, 4)
        rvr = _ring(nc, S4, "rv", [128, 4], F32, 6)

        def load_k(kslot):
            kT_t, kT_tb = kTr.next()
            for h4 in range(4):
                dma("sp", kT_t[:, h4 * 1024:(h4 + 1) * 1024], kT_d[kslot, :, h4 * 1024:(h4 + 1) * 1024], reads=[kT_db], writes=[kT_tb])
            return kT_t, kT_tb

        def load_v(vslot):
            V_t, V_tb = Vr.next()
            for h4 in range(4):
                dma("act", V_t[:, h4 * 8:(h4 + 1) * 8, 0:128],
                    V_d[h4 * 1024:(h4 + 1) * 1024, vslot * 128:(vslot + 1) * 128].rearrange("(c p) d -> p c d", p=128),
                    reads=[V_db], writes=[V_tb])
            return V_t, V_tb

        def shift_const(kT_t, kT_tb, qh):
            nct, nct_b = ncr.next()
            m8a, m8a_b = m8r.next()
            m8b, m8b_b = m8r.next()
            for g in range(8):
                sq_, sq_b = ptr.next()
                op("act", lambda e: e.activation(out=sq_[:], in_=kT_t[:, g * 512:(g + 1) * 512], func=AF.Square), reads=[kT_tb], writes=[sq_b])
                ps, ps_b = pringA.next()
                op("pe", lambda e: e.matmul(ps[:, :], lhsT=ones_b[:], rhs=sq_[:], start=True, stop=True),
                   reads=[ones_bb, sq_b], writes=[ps_b])
                op("dve", lambda e: e.tensor_reduce(out=m8a[:, g:g + 1], in_=ps[:, :], axis=AX.X, op=ALU.max),
                   reads=[ps_b], writes=[m8a_b])
            op("dve", lambda e: e.tensor_reduce(out=nct[:, 0:1], in_=m8a[:], axis=AX.X, op=ALU.max), reads=[m8a_b], writes=[nct_b])
            for g, (c0, wd) in enumerate(((0, 512), (512, 512), (1024, 128))):
                sq_, sq_b = ptr.next()
                op("act", lambda e: e.activation(out=sq_[:, 0:wd], in_=qT[:, qh, c0:c0 + wd], func=AF.Square), reads=[qT_b], writes=[sq_b])
                ps, ps_b = pringA.next()
                op("pe", lambda e: e.matmul(ps[:, 0:wd], lhsT=ones_b[:], rhs=sq_[:, 0:wd], start=True, stop=True),
                   reads=[ones_bb, sq_b], writes=[ps_b])
                op("dve", lambda e: e.tensor_reduce(out=m8b[:, g:g + 1], in_=ps[:, 0:wd], axis=AX.X, op=ALU.max),
                   reads=[ps_b], writes=[m8b_b])
            op("dve", lambda e: e.tensor_reduce(out=nct[:, 1:2], in_=m8b[:, 0:3], axis=AX.X, op=ALU.max), reads=[m8b_b], writes=[nct_b])
            op("dve", lambda e: e.tensor_tensor(out=nct[:, 2:3], in0=nct[:, 0:1], in1=nct[:, 1:2], op=ALU.mult), reads=[nct_b], writes=[nct_b])
            op("act", lambda e: e.activation(out=nct[:, 3:4], in_=nct[:, 2:3], func=AF.Sqrt), reads=[nct_b], writes=[nct_b])
            op("dve", lambda e: e.tensor_scalar(out=nct[:, 0:1], in0=nct[:, 3:4], scalar1=-SCALE * 1.02, scalar2=None, op0=ALU.mult),
               reads=[nct_b], writes=[nct_b])
            return nct, nct_b

        def emit_oT(ob, ob_b, rows, slot, t):
            ps, ps_b = pringA.next()
            psb = ps[:, :].bitcast(BF16)
            op("pe", lambda e: e.transpose(out=psb[:, 0:128], in_=ob[:, :], identity=idb[:, :]),
               reads=[ob_b, idb_b], writes=[ps_b])
            ost, ost_b = ostr.next()
            op("act", lambda e: e.copy(out=ost[:, 0:128], in_=psb[:, 0:128]), reads=[ps_b], writes=[ost_b])
            dma("sp", oT_d[slot, :, t * 128:t * 128 + rows], ost[:, 0:rows], reads=[ost_b], writes=[oT_db])

        with ExitStack() as S4a:
            accr = _ring(nc, S4a, "acc", [128, 129], F32, 9)
            smr = _ring(nc, S4a, "sm", [128, 16], F32, 8)
            selr = _ring(nc, S4a, "sel", [128, NQT, 16], F32, 2)
            kmr = _ring(nc, S4a, "km", [128, 16], F32, 2)
            kmhr = _ring(nc, S4a, "kmh", [128, 16], BF16, 2)
            kmlr = _ring(nc, S4a, "kml", [128, 16], BF16, 2)
            for hd in range(8 if stop_after != "nsa_only" else 0):
                kT_t, kT_tb = load_k(hd)
                V_t, V_tb = load_v(hd)
                nct, nct_b = shift_const(kT_t, kT_tb, hd)
                km, km_b = kmr.next(); kmh, kmh_b = kmhr.next(); kml, kml_b = kmlr.next()
                op("dve", lambda e: e.tensor_reduce(out=km[:], in_=kT_t[:, :].rearrange("p (n k) -> p n k", n=16), axis=AX.X, op=ALU.add),
                   reads=[kT_tb], writes=[km_b])
                op("dve", lambda e: e.tensor_scalar(out=km[:], in0=km[:], scalar1=1.0 / 256, scalar2=None, op0=ALU.mult), reads=[km_b], writes=[km_b])
                op("dve", lambda e: e.tensor_copy(out=kmh[:], in_=km[:]), reads=[km_b], writes=[kmh_b])
                op("dve", lambda e: e.tensor_tensor(out=kml[:], in0=km[:], in1=kmh[:], op=ALU.subtract), reads=[km_b, kmh_b], writes=[kml_b])
                sel, sel_b = selr.next()
                for t in range(NQT):
                    ps, ps_b = pringA.next()
                    op("pe", lambda e: e.matmul(ps[:, 0:16], lhsT=qT[:, hd, t * 128:(t + 1) * 128], rhs=kmh[:], start=True, stop=False),
                       reads=[qT_b, kmh_b], writes=[ps_b], sig=False)
                    op("pe", lambda e: e.matmul(ps[:, 0:16], lhsT=qT[:, hd, t * 128:(t + 1) * 128], rhs=kml[:], start=False, stop=True),
                       reads=[qT_b, kml_b], writes=[ps_b])
                    past, past_b = smr.next(); own, own_b = smr.next(); t1, t1_b = smr.next(); gm, gm_b = smr.next()
                    m8, m8_b = m8r.next()
                    qc = qpc[:, t:t + 1]
                    op("dve", lambda e: e.tensor_scalar(out=past[:], in0=cst[:, 32:48], scalar1=qc, scalar2=None, op0=ALU.is_le),
                       reads=[cst_b, qpc_b], writes=[past_b])
                    op("dve", lambda e: e.tensor_scalar(out=own[:], in0=cst[:, 48:64], scalar1=qc, scalar2=None, op0=ALU.is_le),
                       reads=[cst_b, qpc_b], writes=[own_b])
                    op("dve", lambda e: e.tensor_tensor(out=own[:], in0=own[:], in1=past[:], op=ALU.subtract), reads=[own_b, past_b], writes=[own_b])
                    op("dve", lambda e: e.tensor_scalar(out=t1[:], in0=past[:], scalar1=1e30, scalar2=-1e30, op0=ALU.mult, op1=ALU.add),
                       reads=[past_b], writes=[t1_b])
                    op("dve", lambda e: e.tensor_tensor(out=gm[:], in0=ps[:, 0:16], in1=past[:], op=ALU.mult), reads=[ps_b, past_b], writes=[gm_b])
                    op("dve", lambda e: e.tensor_tensor(out=gm[:], in0=gm[:], in1=t1[:], op=ALU.add), reads=[gm_b, t1_b], writes=[gm_b])
                    op("dve", lambda e: e.max(out=m8[:], in_=gm[:]), reads=[gm_b], writes=[m8_b])
                    op("dve", lambda e: e.tensor_scalar(out=t1[:], in0=gm[:], scalar1=m8[:, 2:3], scalar2=None, op0=ALU.is_ge),
                       reads=[gm_b, m8_b, t1_b], writes=[t1_b])
                    op("dve", lambda e: e.tensor_tensor(out=t1[:], in0=t1[:], in1=past[:], op=ALU.mult), reads=[t1_b, past_b], writes=[t1_b])
                    op("dve", lambda e: e.tensor_tensor(out=sel[:, t, :], in0=t1[:], in1=own[:], op=ALU.add), reads=[t1_b, own_b], writes=[sel_b])
                for (g0, wg, tiles, nvis) in GROUPS:
                    rows = 128 if wg == 512 else wg
                    accs = [accr.next() for _ in tiles]
                    for (a, a_b) in accs:
                        op("pool", lambda e, a=a: e.memset(a[:], 0.0), writes=[a_b])
                    for n in range(nvis // 2):
                        pts = []
                        for r in range(2):
                            c = 2 * n + r
                            ps, ps_b = pringA.next()
                            op("pe", lambda e: e.matmul(ps[:, 0:wg], lhsT=kT_t[:, c * 128:(c + 1) * 128], rhs=qT[:, hd, g0:g0 + wg],
                                                        start=True, stop=True), reads=[kT_tb, qT_b], writes=[ps_b])
                            pt, pt_b = ptr.next()
                            op("act", lambda e: e.activation(out=pt[:, 0:wg], in_=ps[:, 0:wg], func=AF.Exp, scale=SCALE, bias=nct[:, 0:1]),
                               reads=[ps_b, nct_b], writes=[pt_b])
                            op("pool", lambda e: e.tensor_tensor(out=pt[:, 0:wg], in0=pt[:, 0:wg], in1=cm[:, c, g0:g0 + wg], op=ALU.mult),
                               reads=[pt_b, cm_b], writes=[pt_b])
                            pts.append((pt, pt_b))
                        for ti, t in enumerate(tiles):
                            ps, ps_b = pringB.next()
                            for r in range(2):
                                pt, pt_b = pts[r]
                                op("pe", lambda e: e.matmul(ps[:, 0:129], lhsT=pt[:, ti * 128:(ti + 1) * 128], rhs=V_t[:, 2 * n + r, :],
                                                            start=(r == 0), stop=(r == 1)), reads=[pt_b, V_tb], writes=[ps_b], sig=(r == 1))
                            a, a_b = accs[ti]
                            op("dve", lambda e: e.scalar_tensor_tensor(out=a[0:rows, :], in0=ps[0:rows, 0:129], scalar=sel[0:rows, t, n:n + 1],
                                                                       in1=a[0:rows, :], op0=ALU.mult, op1=ALU.add),
                               reads=[ps_b, sel_b, a_b], writes=[a_b])
                    for ti, t in enumerate(tiles):
                        a, a_b = accs[ti]
                        rv, rv_b = rvr.next()
                        op("dve", lambda e: e.tensor_scalar(out=rv[0:rows, 0:1], in0=a[0:rows, 128:129], scalar1=1e-30, scalar2=None, op0=ALU.add),
                           reads=[a_b], writes=[rv_b])
                        op("dve", lambda e: e.reciprocal(out=rv[0:rows, 1:2], in_=rv[0:rows, 0:1]), reads=[rv_b], writes=[rv_b])
                        ob, ob_b = obr.next()
                        op("dve", lambda e: e.tensor_scalar(out=ob[0:rows, :], in0=a[0:rows, 0:128], scalar1=rv[0:rows, 1:2], scalar2=None, op0=ALU.mult),
                           reads=[a_b, rv_b], writes=[ob_b])
                        emit_oT(ob, ob_b, rows, hd, t)
            SC.barrier()
        if stop_after == "moba":
            if dbg:
                SC.barrier()
                dma("sp", dbg_aps["d_oT"][:, :, :], oT_d[:, :, :].rearrange("s d t -> d s t"), reads=[oT_db], writes=[out_buf])
            SC.barrier()
            S4.close(); A.close()
            return _finish(nc, es, SC, out_buf)

        with ExitStack() as S4b:
            sg, sg_b = _sb(nc, S4b, "sg", [128, NQT, 24], F32)
            op("act", lambda e: e.activation(out=sg[:], in_=glog[:], func=AF.Sigmoid), reads=[glog_b], writes=[sg_b])
            reg = qT[:, 0:8, :].rearrange("p a b -> p (a b)")
            aind = reg[:, 4096:8192]
            aind_b = Buf("aind_v")
            dma("sp", aind, aind_in[:, :], writes=[aind_b])
            w1slots = [(reg[:, 0:2048].rearrange("p (l h) -> p l h", h=256), Buf("w1a")),
                       (reg[:, 2048:4096].rearrange("p (l h) -> p l h", h=256), Buf("w1b"))]
            w1ring = Ring(w1slots)
            mb, mb_b = _sb(nc, S4b, "mb", [128, NQT, 255], BF16)
            for t in range(NQT):
                op("dve", lambda e: e.tensor_scalar(out=mb[:, t, :], in0=cst[:, 256:511], scalar1=qpc[:, t:t + 1], scalar2=None, op0=ALU.is_le),
                   reads=[cst_b, qpc_b], writes=[mb_b])
                op("dve", lambda e: e.tensor_scalar(out=mb[:, t, :], in0=mb[:, t, :], scalar1=30000.0, scalar2=-30000.0, op0=ALU.mult, op1=ALU.add),
                   reads=[mb_b], writes=[mb_b])
            oacc = [_sb(nc, S4b, "oacc%d" % g, [128, NQT, 128], F32) for g in range(4)]
            selT, selT_b = _sb(nc, S4b, "selT", [128, NQ], BF16)
            op("pool", lambda e: e.memset(selT[:], 0.0), writes=[selT_b])
            hidT, hidT_b = _sb(nc, S4b, "hidT", [128, 2, 256], BF16)
            kcmpT, kcmpT_b = _sb(nc, S4b, "kcmpT", [128, 256], BF16)
            vcmp, vcmp_b = _sb(nc, S4b, "vcmp", [128, 2, 128], BF16)
            w2t, w2t_b = _sb(nc, S4b, "w2t", [128, 2, 128], BF16)
            posT, posT_b = _sb(nc, S4b, "posT", [128, 32], BF16)
            cbr = _ring(nc, S4b, "cb", [128, 1], F32, 2)
            zr = _ring(nc, S4b, "z", [128, 255], F32, 2)
            p32r = _ring(nc, S4b, "p32", [128, 255], F32, 2)
            pbr = _ring(nc, S4b, "pb", [128, 256], BF16, 2)
            pTr = _ring(nc, S4b, "pT", [128, 2, 128], BF16, 2)
            ips = [_sb(nc, S4b, "ipk%d" % t, [128, 260], F32) for t in range(NQT)]
            s64r = _ring(nc, S4b, "s64", [128, 64], F32, 10)
            sbr = _ring(nc, S4b, "sb16", [128, 64], BF16, 2)
            wtr = _ring(nc, S4b, "wt", [128, 512], BF16, 3)

            def compress(src_slot, w1_in, w2_in, pos_in, is_k):
                srcT, srcT_b = load_k(src_slot)
                dma("pool", w2t[:], w2_in.rearrange("(hh p) d -> p hh d", p=128), writes=[w2t_b])
                dma("pool", posT[:], pos_in[:, :], writes=[posT_b])
                for hh in range(2):
                    ps, ps_b = pringA.next()
                    psc, psc_b = pringA.next()
                    for l4 in range(4):
                        w1s, w1s_b = w1ring.next()
                        dma("pool", w1s, w1_in[l4 * 1024:(l4 + 1) * 1024, :].rearrange("(l d) h -> d l h", d=128), writes=[w1s_b])
                        for l8 in range(8):
                            l = l4 * 8 + l8
                            op("pe", lambda e, l=l, l8=l8: e.matmul(ps[:, 0:255], lhsT=w1s[:, l8, hh * 128:(hh + 1) * 128], rhs=srcT[:, l:l + 4065:16],
                                                                    start=(l == 0), stop=(l == 31)), reads=[w1s_b, srcT_b], writes=[ps_b], sig=(l == 31))
                            op("pe", lambda e, l=l, l8=l8: e.matmul(psc[:, 0:1], lhsT=w1s[:, l8, hh * 128:(hh + 1) * 128], rhs=posT[:, l:l + 1],
                                                                    start=(l == 0), stop=(l == 31)), reads=[w1s_b, posT_b], writes=[psc_b], sig=True)
                    cb, cb_b = cbr.next()
                    op("dve", lambda e: e.tensor_copy(out=cb[:], in_=psc[:, 0:1]), reads=[psc_b], writes=[cb_b])
                    op("act", lambda e: e.activation(out=hidT[:, hh, 0:255], in_=ps[:, 0:255], func=AF.Gelu_apprx_tanh, bias=cb[:, 0:1]),
                       reads=[ps_b, cb_b], writes=[hidT_b])
                if is_k:
                    ps, ps_b = pringA.next()
                    for hh in range(2):
                        op("pe", lambda e: e.matmul(ps[:, 0:255], lhsT=w2t[:, hh, :], rhs=hidT[:, hh, 0:255], start=(hh == 0), stop=(hh == 1)),
                           reads=[w2t_b, hidT_b], writes=[ps_b], sig=(hh == 1))
                    op("act", lambda e: e.copy(out=kcmpT[:, 0:255], in_=ps[:, 0:255]), reads=[ps_b], writes=[kcmpT_b])
                else:
                    for i2, rws in ((0, 128), (1, 127)):
                        ps, ps_b = pringA.next()
                        for hh in range(2):
                            op("pe", lambda e: e.matmul(ps[0:rws, 0:128], lhsT=hidT[:, hh, i2 * 128:i2 * 128 + rws], rhs=w2t[:, hh, :],
                                                        start=(hh == 0), stop=(hh == 1)), reads=[w2t_b, hidT_b], writes=[ps_b], sig=(hh == 1))
                        op("act", lambda e: e.copy(out=vcmp[0:rws, i2, :], in_=ps[0:rws, 0:128]), reads=[ps_b], writes=[vcmp_b])

            for kh in range(2):
                compress(8 + kh, cmp_w1_k, cmp_w2_k, cpos_k, True)
                compress(14 + kh, cmp_w1_v, cmp_w2_v, cpos_v, False)
                for t in range(NQT):
                    ip, ip_b = ips[t]
                    op("pool", lambda e, ip=ip: e.memset(ip[:], 0.0), writes=[ip_b])
                for g in range(4):
                    qh = 4 * kh + g
                    oa, oa_b = oacc[g]
                    for t in range(NQT):
                        ip, ip_b = ips[t]
                        ps, ps_b = pringA.next()
                        op("pe", lambda e: e.matmul(ps[:, 0:255], lhsT=qT[:, 8 + qh, t * 128:(t + 1) * 128], rhs=kcmpT[:, 0:255], start=True, stop=True),
                           reads=[qT_b, kcmpT_b], writes=[ps_b])
                        z, z_b = zr.next()
                        op("dve", lambda e: e.scalar_tensor_tensor(out=z[:], in0=ps[:, 0:255], scalar=SCALE, in1=mb[:, t, :], op0=ALU.mult, op1=ALU.add),
                           reads=[ps_b, mb_b], writes=[z_b])
                        rv, rv_b = rvr.next()
                        op("dve", lambda e: e.tensor_reduce(out=rv[:, 0:1], in_=z[:], axis=AX.X, op=ALU.max), reads=[z_b], writes=[rv_b])
                        op("dve", lambda e: e.tensor_scalar(out=rv[:, 1:2], in0=rv[:, 0:1], scalar1=-10000.0, scalar2=-1.0, op0=ALU.max, op1=ALU.mult),
                           reads=[rv_b], writes=[rv_b])
                        p32, p32_b = p32r.next()
                        op("act", lambda e: e.activation(out=p32[:], in_=z[:], func=AF.Exp, bias=rv[:, 1:2], accum_out=rv[:, 2:3]),
                           reads=[z_b, rv_b], writes=[p32_b, rv_b])
                        op("dve", lambda e: e.tensor_scalar(out=rv[:, 3:4], in0=rv[:, 2:3], scalar1=1e-30, scalar2=None, op0=ALU.add),
                           reads=[rv_b], writes=[rv_b])
                        op("dve", lambda e: e.reciprocal(out=rv[:, 0:1], in_=rv[:, 3:4]), reads=[rv_b], writes=[rv_b])
                        op("dve", lambda e: e.scalar_tensor_tensor(out=ip[:, 1:256], in0=p32[:], scalar=rv[:, 0:1], in1=ip[:, 1:256], op0=ALU.mult, op1=ALU.add),
                           reads=[p32_b, rv_b, ip_b], writes=[ip_b])
                        pb, pb_b = pbr.next()
                        op("pool", lambda e: e.tensor_copy(out=pb[:, 0:255], in_=p32[:]), reads=[p32_b], writes=[pb_b])
                        psT, psT_b = pringA.next()
                        psTb = psT[:, :].bitcast(BF16)
                        op("pe", lambda e: e.transpose(out=psTb[:, 0:128], in_=pb[:, 0:128], identity=idb[:]), reads=[pb_b, idb_b], writes=[psT_b], sig=False)
                        op("pe", lambda e: e.transpose(out=psTb[0:127, 128:256], in_=pb[:, 128:255], identity=idb[:]), reads=[pb_b, idb_b], writes=[psT_b])
                        pT, pT_b = pTr.next()
                        op("act", lambda e: e.copy(out=pT[:, 0, :], in_=psTb[:, 0:128]), reads=[psT_b], writes=[pT_b])
                        op("act", lambda e: e.copy(out=pT[0:127, 1, :], in_=psTb[0:127, 128:256]), reads=[psT_b], writes=[pT_b])
                        po, po_b = pringA.next()
                        op("pe", lambda e: e.matmul(po[:, 0:128], lhsT=pT[:, 0, :], rhs=vcmp[:, 0, :], start=True, stop=False),
                           reads=[pT_b, vcmp_b], writes=[po_b], sig=False)
                        op("pe", lambda e: e.matmul(po[:, 0:128], lhsT=pT[0:127, 1, :], rhs=vcmp[0:127, 1, :], start=False, stop=True),
                           reads=[pT_b, vcmp_b], writes=[po_b])
                        op("dve", lambda e: e.tensor_tensor(out=rv[:, 1:2], in0=rv[:, 0:1], in1=sg[:, t, qh:qh + 1], op=ALU.mult),
                           reads=[rv_b, sg_b], writes=[rv_b])
                        op("dve", lambda e: e.tensor_scalar(out=oa[:, t, :], in0=po[:, 0:128], scalar1=rv[:, 1:2], scalar2=None, op0=ALU.mult),
                           reads=[po_b, rv_b], writes=[oa_b])
                for t in range(NQT):
                    ip, ip_b = ips[t]
                    qc = qpc[:, t:t + 1]
                    a1, a1_b = s64r.next(); a2, a2_b = s64r.next(); vl, vl_b = s64r.next(); fc, fc_b = s64r.next(); sc_, sc_b = s64r.next()
                    op("dve", lambda e: e.tensor_tensor(out=a1[:], in0=ip[:, 0:253:4], in1=ip[:, 4:257:4], op=ALU.add), reads=[ip_b], writes=[a1_b])
                    op("dve", lambda e: e.tensor_tensor(out=a2[:], in0=ip[:, 1:254:4], in1=ip[:, 2:255:4], op=ALU.add), reads=[ip_b], writes=[a2_b])
                    op("dve", lambda e: e.tensor_tensor(out=a2[:], in0=a2[:], in1=ip[:, 3:256:4], op=ALU.add), reads=[ip_b, a2_b], writes=[a2_b])
                    op("dve", lambda e: e.scalar_tensor_tensor(out=a1[:], in0=a2[:], scalar=2.0, in1=a1[:], op0=ALU.mult, op1=ALU.add),
                       reads=[a1_b, a2_b], writes=[a1_b])
                    op("dve", lambda e: e.tensor_scalar(out=vl[:], in0=cst[:, 64:128], scalar1=qc, scalar2=None, op0=ALU.is_le),
                       reads=[cst_b, qpc_b], writes=[vl_b])
                    op("dve", lambda e: e.tensor_scalar(out=fc[:], in0=cst[:, 128:192], scalar1=qc, scalar2=None, op0=ALU.is_gt),
                       reads=[cst_b, qpc_b], writes=[fc_b])
                    op("dve", lambda e: e.tensor_tensor(out=fc[:], in0=fc[:], in1=vl[:], op=ALU.mult), reads=[fc_b, vl_b], writes=[fc_b])
                    op("dve", lambda e: e.tensor_tensor(out=fc[:], in0=fc[:], in1=cst[:, 192:256], op=ALU.max), reads=[fc_b, cst_b], writes=[fc_b])
                    op("dve", lambda e: e.tensor_tensor(out=a1[:], in0=a1[:], in1=vl[:], op=ALU.mult), reads=[a1_b, vl_b], writes=[a1_b])
                    op("dve", lambda e: e.scalar_tensor_tensor(out=a1[:], in0=fc[:], scalar=1e9, in1=a1[:], op0=ALU.mult, op1=ALU.add),
                       reads=[a1_b, fc_b], writes=[a1_b])
                    op("dve", lambda e: e.tensor_scalar(out=a2[:], in0=vl[:], scalar1=1e30, scalar2=-1e30, op0=ALU.mult, op1=ALU.add),
                       reads=[vl_b, a2_b], writes=[a2_b])
                    op("dve", lambda e: e.tensor_tensor(out=sc_[:], in0=a1[:], in1=a2[:], op=ALU.add), reads=[a1_b, a2_b], writes=[sc_b])
                    m8a, m8a_b = m8r.next(); m8b, m8b_b = m8r.next()
                    op("dve", lambda e: e.max(out=m8a[:], in_=sc_[:]), reads=[sc_b], writes=[m8a_b])
                    op("dve", lambda e: e.match_replace(out=a1[:], in_to_replace=m8a[:], in_values=sc_[:], imm_value=-3e38),
                       reads=[m8a_b, sc_b, a1_b], writes=[a1_b])
                    op("dve", lambda e: e.max(out=m8b[:], in_=a1[:]), reads=[a1_b], writes=[m8b_b])
                    op("dve", lambda e: e.tensor_scalar(out=a2[:], in0=sc_[:], scalar1=m8b[:, 7:8], scalar2=None, op0=ALU.is_ge),
                       reads=[sc_b, m8b_b, a2_b], writes=[a2_b])
                    op("dve", lambda e: e.tensor_tensor(out=a2[:], in0=a2[:], in1=vl[:], op=ALU.mult), reads=[a2_b, vl_b], writes=[a2_b])
                    sb16, sb16_b = sbr.next()
                    op("dve", lambda e: e.tensor_scalar(out=sb16[:], in0=a2[:], scalar1=30000.0, scalar2=-30000.0, op0=ALU.mult, op1=ALU.add),
                       reads=[a2_b], writes=[sb16_b])
                    ps, ps_b = pringA.next()
                    psb = ps[:, :].bitcast(BF16)
                    op("pe", lambda e: e.transpose(out=psb[0:64, 0:128], in_=sb16[:, :], identity=idb[:]), reads=[sb16_b, idb_b], writes=[ps_b])
                    op("act", lambda e: e.copy(out=selT[0:64, t * 128:(t + 1) * 128], in_=psb[0:64, 0:128]), reads=[ps_b], writes=[selT_b])
                for branch in (1, 2):
                    kslot = (10 + kh) if branch == 1 else (12 + kh)
                    vslot = (8 + kh) if branch == 1 else (10 + kh)
                    kT_t, kT_tb = load_k(kslot)
                    V_t, V_tb = load_v(vslot)
                    for g in range(4):
                        qh = 4 * kh + g
                        oa, oa_b = oacc[g]
                        nct, nct_b = shift_const(kT_t, kT_tb, 8 + qh)
                        for (g0, wg, tiles, nvis) in GROUPS:
                            rows = 128 if wg == 512 else wg
                            accs = [pringB.next() for _ in tiles]
                            c0 = 12 if (branch == 2 and g0 == 512) else 0
                            for c in range(c0, nvis):
                                ps, ps_b = pringA.next()
                                if branch == 1:
                                    op("pe", lambda e: e.matmul(ps[:, 0:wg], lhsT=kT_t[:, c * 128:(c + 1) * 128], rhs=qT[:, 8 + qh, g0:g0 + wg],
                                                                start=True, stop=False), reads=[kT_tb, qT_b], writes=[ps_b], sig=False)
                                    op("pe", lambda e: e.matmul(ps[:, 0:wg], lhsT=aind[:, c * 128:(c + 1) * 128], rhs=selT[:, g0:g0 + wg],
                                                                start=False, stop=True), reads=[aind_b, selT_b], writes=[ps_b])
                                    msk = cm[:, c, g0:g0 + wg]
                                    msk_bufs = [cm_b]
                                else:
                                    op("pe", lambda e: e.matmul(ps[:, 0:wg], lhsT=kT_t[:, c * 128:(c + 1) * 128], rhs=qT[:, 8 + qh, g0:g0 + wg],
                                                                start=True, stop=True), reads=[kT_tb, qT_b], writes=[ps_b])
                                    if c + 4 < NKT:
                                        wt, wt_b = wtr.next()
                                        op("dve", lambda e: e.tensor_tensor(out=wt[:, 0:wg], in0=cm[:, c, g0:g0 + wg], in1=cm[:, c + 4, g0:g0 + wg],
                                                                            op=ALU.subtract), reads=[cm_b], writes=[wt_b])
                                        msk = wt[:, 0:wg]
                                        msk_bufs = [wt_b]
                                    else:
                                        msk = cm[:, c, g0:g0 + wg]
                                        msk_bufs = [cm_b]
                                pt, pt_b = ptr.next()
                                op("act", lambda e: e.activation(out=pt[:, 0:wg], in_=ps[:, 0:wg], func=AF.Exp, scale=SCALE, bias=nct[:, 0:1]),
                                   reads=[ps_b, nct_b], writes=[pt_b])
                                op("pool", lambda e: e.tensor_tensor(out=pt[:, 0:wg], in0=pt[:, 0:wg], in1=msk, op=ALU.mult),
                                   reads=[pt_b] + msk_bufs, writes=[pt_b])
                                for ti, t in enumerate(tiles):
                                    ac, ac_b = accs[ti]
                                    op("pe", lambda e: e.matmul(ac[:, 0:129], lhsT=pt[:, ti * 128:(ti + 1) * 128], rhs=V_t[:, c, :],
                                                                start=(c == c0), stop=(c == nvis - 1)), reads=[pt_b, V_tb], writes=[ac_b],
                                       sig=(c == nvis - 1))
                            for ti, t in enumerate(tiles):
                                ac, ac_b = accs[ti]
                                rv, rv_b = rvr.next()
                                op("dve", lambda e: e.tensor_scalar(out=rv[0:rows, 0:1], in0=ac[0:rows, 128:129], scalar1=1e-30, scalar2=None, op0=ALU.add),
                                   reads=[ac_b], writes=[rv_b])
                                op("dve", lambda e: e.reciprocal(out=rv[0:rows, 1:2], in_=rv[0:rows, 0:1]), reads=[rv_b], writes=[rv_b])
                                op("dve", lambda e: e.tensor_tensor(out=rv[0:rows, 2:3], in0=rv[0:rows, 1:2], in1=sg[0:rows, t, branch * 8 + qh:branch * 8 + qh + 1],
                                                                    op=ALU.mult), reads=[rv_b, sg_b], writes=[rv_b])
                                op("dve", lambda e: e.scalar_tensor_tensor(out=oa[0:rows, t, :], in0=ac[0:rows, 0:128], scalar=rv[0:rows, 2:3],
                                                                           in1=oa[0:rows, t, :], op0=ALU.mult, op1=ALU.add),
                                   reads=[ac_b, rv_b, oa_b], writes=[oa_b])
                for g in range(4):
                    oa, oa_b = oacc[g]
                    for t in range(NQT):
                        rows = 128 if t < 8 else HW
                        ob, ob_b = obr.next()
                        op("pool", lambda e: e.tensor_copy(out=ob[0:rows, :], in_=oa[0:rows, t, :]), reads=[oa_b], writes=[ob_b])
                        emit_oT(ob, ob_b, rows, 8 + 4 * kh + g, t)
            SC.barrier()
        if dbg:
            SC.barrier()
            dma("sp", dbg_aps["d_oT"][:, :, :], oT_d[:, :, :].rearrange("s d t -> d s t"), reads=[oT_db], writes=[out_buf])
        SC.barrier()
    A.close()
    if stop_after == "attn":
        return _finish(nc, es, SC, out_buf)

    with ExitStack() as S6:
        oTs, oTs_b = _sb(nc, S6, "oTs", [128, 16, NQ], BF16)
        wo, wo_b = _sb(nc, S6, "wo", [128, KC, D], BF16)
        gw, gw_b = _sb(nc, S6, "gw", [128, D], F32)
        tmpw, tmpw_b = _sb(nc, S6, "tmpw", [128, D], F32)
        xr6 = _ring(nc, S6, "x6", [128, D], F32, 2)
        yr6 = _ring(nc, S6, "y6", [128, D], F32, 2)
        st6 = _ring(nc, S6, "st6", [128, 8], F32, 3)
        for h4 in range(4):
            dma("sp", oTs[:, h4 * 4:(h4 + 1) * 4, :], oT_d[h4 * 4:(h4 + 1) * 4, :, :].rearrange("s d t -> d s t"), reads=[oT_db], writes=[oTs_b])
        for h2 in range(4):
            dma("pool", wo[:, h2 * 4:(h2 + 1) * 4, :], w_out[:, h2 * 4:(h2 + 1) * 4, :], writes=[wo_b])
        bcast_mod(gw, gw_b, 2, n_post_mix[0:1, :], False, tmpw, tmpw_b)
        for t in range(NQT):
            x, x_b = xr6.next()
            dma("sp", x[:], xq[t * 128:(t + 1) * 128, :], writes=[x_b])
            y, y_b = yr6.next()
            s8, s8_b = st6.next()
            for g4 in range(4):
                ps, ps_b = pringA.next()
                for e_ in range(16):
                    op("pe", lambda e, e_=e_: e.matmul(ps[:, :], lhsT=oTs[:, e_, t * 128:(t + 1) * 128], rhs=wo[:, e_, g4 * 512:(g4 + 1) * 512],
                                                       start=(e_ == 0), stop=(e_ == 15)), reads=[oTs_b, wo_b], writes=[ps_b], sig=(e_ == 15))
                op("act", lambda e: e.activation(out=y[:, g4 * 512:(g4 + 1) * 512], in_=ps[:, :], func=AF.Square, accum_out=s8[:, g4:g4 + 1]),
                   reads=[ps_b], writes=[y_b, s8_b])
                op("dve", lambda e: e.tensor_copy(out=y[:, g4 * 512:(g4 + 1) * 512], in_=ps[:, :]), reads=[ps_b, y_b], writes=[y_b])
            op("dve", lambda e: e.tensor_reduce(out=s8[:, 4:5], in_=s8[:, 0:4], axis=AX.X, op=ALU.add), reads=[s8_b], writes=[s8_b])
            op("dve", lambda e: e.tensor_scalar(out=s8[:, 5:6], in0=s8[:, 4:5], scalar1=1.0 / D, scalar2=EPS, op0=ALU.mult, op1=ALU.add),
               reads=[s8_b], writes=[s8_b])
            op("act", lambda e: e.activation(out=s8[:, 6:7], in_=s8[:, 5:6], func=AF.Sqrt), reads=[s8_b], writes=[s8_b])
            op("dve", lambda e: e.reciprocal(out=s8[:, 7:8], in_=s8[:, 6:7]), reads=[s8_b], writes=[s8_b])
            op("dve", lambda e: e.scalar_tensor_tensor(out=y[:], in0=y[:], scalar=s8[:, 7:8], in1=gw[:], op0=ALU.mult, op1=ALU.mult),
               reads=[y_b, s8_b, gw_b], writes=[y_b])
            op("pool", lambda e: e.tensor_tensor(out=x[:], in0=x[:], in1=y[:], op=ALU.add), reads=[x_b, y_b], writes=[x_b])
            dma("sp", x1_d[t * 128:(t + 1) * 128, :], x[:], reads=[x_b], writes=[x1_db])
        SC.barrier()

    if stop_after == "p6":
        return _finish(nc, es, SC, out_buf)

    with ExitStack() as S7:
        h2T, h2T_b = _sb(nc, S7, "h2T", [128, KC, NQ], BF16)
        cwt, cwt_b = _sb(nc, S7, "cwt", [128, 128, 3], F32)
        cbt, cbt_b = _sb(nc, S7, "cbt", [128, 128], F32)
        uhs, uhs_b = _sb(nc, S7, "uhs", [128, 64, 2, 4], F32)
        qfl, qfl_b = _sb(nc, S7, "qfl", [128, 4], F32)
        dma("sp", qfl[:], qflag_row[:, 1024:1028], writes=[qfl_b])
        dma("sp", cwt[:], cw_l[:, :, :], writes=[cwt_b])
        dma("sp", cbt[:], cb_l[:, :], writes=[cbt_b])
        with ExitStack() as S7n:
            wmod, wmod_b = _sb(nc, S7n, "wmodf", [128, D], F32)
            shm, shm_b = _sb(nc, S7n, "shmf", [128, D], F32)
            xr = _ring(nc, S7n, "xrf", [128, D], F32, 2)
            tr = _ring(nc, S7n, "trf", [128, D], F32, 1)
            hbr = _ring(nc, S7n, "hbrf", [128, D], BF16, 2)
            st = _ring(nc, S7n, "stf", [128, 4], F32, 3)
            bcast_mod(wmod, wmod_b, 4, n_pre_ffn[0:1, :], True, tr.items[0][0], tr.items[0][1])
            bcast_mod(shm, shm_b, 3, None, False, None, None)
            for t in range(NQT):
                _norm_tile(x1_d[t * 128:(t + 1) * 128, :], [x1_db], wmod, wmod_b, shm, shm_b, h2T, h2T_b, t * 128, xr, tr, hbr, st)
            SC.barrier()
        if stop_after == "p7n":
            S7.close()
            return _finish(nc, es, SC, out_buf)
        gT, gT_b = _sb(nc, S7, "gT", [128, 64, 512], BF16)
        for gi, g0 in enumerate((0, 512)):
            with ExitStack() as S7u:
                wur = _ring(nc, S7u, "wu_%d" % gi, [128, KC, 256], BF16, 4)
                ugr = _ring(nc, S7u, "ug_%d" % gi, [128, 514], F32, 3)
                uvr = _ring(nc, S7u, "uv_%d" % gi, [128, 514], F32, 3)
                cgr = _ring(nc, S7u, "cg_%d" % gi, [128, 512], F32, 2)
                cvr = _ring(nc, S7u, "cv_%d" % gi, [128, 512], F32, 2)
                ggr = _ring(nc, S7u, "gg_%d" % gi, [128, 512], F32, 2)
                uhtr = _ring(nc, S7u, "uht_%d" % gi, [128, 256], F32, 2)
                for i in range(64):
                    wu, wu_b = wur.next()
                    for h4 in range(2):
                        dma("pool", wu[:, h4 * 8:(h4 + 1) * 8, :], w_up_l[i, :, h4 * 8:(h4 + 1) * 8, :], writes=[wu_b])
                    if gi == 0:
                        ph, ph_b = pringB.next()
                        for k in range(KC):
                            op("pe", lambda e, k=k: e.matmul(ph[:, 0:256], lhsT=h2T[:, k, 1024:1152], rhs=wu[:, k, :],
                                                             start=(k == 0), stop=(k == KC - 1)), reads=[wu_b, h2T_b], writes=[ph_b], sig=(k == KC - 1))
                        uht, uht_b = uhtr.next()
                        op("act", lambda e: e.copy(out=uht[:], in_=ph[:, 0:256]), reads=[ph_b], writes=[uht_b])
                        pt2, pt2_b = pringB.next()
                        for half in range(2):
                            op("pe", lambda e, half=half: e.transpose(out=pt2[:, half * 128:(half + 1) * 128], in_=uht[:, half * 128:(half + 1) * 128],
                                                                      identity=idf[:]), reads=[uht_b, idf_b], writes=[pt2_b], sig=(half == 1))
                        for half in range(2):
                            op("dve", lambda e, half=half: e.tensor_tensor(out=uhs[:, i, half, :], in0=pt2[:, half * 128:half * 128 + 4],
                                                                           in1=qfl[:, 0:4], op=ALU.mult), reads=[pt2_b, qfl_b], writes=[uhs_b])
                    ug, ug_b = ugr.next()
                    uv, uv_b = uvr.next()
                    for half, (u, u_b) in enumerate(((ug, ug_b), (uv, uv_b))):
                        ps, ps_b = pringA.next()
                        for k in range(KC):
                            op("pe", lambda e, k=k: e.matmul(ps[:, :], lhsT=wu[:, k, half * 128:(half + 1) * 128], rhs=h2T[:, k, g0:g0 + 512],
                                                             start=(k == 0), stop=(k == KC - 1)), reads=[wu_b, h2T_b], writes=[ps_b], sig=(k == KC - 1))
                        if half == 0:
                            op("act", lambda e: e.copy(out=u[:, 2:514], in_=ps[:, :]), reads=[ps_b], writes=[u_b])
                        else:
                            op("act", lambda e: e.copy(out=u[:, 2:514], in_=ps[:, :]), reads=[ps_b], writes=[u_b])
                        op("pool", lambda e: e.tensor_copy(out=u[:, 0:2], in_=uhs[:, i, half, 2 * gi:2 * gi + 2]), reads=[uhs_b], writes=[u_b])
                    cg, cg_b = cgr.next()
                    cv, cv_b = cvr.next()
                    for half, (u, u_b, cdst, cdst_b) in enumerate(((ug, ug_b, cg, cg_b), (uv, uv_b, cv, cv_b))):
                        ch = half * 64 + i
                        op("dve", lambda e: e.tensor_scalar(out=cdst[:], in0=u[:, 2:514], scalar1=cwt[:, ch, 2:3], scalar2=cbt[:, ch:ch + 1],
                                                            op0=ALU.mult, op1=ALU.add), reads=[u_b, cwt_b, cbt_b], writes=[cdst_b])
                        op("dve", lambda e: e.scalar_tensor_tensor(out=cdst[:], in0=u[:, 1:513], scalar=cwt[:, ch, 1:2], in1=cdst[:],
                                                                    op0=ALU.mult, op1=ALU.add), reads=[u_b, cwt_b, cdst_b], writes=[cdst_b])
                        op("dve", lambda e: e.scalar_tensor_tensor(out=cdst[:], in0=u[:, 0:512], scalar=cwt[:, ch, 0:1], in1=cdst[:],
                                                                    op0=ALU.mult, op1=ALU.add), reads=[u_b, cwt_b, cdst_b], writes=[cdst_b])
                    gg, gg_b = ggr.next()
                    op("act", lambda e: e.activation(out=gg[:], in_=cg[:], func=AF.Gelu_apprx_tanh), reads=[cg_b], writes=[gg_b])
                    op("dve", lambda e: e.tensor_tensor(out=gT[:, i, :], in0=gg[:], in1=cv[:], op=ALU.mult), reads=[gg_b, cv_b], writes=[gT_b])
                SC.barrier()
            if stop_after == "p7u":
                S7.close()
                return _finish(nc, es, SC, out_buf)
            with ExitStack() as S7d:
                wdr = _ring(nc, S7d, "wd_%d" % gi, [128, 8, 512], BF16, 3)
                y2, y2_b = _sb(nc, S7d, "y2_%d" % gi, [128, 4, D], F32)
                gwf, gwf_b = _sb(nc, S7d, "gwf_%d" % gi, [128, D], F32)
                tmpf, tmpf_b = _sb(nc, S7d, "tmpf_%d" % gi, [128, D], F32)
                x1r = _ring(nc, S7d, "x1t_%d" % gi, [128, D], F32, 2)
                ssq, ssq_b = _sb(nc, S7d, "ssq_%d" % gi, [128, 32], F32)
                bcast_mod(gwf, gwf_b, 5, n_post_ffn[0:1, :], False, tmpf, tmpf_b)
                for d4 in range(4):
                    accs = [pringB.next() for _ in range(4)]
                    for s8i in range(8):
                        wd, wd_b = wdr.next()
                        for h2 in range(2):
                            dma("pool", wd[:, h2 * 4:(h2 + 1) * 4, :], w_down[d4, s8i, :, h2 * 4:(h2 + 1) * 4, :], writes=[wd_b])
                        for c in range(8):
                            fc_ = s8i * 8 + c
                            for tt in range(4):
                                ac, ac_b = accs[tt]
                                op("pe", lambda e: e.matmul(ac[:, :], lhsT=gT[:, fc_, tt * 128:(tt + 1) * 128], rhs=wd[:, c, :],
                                                            start=(fc_ == 0), stop=(fc_ == 63)), reads=[gT_b, wd_b], writes=[ac_b], sig=(fc_ == 63 or tt == 3))
                    for tt in range(4):
                        ac, ac_b = accs[tt]
                        op("act", lambda e: e.activation(out=y2[:, tt, d4 * 512:(d4 + 1) * 512], in_=ac[:, :], func=AF.Square,
                                                         accum_out=ssq[:, tt * 8 + d4:tt * 8 + d4 + 1]),
                           reads=[ac_b], writes=[y2_b, ssq_b])
                        op("dve", lambda e: e.tensor_copy(out=y2[:, tt, d4 * 512:(d4 + 1) * 512], in_=ac[:, :]), reads=[ac_b, y2_b], writes=[y2_b])
                for tt in range(4):
                    b8 = tt * 8
                    op("dve", lambda e: e.tensor_reduce(out=ssq[:, b8 + 4:b8 + 5], in_=ssq[:, b8:b8 + 4], axis=AX.X, op=ALU.add), reads=[ssq_b], writes=[ssq_b])
                    op("dve", lambda e: e.tensor_scalar(out=ssq[:, b8 + 5:b8 + 6], in0=ssq[:, b8 + 4:b8 + 5], scalar1=1.0 / D, scalar2=EPS, op0=ALU.mult, op1=ALU.add),
                       reads=[ssq_b], writes=[ssq_b])
                    op("act", lambda e: e.activation(out=ssq[:, b8 + 6:b8 + 7], in_=ssq[:, b8 + 5:b8 + 6], func=AF.Sqrt), reads=[ssq_b], writes=[ssq_b])
                    op("dve", lambda e: e.reciprocal(out=ssq[:, b8 + 7:b8 + 8], in_=ssq[:, b8 + 6:b8 + 7]), reads=[ssq_b], writes=[ssq_b])
                    x1, x1_b = x1r.next()
                    row0 = g0 + tt * 128
                    dma("sp", x1[:], x1_d[row0:row0 + 128, :], reads=[x1_db], writes=[x1_b])
                    op("dve", lambda e: e.scalar_tensor_tensor(out=y2[:, tt, :], in0=y2[:, tt, :], scalar=ssq[:, tt * 8 + 7:tt * 8 + 8], in1=gwf[:],
                                                               op0=ALU.mult, op1=ALU.mult), reads=[y2_b, ssq_b, gwf_b], writes=[y2_b])
                    op("pool", lambda e: e.tensor_tensor(out=x1[:], in0=x1[:], in1=y2[:, tt, :], op=ALU.add), reads=[x1_b, y2_b], writes=[x1_b])
                    dma("sp", out[row0:row0 + 128, :], x1[:], reads=[x1_b], writes=[out_buf])
                SC.barrier()
            if stop_after == "p7d":
                S7.close()
                return _finish(nc, es, SC, out_buf)

    return _finish(nc, es, SC, out_buf)


def _finish(nc, es, SC, out_buf):
    SC.wait_all("sp", [out_buf])
    SC.barrier()
    es.close()
    return nc


def _qtok(j):
    ga, gb = j, 4 + j
    own = np.concatenate([np.arange(512 * ga, 512 * ga + 512), np.arange(512 * gb, 512 * gb + 512)])
    halo = np.full(128, 512 * gb - 1)
    flag = np.ones(NQ, np.float32)
    halo[2] = 512 * gb - 2
    halo[3] = 512 * gb - 1
    if ga > 0:
        halo[0] = 512 * ga - 2
        halo[1] = 512 * ga - 1
    else:
        halo[0] = 0
        halo[1] = 0
        flag[1024:1026] = 0.0
    return np.concatenate([own, halo]), flag


def make_in_maps(inp):
    x = np.asarray(inp["x"], np.float32)
    c = np.asarray(inp["c"], np.float32)
    pos = np.asarray(inp["positions"], np.int32)
    w_in = np.asarray(inp["w_in"], np.float32)[0]
    mq, mk, mv, nq = w_in[:, 0:1024], w_in[:, 1024:2048], w_in[:, 2048:3072], w_in[:, 3072:4096]
    kc, vc, ks, vs, kw, vw = [w_in[:, 4096 + 256 * i: 4096 + 256 * (i + 1)] for i in range(6)]
    ng = w_in[:, 5632:5656]
    w_k = np.concatenate([mk, kc, ks, kw, vc, mv, vs, vw], axis=1)
    w_k = np.ascontiguousarray(w_k.reshape(KC, 128, 7, 512).transpose(2, 1, 0, 3))
    w_q = np.concatenate([mq, nq], axis=1)
    w_q = np.ascontiguousarray(w_q.reshape(KC, 128, 4, 512).transpose(2, 1, 0, 3))
    w_g = np.ascontiguousarray(ng.reshape(KC, 128, 24).transpose(1, 0, 2))
    half = 64
    inv_freq = (10000.0 ** (-np.arange(half, dtype=np.float32) / half)).astype(np.float32)
    consts = np.zeros((128, 1024), np.float32)
    consts[:, 0:32] = 128.0 * np.arange(32)[None, :] + np.arange(128)[:, None]
    consts[:, 32:48] = 256.0 * (np.arange(16)[None, :] + 1)
    consts[:, 48:64] = 256.0 * np.arange(16)[None, :]
    consts[:, 64:128] = 64.0 * np.arange(64)[None, :]
    consts[:, 128:192] = 64.0 * (np.arange(64)[None, :] + 2)
    consts[:, 192] = 1.0
    consts[:, 256:511] = 16.0 * np.arange(255)[None, :] + 31.0
    aind = np.zeros((128, NKT * 128), np.float32)
    aind[np.arange(NKT * 128) // 64, np.arange(NKT * 128)] = 1.0
    w_up = np.asarray(inp["w_up"], np.float32)[0]
    w_up_l = np.stack([w_up[:, :DFF].reshape(D, 64, 128), w_up[:, DFF:].reshape(D, 64, 128)], axis=2).reshape(KC, 128, 64, 256)
    w_up_l = np.ascontiguousarray(w_up_l.transpose(2, 1, 0, 3))
    w_down_l = np.ascontiguousarray(np.asarray(inp["w_down"], np.float32)[0].reshape(8, 8, 128, 4, 512).transpose(3, 0, 2, 1, 4))
    w_ada_l = np.ascontiguousarray(np.asarray(inp["w_ada"], np.float32)[0].reshape(KC, 128, 24, 512).transpose(2, 1, 0, 3))
    w_out_l = np.ascontiguousarray(np.asarray(inp["w_out"], np.float32)[0].reshape(KC, 128, D).transpose(1, 0, 2))
    conv_w = np.asarray(inp["conv_w"], np.float32)[0]
    conv_b = np.asarray(inp["conv_b"], np.float32)[0]
    cw_l = np.ascontiguousarray(conv_w.reshape(3, 128, 128).transpose(2, 1, 0))
    cb_l = np.ascontiguousarray(conv_b.reshape(128, 128).T)
    shared = {
        "w_ada": w_ada_l,
        "b_ada": np.ascontiguousarray(inp["b_ada"], dtype=np.float32).reshape(1, -1),
        "n_pre_mix": np.asarray(inp["norm_pre_mix"], np.float32).reshape(1, -1),
        "n_post_mix": np.asarray(inp["norm_post_mix"], np.float32).reshape(1, -1),
        "n_pre_ffn": np.asarray(inp["norm_pre_ffn"], np.float32).reshape(1, -1),
        "n_post_ffn": np.asarray(inp["norm_post_ffn"], np.float32).reshape(1, -1),
        "w_k": w_k, "w_q": w_q, "w_g": w_g,
        "w_out": w_out_l,
        "ident_f": np.eye(128, dtype=np.float32),
        "ident_b": np.eye(128, dtype=np.float32).astype(ml_dtypes.bfloat16),
        "invf": np.ascontiguousarray(np.broadcast_to(inv_freq[None, :], (128, 64))),
        "consts": consts,
        "aind": aind.astype(ml_dtypes.bfloat16),
        "cmp_w1_k": np.ascontiguousarray(inp["cmp_w1_k"][0], dtype=np.float32),
        "cmp_w2_k": np.ascontiguousarray(inp["cmp_w2_k"][0], dtype=np.float32),
        "cmp_w1_v": np.ascontiguousarray(inp["cmp_w1_v"][0], dtype=np.float32),
        "cmp_w2_v": np.ascontiguousarray(inp["cmp_w2_v"][0], dtype=np.float32),
        "cpos_k": np.ascontiguousarray(np.asarray(inp["cmp_pos_k"], np.float32)[0].T),
        "cpos_v": np.ascontiguousarray(np.asarray(inp["cmp_pos_v"], np.float32)[0].T),
        "w_up_l": w_up_l,
        "w_down": w_down_l,
        "cw_l": cw_l, "cb_l": cb_l,
    }
    maps = []
    for core in range(8):
        b, j = core // 4, core % 4
        qt, flag = _qtok(j)
        m = dict(shared)
        m["xall"] = np.ascontiguousarray(x[b])
        m["xq"] = np.ascontiguousarray(x[b][qt])
        m["posall"] = np.ascontiguousarray(pos[b].reshape(NKT, 128).T)
        m["posq"] = np.ascontiguousarray(pos[b][qt].reshape(NQT, 128).T)
        qf = qt.astype(np.float32)
        m["qpos_col"] = np.ascontiguousarray(qf.reshape(NQT, 128).T)
        m["qpos_row"] = np.ascontiguousarray(np.broadcast_to(qf[None, :], (128, NQ)))
        m["qflag_row"] = np.ascontiguousarray(np.broadcast_to(flag[None, :], (128, NQ)))
        m["c_col"] = np.ascontiguousarray(c[b].reshape(KC, 128).T)
        maps.append(m)
    return maps


def kernel(**inputs):
    nc = build_program()
    maps = make_in_maps(inputs)
    res = run_bass_kernel_spmd(nc, maps, core_ids=list(range(8)))
    outp = np.zeros((2, S, D), np.float32)
    for core in range(8):
        b, j = core // 4, core % 4
        o = res.results[core]["out"]
        outp[b, 512 * j:512 * j + 512] = o[0:512]
        outp[b, 512 * (4 + j):512 * (4 + j) + 512] = o[512:1024]
    return outp
```
